# Optimizing a Trainium2 kernel written in Bass

```python
import math
import jax, jax.numpy as jnp
from jax import lax
import numpy as np

D_MODEL = 1024
BATCH = 8
SEQ = 4096
DEPTH = 4

CTX_LEN = 256
GRID_W = 64
CHUNK = 64
EPS = 1e-6
N_MOD = 6

GDN_HEADS = 4
GDN_DK = 128
GDN_DV = 128
GDN_W = GDN_HEADS * GDN_DV
CONV_K = 3

RWKV_HEADS = 8
RWKV_N = 64
RWKV_W = RWKV_HEADS * RWKV_N
DECAY_LORA = 64
ICL_LORA = 64
GATE_LORA = 128
RWKV_GN_EPS = 64e-5

MLSTM_HEADS = 4
MLSTM_DQK = 64
MLSTM_DV = 128
MLSTM_QK_W = MLSTM_HEADS * MLSTM_DQK
MLSTM_V_W = MLSTM_HEADS * MLSTM_DV

N_BRANCH = 3
BRANCH_W = 512

GDN_COLS = 4 * GDN_W + 4 * GDN_HEADS
RWKV_COLS = 3 * RWKV_W + 2 * DECAY_LORA + 2 * ICL_LORA + GATE_LORA
MLSTM_COLS = 2 * MLSTM_QK_W + 2 * MLSTM_V_W + 4 * MLSTM_HEADS
GATE_COLS = N_BRANCH * D_MODEL
IN_COLS = GDN_COLS + RWKV_COLS + MLSTM_COLS + GATE_COLS

N_EXPERTS = 16
CAP_FACTOR = 2
MOE_D_FF = 512

kernel_name = "hybrid_gdn_rwkv7_mlstm_ec_moe_dit"


def split_cols(p, sizes):
    return jnp.split(p, np.cumsum(sizes)[:-1].tolist(), axis=-1)


def rmsnorm(x, g):
    xf = x.astype(jnp.float32)
    y = xf * lax.rsqrt(jnp.mean(xf * xf, axis=-1, keepdims=True) + EPS)
    return (y * g.astype(jnp.float32)).astype(x.dtype)


def l2norm(x):
    xf = x.astype(jnp.float32)
    return (xf * lax.rsqrt(jnp.sum(xf * xf, axis=-1, keepdims=True) + EPS)).astype(x.dtype)


def modulate(h, shift, scale):
    return h * (1 + scale[:, None]) + shift[:, None]


def dwconv_grid(t, w, rows):
    B, T, C = t.shape
    img = t.reshape(B, rows, T // rows, C)
    out = lax.conv_general_dilated(img, w[:, :, None, :].astype(t.dtype), (1, 1), 'SAME',
                                   dimension_numbers=('NHWC', 'HWIO', 'NHWC'),
                                   feature_group_count=C)
    return out.reshape(B, T, C)


def qshift(p, rows):
    B, T, C = p.shape
    g = p.reshape(B, rows, T // rows, C // 4, 4)
    z_col = jnp.zeros_like(g[:, :, :1, :, 0])
    z_row = jnp.zeros_like(g[:, :1, :, :, 0])
    from_left = jnp.concatenate([z_col, g[:, :, :-1, :, 0]], axis=2)
    from_right = jnp.concatenate([g[:, :, 1:, :, 1], z_col], axis=2)
    from_up = jnp.concatenate([z_row, g[:, :-1, :, :, 2]], axis=1)
    from_down = jnp.concatenate([g[:, 1:, :, :, 3], z_row], axis=1)
    return jnp.stack([from_left, from_right, from_up, from_down], axis=-1).reshape(B, T, C)


def to_chunks(t):
    B, T, H = t.shape[:3]
    t = t.reshape(B, T // CHUNK, CHUNK, H, *t.shape[3:])
    return jnp.moveaxis(t, (1, 3), (0, 2))


def from_chunks(t):
    t = jnp.moveaxis(t, (0, 2), (1, 3))
    B, n, L, H = t.shape[:4]
    return t.reshape(B, n * L, H, *t.shape[4:])


def run_two_directions(scan_fn, ctx_dirs, lat_dirs, state0):
    yc, yx = 0, 0
    for d in range(2):
        f = (lambda t: jnp.flip(t, axis=1)) if d == 1 else (lambda t: t)
        yc_d, state = scan_fn(tuple(f(t) for t in ctx_dirs[d]), state0)
        yx_d, _ = scan_fn(tuple(f(t) for t in lat_dirs[d]), state)
        yc = yc + f(yc_d)
        yx = yx + f(yx_d)
    return yc, yx


def gdn_chunked(inp, S0):
    dtype = inp[2].dtype
    q, k, v, g, beta = [to_chunks(t.astype(jnp.float32)) for t in inp]
    dv = v.shape[-1]
    G = jnp.cumsum(g, axis=-1)
    idx = jnp.arange(CHUNK)
    causal = idx[:, None] >= idx[None, :]
    strict = idx[:, None] > idx[None, :]
    decay = jnp.exp(jnp.where(causal, G[..., :, None] - G[..., None, :], -jnp.inf))
    kb = k * beta[..., None]
    Lm = jnp.where(strict, jnp.einsum('nbhik,nbhjk->nbhij', kb, k) * decay, 0.0)
    M = Lm + jnp.eye(CHUNK, dtype=jnp.float32)
    rhs = jnp.concatenate([v * beta[..., None], kb * jnp.exp(G)[..., None]], axis=-1)
    sol = lax.linalg.triangular_solve(M, rhs, left_side=True, lower=True, unit_diagonal=True)
    u, w = sol[..., :dv], sol[..., dv:]
    Aqk = jnp.einsum('nbhik,nbhjk->nbhij', q, k) * decay
    qg = q * jnp.exp(G)[..., None]
    g_last = G[..., -1]
    kg = k * jnp.exp(g_last[..., None] - G)[..., None]

    def step(S, xs):
        u_c, w_c, qg_c, A_c, kg_c, gl = xs
        v_new = u_c - jnp.einsum('bhlk,bhkv->bhlv', w_c, S)
        o = jnp.einsum('bhlk,bhkv->bhlv', qg_c, S) + jnp.einsum('bhij,bhjv->bhiv', A_c, v_new)
        S = S * jnp.exp(gl)[..., None, None] + jnp.einsum('bhlk,bhlv->bhkv', kg_c, v_new)
        return S, o

    S, o = lax.scan(step, S0, (u, w, qg, Aqk, kg, g_last))
    return from_chunks(o).astype(dtype), S


def rwkv7_scan(inp, S0):
    dtype = inp[3].dtype
    xs = tuple(jnp.moveaxis(t.astype(jnp.float32), 1, 0) for t in inp)

    def step(S, xt):
        r_t, w_t, k_t, v_t, a_t, b_t = xt
        sa = jnp.einsum('bhvk,bhk->bhv', S, a_t)
        S = S * w_t[:, :, None, :] + sa[..., None] * b_t[:, :, None, :] + v_t[..., None] * k_t[:, :, None, :]
        return S, jnp.einsum('bhvk,bhk->bhv', S, r_t)

    S, y = lax.scan(step, S0, xs)
    return jnp.moveaxis(y, 0, 1).astype(dtype), S


def mlstm_chunked(inp, state):
    dtype = inp[2].dtype
    q, k, v, ig, lf = [to_chunks(t.astype(jnp.float32)) for t in inp]
    idx = jnp.arange(CHUNK)
    causal = idx[:, None] >= idx[None, :]
    b = jnp.cumsum(lf, axis=-1)
    Dm = jnp.where(causal, b[..., :, None] - b[..., None, :] + ig[..., None, :], -jnp.inf)
    m_intra = jnp.max(Dm, axis=-1)
    qk = jnp.einsum('nbhik,nbhjk->nbhij', q, k)
    b_last = b[..., -1]
    dl = b_last[..., None] - b + ig

    def step(carry, xs):
        C, nv, m = carry
        q_c, k_c, v_c, b_c, D_c, mi_c, qk_c, dl_c, bl_c = xs
        m_t = jnp.maximum(b_c + m[..., None], mi_c)
        inter = jnp.exp(b_c + m[..., None] - m_t)
        P = qk_c * jnp.exp(D_c - m_t[..., None])
        num = inter[..., None] * jnp.einsum('bhlk,bhkv->bhlv', q_c, C) + jnp.einsum('bhij,bhjv->bhiv', P, v_c)
        den = inter * jnp.einsum('bhlk,bhk->bhl', q_c, nv) + jnp.sum(P, axis=-1)
        h = num / jnp.maximum(jnp.abs(den), jnp.exp(-m_t))[..., None]
        m_new = jnp.maximum(bl_c + m, jnp.max(dl_c, axis=-1))
        wk = k_c * jnp.exp(dl_c - m_new[..., None])[..., None]
        dec = jnp.exp(bl_c + m - m_new)
        C = dec[..., None, None] * C + jnp.einsum('bhlk,bhlv->bhkv', wk, v_c)
        nv = dec[..., None] * nv + jnp.sum(wk, axis=-2)
        return (C, nv, m_new), h

    state, h = lax.scan(step, state, (q, k, v, b, Dm, m_intra, qk, dl, b_last))
    return from_chunks(h).astype(dtype), state


def gdn_branch(pc, px, rows_x, conv_w, a_log, dt_bias, norm_g):
    def prep(p, rows):
        B, T, _ = p.shape
        qkv, z, bg, ag = split_cols(p, (3 * GDN_W, GDN_W, 2 * GDN_HEADS, 2 * GDN_HEADS))
        qkv = jax.nn.silu(dwconv_grid(qkv, conv_w, rows))
        q, k, v = [t.reshape(B, T, GDN_HEADS, GDN_DK) for t in jnp.split(qkv, 3, axis=-1)]
        q = l2norm(q) * (GDN_DK ** -0.5)
        k = l2norm(k)
        beta = jax.nn.sigmoid(bg.reshape(B, T, 2, GDN_HEADS))
        g = -jnp.exp(a_log) * jax.nn.softplus(ag.reshape(B, T, 2, GDN_HEADS) + dt_bias)
        return [(q, k, v, g[:, :, d], beta[:, :, d]) for d in range(2)], z

    dc, zc = prep(pc, 1)
    dx, zx = prep(px, rows_x)
    S0 = jnp.zeros((pc.shape[0], GDN_HEADS, GDN_DK, GDN_DV), jnp.float32)
    oc, ox = run_two_directions(gdn_chunked, dc, dx, S0)

    def finish(o, z):
        B, T = o.shape[:2]
        return (rmsnorm(o, norm_g) * jax.nn.silu(z.reshape(B, T, GDN_HEADS, GDN_DV))).reshape(B, T, GDN_W)

    return finish(oc, zc), finish(ox, zx)


def rwkv_branch(pc, px, rows_x, mu, w0, w_up, a0, a_up, g_up, k_k, k_a, r_k, ln_g, ln_b):
    def prep(p, rows):
        B, T, _ = p.shape
        p = p + (qshift(p, rows) - p) * mu
        r, k, v, wd, ad, gd = split_cols(p, (RWKV_W, RWKV_W, RWKV_W, 2 * DECAY_LORA, 2 * ICL_LORA, GATE_LORA))
        hd = lambda t: t.reshape(B, T, RWKV_HEADS, RWKV_N)
        kk = l2norm(hd(k * k_k))
        g = jax.nn.sigmoid(gd) @ g_up
        wd = wd.reshape(B, T, 2, DECAY_LORA)
        ad = ad.reshape(B, T, 2, ICL_LORA)
        dirs, bonus = [], 0
        for d in range(2):
            w_raw = w0[d] + jnp.tanh(wd[:, :, d]) @ w_up[d]
            decay = jnp.exp(-jnp.exp(-jax.nn.softplus(-w_raw) - 0.5))
            a = jax.nn.sigmoid(a0[d] + ad[:, :, d] @ a_up[d])
            kd = hd(k * (1 + (a - 1) * k_a))
            dirs.append((hd(r), hd(decay), kd, hd(v), -kk, kk * hd(a)))
            bonus = bonus + jnp.sum(hd(r) * kd * r_k, axis=-1, keepdims=True) * hd(v)
        return dirs, bonus, g

    dc, bc, gc = prep(pc, 1)
    dx, bx, gx = prep(px, rows_x)
    S0 = jnp.zeros((pc.shape[0], RWKV_HEADS, RWKV_N, RWKV_N), jnp.float32)
    yc, yx = run_two_directions(rwkv7_scan, dc, dx, S0)

    def finish(y, bonus, g):
        B, T = y.shape[:2]
        yf = y.astype(jnp.float32)
        m = jnp.mean(yf, axis=-1, keepdims=True)
        var = jnp.mean(jnp.square(yf - m), axis=-1, keepdims=True)
        yn = ((yf - m) * lax.rsqrt(var + RWKV_GN_EPS)).reshape(B, T, RWKV_W) * ln_g + ln_b
        return (yn.astype(g.dtype) + bonus.reshape(B, T, RWKV_W)) * g

    return finish(yc, bc, gc), finish(yx, bx, gx)


def mlstm_branch(pc, px, i_bias, f_bias, norm_g):
    def prep(p):
        B, T, _ = p.shape
        q, k, v, o, ig, fg = split_cols(p, (MLSTM_QK_W, MLSTM_QK_W, MLSTM_V_W, MLSTM_V_W, 2 * MLSTM_HEADS, 2 * MLSTM_HEADS))
        q = q.reshape(B, T, MLSTM_HEADS, MLSTM_DQK) * (MLSTM_DQK ** -0.5)
        k = k.reshape(B, T, MLSTM_HEADS, MLSTM_DQK)
        v = v.reshape(B, T, MLSTM_HEADS, MLSTM_DV)
        ig = ig.reshape(B, T, 2, MLSTM_HEADS) + i_bias
        lf = jax.nn.log_sigmoid(fg.reshape(B, T, 2, MLSTM_HEADS) + f_bias)
        return [(q, k, v, ig[:, :, d], lf[:, :, d]) for d in range(2)], jax.nn.sigmoid(o)

    dc, oc = prep(pc)
    dx, ox = prep(px)
    B = pc.shape[0]
    state0 = (jnp.zeros((B, MLSTM_HEADS, MLSTM_DQK, MLSTM_DV), jnp.float32),
              jnp.zeros((B, MLSTM_HEADS, MLSTM_DQK), jnp.float32),
              jnp.zeros((B, MLSTM_HEADS), jnp.float32))
    hc, hx = run_two_directions(mlstm_chunked, dc, dx, state0)

    def finish(h, o):
        Bh, T = h.shape[:2]
        return rmsnorm(h, norm_g).reshape(Bh, T, MLSTM_V_W) * o

    return finish(hc, oc), finish(hx, ox)


def merge_branches(branches, gate_pre, w_branch, w_out):
    gates = jnp.split(gate_pre, N_BRANCH, axis=-1)
    merged = sum(jax.nn.sigmoid(gates[i]) * (branches[i] @ w_branch[i]) for i in range(N_BRANCH))
    return merged @ w_out


def moe_ec(h, router_w, w1, w3, w2):
    B, n, _ = h.shape
    cap = max(1, CAP_FACTOR * n // N_EXPERTS)
    probs = jax.nn.softmax((h @ router_w).astype(jnp.float32), axis=-1)
    aff, idx = lax.top_k(jnp.swapaxes(probs, 1, 2), cap)
    bidx = jnp.arange(B)[:, None, None]
    xe = h[bidx, idx]
    hid = jax.nn.silu(jnp.einsum('becd,edf->becf', xe, w1)) * jnp.einsum('becd,edf->becf', xe, w3)
    ye = jnp.einsum('becf,efd->becd', hid, w2) * aff[..., None].astype(h.dtype)
    return jnp.zeros_like(h).at[bidx, idx].add(ye)


def setup_inputs(seed: int = 0) -> dict:
    key = jax.random.key(seed)
    ks = jax.random.split(key, 34)
    f32 = jnp.float32
    L = DEPTH

    def nrm(i, shape, scale=1.0):
        return scale * jax.random.normal(ks[i], shape, f32)

    def unif(i, shape, lo, hi):
        return jax.random.uniform(ks[i], shape, f32, lo, hi)

    dt = jnp.exp(unif(11, (L, 2, GDN_HEADS), math.log(1e-3), math.log(1e-1)))
    return {
        "x": nrm(0, (BATCH, SEQ, D_MODEL)),
        "c": nrm(1, (BATCH, D_MODEL)),
        "ctx": nrm(2, (BATCH, CTX_LEN, D_MODEL)),
        "c_ctx": nrm(3, (D_MODEL,)),
        "ada_w": nrm(4, (L, D_MODEL, N_MOD * D_MODEL), 0.5 * D_MODEL ** -0.5),
        "ada_b": nrm(5, (L, N_MOD * D_MODEL), 0.02),
        "norm1_g": 1.0 + nrm(6, (L, D_MODEL), 0.02),
        "norm2_g": 1.0 + nrm(7, (L, D_MODEL), 0.02),
        "w_in": nrm(8, (L, D_MODEL, IN_COLS), D_MODEL ** -0.5),
        "gdn_conv": nrm(9, (L, CONV_K, CONV_K, 3 * GDN_W), 1.0 / CONV_K),
        "gdn_a_log": jnp.log(unif(10, (L, 2, GDN_HEADS), 1.0, 16.0)),
        "gdn_dt_bias": dt + jnp.log(-jnp.expm1(-dt)),
        "gdn_norm_g": 1.0 + nrm(12, (L, GDN_DV), 0.02),
        "rwkv_mu": unif(13, (L, RWKV_COLS), 0.0, 1.0),
        "rwkv_w0": jnp.linspace(-6.5, -1.5, RWKV_W, dtype=f32) + nrm(14, (L, 2, RWKV_W), 0.1),
        "rwkv_w_up": nrm(15, (L, 2, DECAY_LORA, RWKV_W), 0.1),
        "rwkv_a0": nrm(16, (L, 2, RWKV_W), 0.1),
        "rwkv_a_up": nrm(17, (L, 2, ICL_LORA, RWKV_W), 0.1),
        "rwkv_g_up": nrm(18, (L, GATE_LORA, RWKV_W), GATE_LORA ** -0.5),
        "rwkv_k_k": 0.85 + nrm(19, (L, RWKV_W), 0.05),
        "rwkv_k_a": 1.0 + nrm(20, (L, RWKV_W), 0.05),
        "rwkv_r_k": -0.04 + nrm(21, (L, RWKV_HEADS, RWKV_N), 0.05),
        "rwkv_ln_g": 1.0 + nrm(22, (L, RWKV_W), 0.02),
        "rwkv_ln_b": nrm(23, (L, RWKV_W), 0.02),
        "mlstm_i_bias": nrm(24, (L, 2, MLSTM_HEADS), 0.1),
        "mlstm_f_bias": jnp.linspace(3.0, 6.0, MLSTM_HEADS, dtype=f32) + nrm(25, (L, 2, MLSTM_HEADS), 0.1),
        "mlstm_norm_g": 1.0 + nrm(26, (L, MLSTM_DV), 0.02),
        "w_branch": nrm(27, (L, N_BRANCH, BRANCH_W, D_MODEL), BRANCH_W ** -0.5),
        "w_out": nrm(28, (L, D_MODEL, D_MODEL), D_MODEL ** -0.5),
        "router_w": nrm(29, (L, D_MODEL, N_EXPERTS), D_MODEL ** -0.5),
        "moe_w1": nrm(30, (L, N_EXPERTS, D_MODEL, MOE_D_FF), D_MODEL ** -0.5),
        "moe_w3": nrm(31, (L, N_EXPERTS, D_MODEL, MOE_D_FF), D_MODEL ** -0.5),
        "moe_w2": nrm(32, (L, N_EXPERTS, MOE_D_FF, D_MODEL), MOE_D_FF ** -0.5),
        "final_g": 1.0 + nrm(33, (D_MODEL,), 0.02),
    }


def reference(x, c, ctx, c_ctx, ada_w, ada_b, norm1_g, norm2_g, w_in, gdn_conv, gdn_a_log,
              gdn_dt_bias, gdn_norm_g, rwkv_mu, rwkv_w0, rwkv_w_up, rwkv_a0, rwkv_a_up, rwkv_g_up,
              rwkv_k_k, rwkv_k_a, rwkv_r_k, rwkv_ln_g, rwkv_ln_b, mlstm_i_bias, mlstm_f_bias,
              mlstm_norm_g, w_branch, w_out, router_w, moe_w1, moe_w3, moe_w2, final_g):
    rows = x.shape[1] // GRID_W
    s_lat = jax.nn.silu(c)
    s_ctx = jax.nn.silu(c_ctx)[None]
    col_groups = (GDN_COLS, RWKV_COLS, MLSTM_COLS, GATE_COLS)
    for l in range(DEPTH):
        last = l == DEPTH - 1
        mod_x = jnp.split(s_lat @ ada_w[l] + ada_b[l], N_MOD, axis=-1)
        mod_c = jnp.split(s_ctx @ ada_w[l] + ada_b[l], N_MOD, axis=-1)
        hx = modulate(rmsnorm(x, norm1_g[l]), mod_x[0], mod_x[1])
        hc = modulate(rmsnorm(ctx, norm1_g[l]), mod_c[0], mod_c[1])
        gdn_x, rwkv_x, mlstm_x, gate_x = split_cols(hx @ w_in[l], col_groups)
        gdn_c, rwkv_c, mlstm_c, gate_c = split_cols(hc @ w_in[l], col_groups)
        a_c, a_x = gdn_branch(gdn_c, gdn_x, rows, gdn_conv[l], gdn_a_log[l], gdn_dt_bias[l], gdn_norm_g[l])
        b_c, b_x = rwkv_branch(rwkv_c, rwkv_x, rows, rwkv_mu[l], rwkv_w0[l], rwkv_w_up[l], rwkv_a0[l],
                               rwkv_a_up[l], rwkv_g_up[l], rwkv_k_k[l], rwkv_k_a[l], rwkv_r_k[l],
                               rwkv_ln_g[l], rwkv_ln_b[l])
        m_c, m_x = mlstm_branch(mlstm_c, mlstm_x, mlstm_i_bias[l], mlstm_f_bias[l], mlstm_norm_g[l])
        x = x + mod_x[2][:, None] * merge_branches((a_x, b_x, m_x), gate_x, w_branch[l], w_out[l])
        h2x = modulate(rmsnorm(x, norm2_g[l]), mod_x[3], mod_x[4])
        x = x + mod_x[5][:, None] * moe_ec(h2x, router_w[l], moe_w1[l], moe_w3[l], moe_w2[l])
        if not last:
            ctx = ctx + mod_c[2][:, None] * merge_branches((a_c, b_c, m_c), gate_c, w_branch[l], w_out[l])
            h2c = modulate(rmsnorm(ctx, norm2_g[l]), mod_c[3], mod_c[4])
            ctx = ctx + mod_c[5][:, None] * moe_ec(h2c, router_w[l], moe_w1[l], moe_w3[l], moe_w2[l])
    return rmsnorm(x, final_g)
```

```python
import math
import numpy as np
import concourse.bass as bass
import concourse.mybir as mybir
from concourse.bass_utils import run_bass_kernel_spmd

F32 = mybir.dt.float32
BF16 = mybir.dt.bfloat16
U32 = mybir.dt.uint32
I32 = mybir.dt.int32
F32R = mybir.dt.float32r


def f32(ap):
    return ap.bitcast(F32)
AF = mybir.ActivationFunctionType
ALU = mybir.AluOpType
AX = mybir.AxisListType

D = 1024
NMOD = 6
GH, GDK = 4, 128
RH, RN = 8, 64
MH, MDQK, MDV = 4, 64, 128
GDN_COLS = 4 * 512 + 16
RWKV_COLS = 3 * 512 + 128 + 128 + 128
MLSTM_COLS = 2 * 256 + 2 * 512 + 16
GATE_COLS = 3 * D
IN_COLS = GDN_COLS + RWKV_COLS + MLSTM_COLS + GATE_COLS
NE = 16
DFF = 512
EPS = 1e-6
GN_EPS = 64e-5
P = 128


class Buf:
    __slots__ = ("name", "w", "r", "multi", "ws")

    def __init__(self, name, multi=False):
        self.name = name
        self.w = None
        self.r = {}
        self.multi = multi
        self.ws = {}


class KB:
    def __init__(self):
        nc = bass.Bass("TRN2", target_bir_lowering=False)
        self.nc = nc
        self.eng = {"pe": nc.tensor, "dve": nc.vector, "act": nc.scalar, "pool": nc.gpsimd, "sp": nc.sync}
        self.sems = {}
        self.cnt = {}
        for e in ("pe", "dve", "act", "pool"):
            self.sems[e] = nc.semaphore("s_" + e).__enter__()
            self.cnt[e] = 0
        self.ndma = 20
        for i in range(self.ndma):
            k = "d%d" % i
            self.sems[k] = nc.semaphore("s_" + k).__enter__()
            self.cnt[k] = 0
        self.dma_rr = 0
        self.seen = {e: {} for e in self.eng}
        self.ninstr = 0
        self._uid = 0
        self.guards = []
        self.banks = []
        self.bank_rr = 0

    def init_psum(self):
        for i in range(8):
            t = self.nc.psum_tensor("bank%d" % i, [P, 512], F32).__enter__()
            self.banks.append((t, Buf("bank%d" % i)))

    def bank(self):
        g = getattr(self, "group", None)
        if g is None:
            b = self.banks[self.bank_rr]
            self.bank_rr = (self.bank_rr + 1) % 8
            return b
        i = self.grr[g]
        self.grr[g] = (i + 1) % 4
        return self.banks[g * 4 + i]

    def run_chains(self, chains):
        self.grr = [0, 0]
        active = list(enumerate(chains))
        while active:
            for it in list(active):
                self.group = it[0]
                try:
                    next(it[1])
                except StopIteration:
                    active.remove(it)
        self.group = None

    def barrier(self):
        allk = [(k, v) for k, v in self.cnt.items() if v > 0]
        for e in self.eng:
            self._wait(e, allk)

    def mark(self):
        return len(self.guards)

    def release(self, m):
        self.barrier()
        while len(self.guards) > m:
            g = self.guards.pop()
            g.__exit__(None, None, None)

    def sb(self, shape, dt=F32, name=None):
        self._uid += 1
        g = self.nc.sbuf_tensor("%s_%d" % (name or "sb", self._uid), list(shape), dt)
        t = g.__enter__()
        self.guards.append(g)
        return t, Buf(name or "sb")

    def ps(self, shape, dt=F32, name=None):
        self._uid += 1
        t = self.nc.psum_tensor("%s_%d" % (name or "ps", self._uid), list(shape), dt).__enter__()
        return t, Buf(name or "ps")

    def dram(self, name, shape, dt=F32, kind="Internal"):
        return self.nc.dram_tensor(name, list(shape), dt, kind=kind).ap()

    def _wait(self, e, deps):
        eng = self.eng[e]
        seen = self.seen[e]
        best = {}
        for d in deps:
            if d is None:
                continue
            k, v = d
            if best.get(k, 0) < v:
                best[k] = v
        for k, v in best.items():
            if e == "pe" and k == "pe":
                continue
            if seen.get(k, 0) < v:
                eng.wait_ge(self.sems[k], v)
                self.ninstr += 1
                seen[k] = v

    def _deps(self, reads, writes):
        deps = []
        for b in reads:
            deps.append(b.w)
            if b.multi:
                deps.extend(b.ws.items())
        for b in writes:
            if not b.multi:
                deps.append(b.w)
            for k, v in b.r.items():
                deps.append((k, v))
        return deps

    def _commit(self, key, val, reads, writes):
        for b in reads:
            if b.r.get(key, 0) < val:
                b.r[key] = val
        for b in writes:
            b.w = (key, val)
            b.r = {}
            if b.multi:
                if b.ws.get(key, 0) < val:
                    b.ws[key] = val

    def op(self, e, fn, reads=(), writes=()):
        self._wait(e, self._deps(reads, writes))
        ins = fn()
        self.cnt[e] += 1
        ins.then_inc(self.sems[e], 1)
        self.ninstr += 1
        self._commit(e, self.cnt[e], reads, writes)
        return ins

    def dma(self, q, out, in_, reads=(), writes=(), **kw):
        k = "d%d" % self.dma_rr
        self.dma_rr = (self.dma_rr + 1) % self.ndma
        deps = self._deps(reads, writes)
        if self.cnt[k] > 0:
            deps.append((k, self.cnt[k]))
        self._wait(q, deps)
        ins = self.eng[q].dma_start(out=out, in_=in_, **kw)
        self.cnt[k] += 16
        ins.then_inc(self.sems[k], 16)
        self.ninstr += 1
        self._commit(k, self.cnt[k], reads, writes)
        return ins

    def idma(self, out, out_offset, in_, in_offset, reads=(), writes=(), **kw):
        k = "d%d" % self.dma_rr
        self.dma_rr = (self.dma_rr + 1) % self.ndma
        deps = self._deps(reads, writes)
        if self.cnt[k] > 0:
            deps.append((k, self.cnt[k]))
        self._wait("pool", deps)
        ins = self.nc.gpsimd.indirect_dma_start(out=out, out_offset=out_offset, in_=in_, in_offset=in_offset, **kw)
        self.cnt[k] += 16
        ins.then_inc(self.sems[k], 16)
        self.ninstr += 1
        self._commit(k, self.cnt[k], reads, writes)
        return ins

    def finish(self, bufs):
        deps = []
        for b in bufs:
            deps.append(b.w)
        self._wait("sp", deps)


WNAMES = ["ada_w", "ada_b", "norm1_g", "norm2_g", "w_in", "gdn_conv", "gdn_a_log", "gdn_dt_bias",
          "gdn_norm_g", "rwkv_mu", "rwkv_w0", "rwkv_w_up", "rwkv_a0", "rwkv_a_up", "rwkv_g_up",
          "rwkv_k_k", "rwkv_k_a", "rwkv_r_k", "rwkv_ln_g", "rwkv_ln_b", "mlstm_i_bias",
          "mlstm_f_bias", "mlstm_norm_g", "w_branch", "w_out", "router_w", "moe_w1", "moe_w3",
          "moe_w2", "final_g"]


def build(TC, TL, depth, shapes, dbg=(), upto=99):
    kb = KB()
    nc = kb.nc
    kb.init_psum()
    NT = TC + TL
    NCT, NTT = TC // P, NT // P
    V, A, G, T = (lambda f, r=(), w=(): kb.op("dve", f, r, w)), (lambda f, r=(), w=(): kb.op("act", f, r, w)), \
        (lambda f, r=(), w=(): kb.op("pool", f, r, w)), (lambda f, r=(), w=(): kb.op("pe", f, r, w))
    vec, act, pool, pe = nc.vector, nc.scalar, nc.gpsimd, nc.tensor

    def mm(ps, lhsT, rhs, st, sp, r, w):
        return T(lambda: pe.matmul(ps, lhsT=lhsT, rhs=rhs, start=st, stop=sp), r, w)

    I = {}
    I["x"] = nc.dram_tensor("x", [TL, D], F32, kind="ExternalInput").ap()
    I["c"] = nc.dram_tensor("c", [1, D], F32, kind="ExternalInput").ap()
    I["ctx"] = nc.dram_tensor("ctx", [TC, D], F32, kind="ExternalInput").ap()
    I["c_ctx"] = nc.dram_tensor("c_ctx", [1, D], F32, kind="ExternalInput").ap()
    for n in WNAMES:
        I[n] = nc.dram_tensor(n, list(shapes[n]), F32, kind="ExternalInput").ap()
    OUT = nc.dram_tensor("out", [TL, D], F32, kind="ExternalOutput").ap()
    bOUT = Buf("out")

    def scratch(name, shape, dt=F32):
        k = "ExternalOutput" if name in dbg else "Internal"
        return nc.dram_tensor(name, list(shape), dt, kind=k).ap(), Buf(name, multi=(name != "YA"))

    NTM = IN_COLS - 1536
    XR, bXR = scratch("XR", [NT, D])
    PTM, bPTM = scratch("PTM", [NT + 192, NTM])
    PQT, bPQT = scratch("PQT", [1536, NT])
    QKV, bQKV = scratch("QKV", [NT, 1536])
    OGG, bOGG = scratch("OGG", [2, NT, 512])
    OGM, bOGM = scratch("OGM", [2, NT, 512])
    OGR, bOGR = scratch("OGR", [2, NT, 512])
    BR, bBR = scratch("BR", [NT, 1536])
    RWP, bRWP = scratch("RWP", [NT, 9 * 512])
    RWF, bRWF = scratch("RWF", [NT, 1024])
    W1B, bW1B = scratch("W1B", [NE, D, DFF], BF16)
    W3B, bW3B = scratch("W3B", [NE, D, DFF], BF16)
    W2B, bW2B = scratch("W2B", [NE, DFF, D], BF16)

    def prow(t):
        return (64 if t < NCT else 128) + t * P

    ident, b_id = kb.sb([P, P], F32, "ident")
    ones, b_on = kb.sb([P, P], F32, "ones")
    identb, b_idb = kb.sb([P, P], BF16, "identb")
    I4, b_I4 = kb.sb([P, 4, P], F32, "I4")
    MI4 = {}
    MS4 = {}
    NMS4 = {}
    b_c = Buf("consts")
    G(lambda: pool.memset(ones[:], 1.0), (), [b_on])
    G(lambda: pool.affine_select(out=ident[:], in_=ones[:], pattern=[[-1, P]], compare_op=ALU.is_equal,
                                 fill=0.0, base=0, channel_multiplier=1), [b_on], [b_id])
    V(lambda: vec.tensor_copy(out=identb[:], in_=ident[:]), [b_id], [b_idb])
    for h in range(4):
        V(lambda: vec.tensor_copy(out=I4[:, h, :], in_=ident[:]), [b_id], [b_I4])
    for d in range(2):
        MI4[d], _ = kb.sb([P, 4, P], F32, "MI4")
        MS4[d], _ = kb.sb([P, 4, P], F32, "MS4")
        NMS4[d], _ = kb.sb([P, 4, P], F32, "NMS4")
        sg = 1 if d == 0 else -1
        for h in range(4):
            G(lambda: pool.affine_select(out=MI4[d][:, h, :], in_=ones[:], pattern=[[sg, P]], compare_op=ALU.is_ge,
                                         fill=0.0, base=0, channel_multiplier=-sg), [b_on], [b_c])
            G(lambda: pool.affine_select(out=MS4[d][:, h, :], in_=ones[:], pattern=[[sg, P]], compare_op=ALU.is_gt,
                                         fill=0.0, base=0, channel_multiplier=-sg), [b_on], [b_c])
        G(lambda: pool.tensor_scalar(out=NMS4[d][:], in0=MS4[d][:], scalar1=-1.0, scalar2=None, op0=ALU.mult), [b_c], [b_c])
    mLR, b_mLR = kb.sb([P, 2], F32, "mLR")
    G(lambda: pool.memset(mLR[:], 1.0), (), [b_mLR])
    for base_p in (0, 64):
        pass
    for q in (0, 64):
        G(lambda: pool.affine_select(out=mLR[:, 0:1], in_=mLR[:, 0:1], pattern=[[0, 1]], compare_op=ALU.not_equal,
                                     fill=0.0, base=-q, channel_multiplier=1), [b_mLR], [b_mLR])
    for q in (63, 127):
        G(lambda: pool.affine_select(out=mLR[:, 1:2], in_=mLR[:, 1:2], pattern=[[0, 1]], compare_op=ALU.not_equal,
                                     fill=0.0, base=-q, channel_multiplier=1), [b_mLR], [b_mLR])

    kb.dma("sp", XR[0:TC, :], I["ctx"], (), [bXR])
    kb.dma("sp", XR[TC:NT, :], I["x"], (), [bXR])
    m0 = kb.mark()
    zt, b_zt = kb.sb([P, 1920], F32, "zeros")
    G(lambda: pool.memset(zt[:], 0.0), (), [b_zt])
    for r0 in (0, 64 + TC, 128 + NT):
        kb.dma("sp", PTM[r0:r0 + 64, 528:2448], zt[0:64, :], [b_zt], [bPTM])
    kb.release(m0)

    SBC = []
    with nc.allow_non_contiguous_dma(reason="tiny vector loads"):
        for s, nm in enumerate(("c_ctx", "c")):
            st_, b_st = kb.sb([P, 8, 1], F32, "sT")
            kb.dma("sp", st_[:, :, 0], I[nm].rearrange("o (k p) -> p (o k)", p=P), (), [b_st])
            A(lambda: act.activation(out=st_[:], in_=st_[:], func=AF.Silu), [b_st], [b_st])
            sbc, b_sbc = kb.sb([P, 8, P], F32, "SBC")
            V(lambda: vec.tensor_copy(out=sbc[:], in_=st_[:].to_broadcast([P, 8, P])), [b_st], [b_sbc])
            SBC.append((sbc, b_sbc))
    MODD, bMODD = scratch("MODD", [2, P, NMOD * D])
    MOD = {}

    def load_mods(items):
        for (s, idx) in items:
            t_, b_ = kb.sb([P, D], F32, "mod")
            kb.dma("act", t_[:], MODD[s][:, idx * D:(idx + 1) * D], [bMODD], [b_])
            MOD[(s, idx)] = (t_, b_)

    def stage_mod(l):
        m = kb.mark()
        MODB = [kb.sb([P, NMOD * D], F32, "MODB") for _ in range(2)]
        wn = [kb.sb([P, 8, 512], F32, "adaw") for _ in range(2)]
        brow = [kb.sb([1, 512], F32, "adab") for _ in range(2)]
        for n in range(12):
            w_, bw = wn[n % 2]
            br_, bbr = brow[n % 2]
            kb.dma("sp", w_[:], I["ada_w"][l][:, n * 512:(n + 1) * 512].rearrange("(k p) n -> p k n", p=P), (), [bw])
            kb.dma("sp", br_[:], I["ada_b"][l:l + 1, n * 512:(n + 1) * 512], (), [bbr])
            for s in range(2):
                ps, bps = kb.bank()
                for k in range(8):
                    mm(ps[:], SBC[s][0][:, k, :], w_[:, k, :], k == 0, False, [SBC[s][1], bw], [bps])
                mm(ps[:], ones[0:1, :], br_[0:1, :], False, True, [b_on, bbr], [bps])
                (A if s else V)(lambda: (act.copy if s else vec.tensor_copy)(out=MODB[s][0][:, n * 512:(n + 1) * 512], in_=ps[:]),
                                [bps], [MODB[s][1]])
        gb, b_gb = kb.sb([P, D], F32, "gB")
        for nm, c0 in (("norm1_g", 1 * D), ("norm2_g", 4 * D)):
            kb.dma("sp", gb[:], I[nm][l:l + 1, :].partition_broadcast(P), (), [b_gb])
            for s in range(2):
                V(lambda: vec.scalar_tensor_tensor(out=MODB[s][0][:, c0:c0 + D], in0=MODB[s][0][:, c0:c0 + D], scalar=1.0,
                                                   in1=gb[:], op0=ALU.add, op1=ALU.mult), [b_gb, MODB[s][1]], [MODB[s][1]])
        for s in range(2):
            kb.dma("sp", MODD[s], MODB[s][0][:], [MODB[s][1]], [bMODD])
        kb.release(m)

    def norm_mod(xt, bx, s, c_shift, c_scale, hb, bhb, sc, bsc):
        msc, bmsc = MOD[(s, c_scale // D)]
        msh, bmsh = MOD[(s, c_shift // D)]
        junk, bj = sc_j
        A(lambda: act.activation(out=junk[:], in_=xt, func=AF.Square, accum_out=sc[:, 0:1]), [bx], [bj, bsc])
        V(lambda: vec.tensor_scalar(out=sc[:, 1:2], in0=sc[:, 0:1], scalar1=1.0 / D, scalar2=EPS, op0=ALU.mult, op1=ALU.add), [bsc], [bsc])
        A(lambda: act.activation(out=sc[:, 2:3], in_=sc[:, 1:2], func=AF.Sqrt), [bsc], [bsc])
        V(lambda: vec.reciprocal(out=sc[:, 3:4], in_=sc[:, 2:3]), [bsc], [bsc])
        V(lambda: vec.scalar_tensor_tensor(out=junk[:], in0=xt, scalar=sc[:, 3:4], in1=msc[:],
                                           op0=ALU.mult, op1=ALU.mult), [bx, bsc, bmsc], [bj])
        V(lambda: vec.tensor_tensor(out=hb, in0=junk[:], in1=msh[:], op=ALU.add), [bj, bmsh], [bhb])

    sc_j = kb.sb([P, D], F32, "junk")
    HTbox = [None, None]

    def to_HT(hb, bhb, t):
        HT, b_HT = HTbox
        ps, bps = kb.bank()
        psb = ps[:].bitcast(BF16)
        for k in range(8):
            T(lambda: pe.transpose(out=psb[:, k * P:(k + 1) * P], in_=hb[:, k * P:(k + 1) * P], identity=identb[:]), [bhb, b_idb], [bps])
        A(lambda: act.copy(out=HT[:, :, t * P:(t + 1) * P], in_=psb.rearrange("p (k t) -> p k t", k=8)), [bps], [b_HT])

    def stage_proj(l):
        m = kb.mark()
        HTbox[0], HTbox[1] = kb.sb([P, 8, NT], BF16, "HT")
        HT, b_HT = HTbox
        load_mods([(0, 0), (0, 1), (1, 0), (1, 1)])
        xt2 = [kb.sb([P, D], F32, "xt") for _ in range(2)]
        hb2 = [kb.sb([P, D], BF16, "hb") for _ in range(2)]
        sc2 = [kb.sb([P, 4], F32, "sc") for _ in range(2)]
        for t in range(NTT):
            xt, bx = xt2[t % 2]
            hb, bhb = hb2[t % 2]
            sc, bsc = sc2[t % 2]
            kb.dma("sp", xt[:], XR[t * P:(t + 1) * P, :], [bXR], [bx])
            norm_mod(xt[:], bx, 0 if t < NCT else 1, 0, D, hb[:], bhb, sc, bsc)
            to_HT(hb, bhb, t)
        wf2 = [kb.sb([P, 8, 512], F32, "wf") for _ in range(2)]
        wb2 = [kb.sb([P, 8, 512], BF16, "wb") for _ in range(2)]
        ob2 = [kb.sb([P, 512], F32, "ob") for _ in range(4)]
        nblk = (IN_COLS + 511) // 512
        oi = 0
        for bi in range(nblk):
            c0 = bi * 512
            cw = min(512, IN_COLS - c0)
            wf, bwf = wf2[bi % 2]
            wb, bwb = wb2[bi % 2]
            kb.dma("sp", wf[:, :, 0:cw], I["w_in"][l][:, c0:c0 + cw].rearrange("(k p) n -> p k n", p=P), (), [bwf])
            (G if bi % 2 else V)(lambda: (pool if bi % 2 else vec).tensor_copy(out=wb[:, :, 0:cw], in_=wf[:, :, 0:cw]), [bwf], [bwb])
            if c0 < 1536:
                for cc in range(4):
                    for t0 in range(0, NT, 512):
                        tw = min(512, NT - t0)
                        ps, bps = kb.bank()
                        for k in range(8):
                            mm(ps[:, 0:tw], wb[:, k, cc * P:(cc + 1) * P], HT[:, k, t0:t0 + tw], k == 0, k == 7, [bwb, b_HT], [bps])
                        ob, bob = ob2[oi % 4]
                        oi += 1
                        (A if oi % 2 else V)(lambda: (act.copy if oi % 2 else vec.tensor_copy)(out=ob[:, 0:tw], in_=ps[:, 0:tw]), [bps], [bob])
                        kb.dma("pool", PQT[c0 + cc * P:c0 + (cc + 1) * P, t0:t0 + tw], ob[:, 0:tw], [bob], [bPQT])
            else:
                for t in range(NTT):
                    ps, bps = kb.bank()
                    for k in range(8):
                        mm(ps[:, 0:cw], HT[:, k, t * P:(t + 1) * P], wb[:, k, 0:cw], k == 0, k == 7, [bwb, b_HT], [bps])
                    ob, bob = ob2[oi % 4]
                    oi += 1
                    (A if oi % 2 else V)(lambda: (act.copy if oi % 2 else vec.tensor_copy)(out=ob[:, 0:cw], in_=ps[:, 0:cw]), [bps], [bob])
                    kb.dma("pool", PTM[prow(t):prow(t) + P, c0 - 1536:c0 - 1536 + cw], ob[:, 0:cw], [bob], [bPTM])
        kb.release(m)

    def acopy(out, in_):
        return act.activation(out=out, in_=in_, func=AF.Copy)

    def stage_conv(l):
        m = kb.mark()
        ROWS = TL // 64
        X2 = [kb.sb([P, NT], F32, "cx") for _ in range(2)]
        A2 = [kb.sb([P, NT], F32, "ca") for _ in range(2)]
        cw, b_cw = kb.sb([P, 12, 9], F32, "cw")
        with nc.allow_non_contiguous_dma(reason="conv w"):
            for ct in range(12):
                kb.dma("sp", cw[:, ct, :], I["gdn_conv"][l].rearrange("a b (t c) -> t c (a b)", c=P)[ct], (), [b_cw])
        ob2 = [kb.sb([P, 4, P], F32, "cob") for _ in range(2)]
        oi = 0
        for ct in range(12):
            X, bX = X2[ct % 2]
            AC, bA = A2[ct % 2]
            kb.dma("sp", X[:], PQT[ct * P:(ct + 1) * P, :], [bPQT], [bX])
            V(lambda: vec.tensor_scalar(out=AC[:], in0=X[:], scalar1=cw[:, ct, 4:5], scalar2=None, op0=ALU.mult), [bX, b_cw], [bA])
            for dx in (-1, 1):
                tap = 3 + dx + 1
                o0, o1 = max(0, -dx), TC - max(0, dx)
                V(lambda: vec.scalar_tensor_tensor(out=AC[:, o0:o1], in0=X[:, o0 + dx:o1 + dx], scalar=cw[:, ct, tap:tap + 1],
                                                   in1=AC[:, o0:o1], op0=ALU.mult, op1=ALU.add), [bX, b_cw, bA], [bA])
            Xl = X[:, TC:NT].rearrange("p (r c) -> p r c", c=64)
            Al = AC[:, TC:NT].rearrange("p (r c) -> p r c", c=64)
            for dy in (-1, 0, 1):
                for dx in (-1, 0, 1):
                    if dy == 0 and dx == 0:
                        continue
                    tap = (dy + 1) * 3 + dx + 1
                    r0, r1 = max(0, -dy), ROWS - max(0, dy)
                    c0, c1 = max(0, -dx), 64 - max(0, dx)
                    V(lambda: vec.scalar_tensor_tensor(out=Al[:, r0:r1, c0:c1], in0=Xl[:, r0 + dy:r1 + dy, c0 + dx:c1 + dx],
                                                       scalar=cw[:, ct, tap:tap + 1], in1=Al[:, r0:r1, c0:c1],
                                                       op0=ALU.mult, op1=ALU.add), [bX, b_cw, bA], [bA])
            A(lambda: act.activation(out=AC[:], in_=AC[:], func=AF.Silu), [bA], [bA])
            for t0 in range(0, NTT, 4):
                n_ = min(4, NTT - t0)
                ps, bps = kb.bank()
                for j in range(n_):
                    T(lambda: pe.transpose(out=ps[:, j * P:(j + 1) * P], in_=AC[:, (t0 + j) * P:(t0 + j + 1) * P], identity=ident[:]),
                      [bA, b_id], [bps])
                ob, bob = ob2[oi % 2]
                oi += 1
                A(lambda: acopy(ob[:, 0:n_, :], ps[:, 0:n_ * P].rearrange("p (j c) -> p j c", c=P)), [bps], [bob])
                kb.dma("pool", QKV[t0 * P:(t0 + n_) * P, ct * P:(ct + 1) * P].rearrange("(j p) c -> p j c", p=P), ob[:, 0:n_, :], [bob], [bQKV])
        kb.release(m)

    def scan_order(d):
        if d == 0:
            return list(range(NTT))
        return list(range(NCT))[::-1] + list(range(NCT, NTT))[::-1]

    def bc(ap, n):
        return ap.to_broadcast([P, ap.shape[1], n])

    def neumann(Pm, bP, Qm, bQ, Y, bY, nb, levels=6):
        cur = 0
        for lv in range(levels):
            nxt = 1 - cur
            psP, bpsP = kb.bank()
            for h in range(nb):
                mm(psP[:, h * P:(h + 1) * P], Qm[cur][:, h, :], Pm[cur][:, h, :], True, True, [bP[cur], bQ[cur]], [bpsP])
            A(lambda: acopy(Pm[nxt][:, 0:nb, :], psP[:, 0:nb * P].rearrange("p (h c) -> p h c", c=P)), [bpsP], [bP[nxt]])
            if lv < levels - 1:
                psQ, bpsQ = kb.bank()
                for h in range(nb):
                    mm(psQ[:, h * P:(h + 1) * P], Pm[cur][:, h, :], Qm[cur][:, h, :], True, True, [bP[cur], bQ[cur]], [bpsQ])
                V(lambda: vec.tensor_copy(out=Qm[nxt][:, 0:nb, :], in_=psQ[:, 0:nb * P].rearrange("p (h c) -> p h c", c=P)), [bpsQ], [bQ[nxt]])
            psY, bpsY = kb.bank()
            for h in range(nb):
                mm(psY[:, h * P:(h + 1) * P], Pm[nxt][:, h, :], Y[:, h, :], True, True, [bP[nxt], bY], [bpsY])
            V(lambda: vec.tensor_tensor(out=Y[:, 0:nb, :], in0=f32(Y[:, 0:nb, :]), in1=psY[:, 0:nb * P].rearrange("p (h c) -> p h c", c=P), op=ALU.add),
              [bpsY, bY], [bY])
            cur = nxt
            yield

    def decay_T(gB, b_gB, Gcol, bG, d, dect, b_dect, nh=4):
        ps, bps = kb.bank()
        for h in range(nh):
            mm(ps[:, h * P:(h + 1) * P], gB[:, h, :], MI4[d][:, 0, :], True, True, [b_gB, b_c], [bps])
        V(lambda: vec.tensor_tensor(out=dect[:, 0:nh, :], in0=ps[:, 0:nh * P].rearrange("p (h c) -> p h c", c=P), in1=bc(Gcol, P), op=ALU.subtract),
          [bps, bG], [b_dect])
        V(lambda: vec.tensor_scalar(out=dect[:, 0:nh, :], in0=dect[:, 0:nh, :], scalar1=0.0, scalar2=None, op0=ALU.min), [b_dect], [b_dect])
        A(lambda: act.activation(out=dect[:, 0:nh, :], in_=dect[:, 0:nh, :], func=AF.Exp), [b_dect], [b_dect])
        V(lambda: vec.tensor_tensor(out=dect[:, 0:nh, :], in0=dect[:, 0:nh, :], in1=MI4[d][:, 0:nh, :], op=ALU.mult), [b_dect, b_c], [b_dect])

    def gdn_chain(l, dsel):
        S, bS = kb.sb([P, 4, P], F32R, "gS")
        aexp, b_ae = kb.sb([P, 8], F32, "aexp")
        dtb, b_dt = kb.sb([P, 8], F32, "dtb")
        kb.dma("sp", aexp[:], I["gdn_a_log"][l:l + 1].rearrange("o d h -> o (d h)").partition_broadcast(P), (), [b_ae])
        kb.dma("sp", dtb[:], I["gdn_dt_bias"][l:l + 1].rearrange("o d h -> o (d h)").partition_broadcast(P), (), [b_dt])
        A(lambda: act.activation(out=aexp[:], in_=aexp[:], func=AF.Exp), [b_ae], [b_ae])
        V(lambda: vec.tensor_scalar(out=aexp[:], in0=aexp[:], scalar1=-1.0, scalar2=None, op0=ALU.mult), [b_ae], [b_ae])
        qkv, bq = kb.sb([P, 1536], F32, "qkv")
        gt, bgt = kb.sb([P, 16], F32, "gt")
        tmp, btmp = kb.sb([P, 8, P], F32, "tmp")
        ss, bss = kb.sb([P, 8, 1], F32, "ss")
        qkh, bqkh = kb.sb([P, 8, P], F32, "qkh")
        CF, bCF = kb.sb([P, 8, 4, 1], F32, "CF")
        gB, b_gB = kb.sb([P, 4, P], F32, "gB")
        dect, b_dect = kb.sb([P, 4, P], F32, "dect")
        dects, b_dects = kb.sb([P, 4, P], F32, "dects")
        KBt, bKBt = kb.sb([P, 4, P], F32, "kb_")
        R2, bR2 = kb.sb([P, 4, P], F32R, "R2")
        KG, bKG = kb.sb([P, 4, P], F32R, "KG")
        QG, bQG = kb.sb([P, 4, P], F32, "qg")
        R1, bR1 = kb.sb([P, 4, P], F32R, "R1")
        KT, bKT = kb.sb([P, 4, P], F32R, "KT")
        QT, bQT = kb.sb([P, 4, P], F32R, "QT")
        KBT, bKBT = kb.sb([P, 4, P], F32R, "KBT")
        QGT, bQGT = kb.sb([P, 4, P], F32R, "QGT")
        AT, bAT = kb.sb([P, 4, P], F32R, "AT")
        Pm = [kb.sb([P, 4, P], F32, "Pm") for _ in range(2)]
        Qm = [kb.sb([P, 4, P], F32, "Qm") for _ in range(2)]
        Y, bY = kb.sb([P, 4, P], F32, "Y")
        Yr, bYr = kb.sb([P, 4, P], F32R, "Yr")
        NWT, bNWT = kb.sb([P, 4, P], F32R, "NWT")
        VN, bVN = kb.sb([P, 4, P], F32R, "VN")
        O, bO = kb.sb([P, 4, P], F32, "O")
        for d in (dsel,):
            V(lambda: vec.tensor_scalar(out=S[:], in0=I4[:], scalar1=0.0, scalar2=None, op0=ALU.mult), [b_I4], [bS])
            for t in scan_order(d):
                kb.dma("sp", qkv[:], QKV[t * P:(t + 1) * P, :], [bQKV], [bq])
                kb.dma("sp", gt[:], PTM[prow(t):prow(t) + P, 512:528], [bPTM], [bgt])
                A(lambda: act.activation(out=CF[:, 0, :, 0], in_=gt[:, d * 4:(d + 1) * 4], func=AF.Sigmoid), [bgt], [bCF])
                V(lambda: vec.tensor_tensor(out=CF[:, 6, :, 0], in0=gt[:, 8 + d * 4:12 + d * 4], in1=dtb[:, d * 4:(d + 1) * 4], op=ALU.add), [bgt, b_dt], [bCF])
                A(lambda: act.activation(out=CF[:, 6, :, 0], in_=CF[:, 6, :, 0], func=AF.Exp), [bCF], [bCF])
                V(lambda: vec.tensor_scalar(out=CF[:, 6, :, 0], in0=CF[:, 6, :, 0], scalar1=1.0, scalar2=None, op0=ALU.add), [bCF], [bCF])
                A(lambda: act.activation(out=CF[:, 6, :, 0], in_=CF[:, 6, :, 0], func=AF.Ln), [bCF], [bCF])
                V(lambda: vec.tensor_tensor(out=CF[:, 6, :, 0], in0=CF[:, 6, :, 0], in1=aexp[:, d * 4:(d + 1) * 4], op=ALU.mult), [bCF, b_ae], [bCF])
                yield
                qk3 = qkv[:, 0:1024].rearrange("p (h c) -> p h c", c=P)
                V(lambda: vec.tensor_tensor(out=tmp[:], in0=qk3, in1=qk3, op=ALU.mult), [bq], [btmp])
                V(lambda: vec.tensor_reduce(out=ss[:, :, 0], in_=tmp[:], axis=AX.X, op=ALU.add), [btmp], [bss])
                V(lambda: vec.tensor_scalar(out=ss[:], in0=ss[:], scalar1=EPS, scalar2=None, op0=ALU.add), [bss], [bss])
                A(lambda: act.activation(out=ss[:], in_=ss[:], func=AF.Sqrt), [bss], [bss])
                V(lambda: vec.reciprocal(out=ss[:], in_=ss[:]), [bss], [bss])
                V(lambda: vec.tensor_scalar(out=ss[:, 0:4, :], in0=ss[:, 0:4, :], scalar1=GDK ** -0.5, scalar2=None, op0=ALU.mult), [bss], [bss])
                V(lambda: vec.tensor_tensor(out=qkh[:], in0=qk3, in1=bc(ss[:], P), op=ALU.mult), [bq, bss], [bqkh])
                yield
                psg, bpsg = kb.bank()
                mm(psg[:, 0:4], MI4[d][:, 0, :], CF[:, 6, :, 0], True, True, [b_c, bCF], [bpsg])
                mm(psg[:, 4:8], ones[:], CF[:, 6, :, 0], True, True, [b_on, bCF], [bpsg])
                V(lambda: vec.tensor_copy(out=CF[:, 5, :, 0], in_=psg[:, 0:4]), [bpsg], [bCF])
                V(lambda: vec.tensor_copy(out=CF[:, 7, :, 0], in_=psg[:, 4:8]), [bpsg], [bCF])
                A(lambda: act.activation(out=CF[:, 1, :, 0], in_=CF[:, 5, :, 0], func=AF.Exp), [bCF], [bCF])
                A(lambda: act.activation(out=CF[:, 2, :, 0], in_=CF[:, 7, :, 0], func=AF.Exp), [bCF], [bCF])
                V(lambda: vec.tensor_tensor(out=CF[:, 3, :, 0], in0=CF[:, 7, :, 0], in1=CF[:, 5, :, 0], op=ALU.subtract), [bCF], [bCF])
                A(lambda: act.activation(out=CF[:, 3, :, 0], in_=CF[:, 3, :, 0], func=AF.Exp), [bCF], [bCF])
                V(lambda: vec.tensor_tensor(out=CF[:, 4, :, 0], in0=CF[:, 0, :, 0], in1=CF[:, 1, :, 0], op=ALU.mult), [bCF], [bCF])
                yield
                kh = qkh[:, 4:8, :]
                qh = qkh[:, 0:4, :]
                v3 = qkv[:, 1024:1536].rearrange("p (h c) -> p h c", c=P)
                V(lambda: vec.tensor_tensor(out=KBt[:], in0=kh, in1=bc(CF[:, 0], P), op=ALU.mult), [bqkh, bCF], [bKBt])
                G(lambda: pool.tensor_tensor(out=R2[:], in0=kh, in1=bc(CF[:, 4], P), op=ALU.mult), [bqkh, bCF], [bR2])
                V(lambda: vec.tensor_tensor(out=KG[:], in0=kh, in1=bc(CF[:, 3], P), op=ALU.mult), [bqkh, bCF], [bKG])
                G(lambda: pool.tensor_tensor(out=QG[:], in0=qh, in1=bc(CF[:, 1], P), op=ALU.mult), [bqkh, bCF], [bQG])
                V(lambda: vec.tensor_tensor(out=R1[:], in0=v3, in1=bc(CF[:, 0], P), op=ALU.mult), [bq, bCF], [bR1])
                G(lambda: pool.tensor_copy(out=gB[:], in_=bc(CF[:, 6], P)), [bCF], [b_gB])
                yield
                for src, bsrc, dst, bdst, eng in ((kh, bqkh, KT, bKT, "a"), (qh, bqkh, QT, bQT, "v"), (KBt[:], bKBt, KBT, bKBT, "a"), (QG[:], bQG, QGT, bQGT, "v")):
                    ps, bps = kb.bank()
                    for h in range(4):
                        T(lambda: pe.transpose(out=ps[:, h * P:(h + 1) * P], in_=src[:, h, :], identity=ident[:]), [bsrc, b_id], [bps])
                    if eng == "a":
                        A(lambda: acopy(dst[:], ps[:].rearrange("p (h c) -> p h c", c=P)), [bps], [bdst])
                    else:
                        V(lambda: vec.tensor_copy(out=dst[:], in_=ps[:].rearrange("p (h c) -> p h c", c=P)), [bps], [bdst])
                yield
                decay_T(gB, b_gB, CF[:, 5], bCF, d, dect, b_dect)
                G(lambda: pool.tensor_tensor(out=dects[:], in0=dect[:], in1=I4[:], op=ALU.subtract), [b_dect, b_I4], [b_dects])
                yield
                ps, bps = kb.bank()
                for h in range(4):
                    mm(ps[:, h * P:(h + 1) * P], KT[:, h, :], KBT[:, h, :], True, True, [bKT, bKBT], [bps])
                V(lambda: vec.tensor_tensor(out=Qm[0][0][:], in0=ps[:].rearrange("p (h c) -> p h c", c=P), in1=dects[:], op=ALU.mult), [bps, b_dects], [Qm[0][1]])
                ps, bps = kb.bank()
                for h in range(4):
                    mm(ps[:, h * P:(h + 1) * P], KT[:, h, :], QT[:, h, :], True, True, [bKT, bQT], [bps])
                V(lambda: vec.tensor_tensor(out=AT[:], in0=ps[:].rearrange("p (h c) -> p h c", c=P), in1=dect[:], op=ALU.mult), [bps, b_dect], [bAT])
                ps, bps = kb.bank()
                for h in range(4):
                    T(lambda: pe.transpose(out=ps[:, h * P:(h + 1) * P], in_=f32(Qm[0][0][:, h, :]), identity=ident[:]), [Qm[0][1], b_id], [bps])
                A(lambda: acopy(Pm[0][0][:], ps[:].rearrange("p (h c) -> p h c", c=P)), [bps], [Pm[0][1]])
                V(lambda: vec.tensor_tensor(out=Y[:], in0=I4[:], in1=f32(Qm[0][0][:]), op=ALU.subtract), [b_I4, Qm[0][1]], [bY])
                yield from neumann([Pm[0][0], Pm[1][0]], [Pm[0][1], Pm[1][1]], [Qm[0][0], Qm[1][0]], [Qm[0][1], Qm[1][1]], Y, bY, 4)
                G(lambda: pool.tensor_copy(out=Yr[:], in_=Y[:]), [bY], [bYr])
                ps, bps = kb.bank()
                for h in range(4):
                    mm(ps[:, h * P:(h + 1) * P], R2[:, h, :], Yr[:, h, :], True, True, [bR2, bYr], [bps])
                V(lambda: vec.tensor_scalar(out=NWT[:], in0=ps[:].rearrange("p (h c) -> p h c", c=P), scalar1=-1.0, scalar2=None, op0=ALU.mult), [bps], [bNWT])
                yield
                ps, bps = kb.bank()
                for h in range(4):
                    mm(ps[:, h * P:(h + 1) * P], Yr[:, h, :], R1[:, h, :], True, False, [bYr, bR1], [bps])
                    mm(ps[:, h * P:(h + 1) * P], NWT[:, h, :], S[:, h, :], False, True, [bNWT, bS], [bps])
                A(lambda: acopy(VN[:], ps[:].rearrange("p (h c) -> p h c", c=P)), [bps], [bVN])
                ps, bps = kb.bank()
                for h in range(4):
                    mm(ps[:, h * P:(h + 1) * P], QGT[:, h, :], S[:, h, :], True, False, [bQGT, bS], [bps])
                    mm(ps[:, h * P:(h + 1) * P], AT[:, h, :], VN[:, h, :], False, True, [bAT, bVN], [bps])
                V(lambda: vec.tensor_copy(out=O[:], in_=ps[:].rearrange("p (h c) -> p h c", c=P)), [bps], [bO])
                yield
                ps, bps = kb.bank()
                for h in range(4):
                    mm(ps[:, h * P:(h + 1) * P], KG[:, h, :], VN[:, h, :], True, True, [bKG, bVN], [bps])
                V(lambda: vec.tensor_tensor(out=S[:], in0=f32(S[:]), in1=bc(CF[:, 2], P), op=ALU.mult), [bS, bCF], [bS])
                V(lambda: vec.tensor_tensor(out=S[:], in0=f32(S[:]), in1=ps[:].rearrange("p (h c) -> p h c", c=P), op=ALU.add), [bS, bps], [bS])
                kb.dma("pool", OGG[d, t * P:(t + 1) * P, :].rearrange("p (h c) -> p h c", c=P), O[:], [bO], [bOGG])
                yield

    def gdn_finish(l, skip_ctx_out):
        m = kb.mark()
        ng, b_ng = kb.sb([P, 1, P], F32, "ng")
        kb.dma("sp", ng[:, 0, :], I["gdn_norm_g"][l:l + 1, :].partition_broadcast(P), (), [b_ng])
        bufs = [[kb.sb([P, 4, P], F32, "gf") for _ in range(4)] for _ in range(2)]
        ss2 = [kb.sb([P, 4, 1], F32, "gfs") for _ in range(2)]
        for t in range(NTT):
            if skip_ctx_out and t < NCT:
                continue
            (O, bO), (og, bog), (zt_, bz), (tmp, btmp) = bufs[t % 2]
            ss, bss = ss2[t % 2]
            kb.dma("sp", O[:], OGG[0, t * P:(t + 1) * P, :].rearrange("p (h c) -> p h c", c=P), [bOGG], [bO])
            kb.dma("act", og[:], OGG[1, t * P:(t + 1) * P, :].rearrange("p (h c) -> p h c", c=P), [bOGG], [bog])
            kb.dma("sp", zt_[:], PTM[prow(t):prow(t) + P, 0:512].rearrange("p (h c) -> p h c", c=P), [bPTM], [bz])
            V(lambda: vec.tensor_tensor(out=O[:], in0=O[:], in1=og[:], op=ALU.add), [bO, bog], [bO])
            G(lambda: pool.tensor_tensor(out=tmp[:], in0=O[:], in1=O[:], op=ALU.mult), [bO], [btmp])
            V(lambda: vec.tensor_reduce(out=ss[:, :, 0], in_=tmp[:], axis=AX.X, op=ALU.add), [btmp], [bss])
            V(lambda: vec.tensor_scalar(out=ss[:], in0=ss[:], scalar1=1.0 / GDK, scalar2=EPS, op0=ALU.mult, op1=ALU.add), [bss], [bss])
            A(lambda: act.activation(out=ss[:], in_=ss[:], func=AF.Sqrt), [bss], [bss])
            V(lambda: vec.reciprocal(out=ss[:], in_=ss[:]), [bss], [bss])
            V(lambda: vec.tensor_tensor(out=O[:], in0=O[:], in1=bc(ss[:], P), op=ALU.mult), [bO, bss], [bO])
            G(lambda: pool.tensor_tensor(out=O[:], in0=O[:], in1=ng[:].to_broadcast([P, 4, P]), op=ALU.mult), [bO, b_ng], [bO])
            A(lambda: act.activation(out=zt_[:], in_=zt_[:], func=AF.Silu), [bz], [bz])
            V(lambda: vec.tensor_tensor(out=O[:], in0=O[:], in1=zt_[:], op=ALU.mult), [bO, bz], [bO])
            kb.dma("pool", BR[t * P:(t + 1) * P, 0:512].rearrange("p (h c) -> p h c", c=P), O[:], [bO], [bBR])
        kb.release(m)

    def mlstm_chain(l, dsel):
        C, bC = kb.sb([P, 2, 130], F32R, "mC")
        ibt, b_ib = kb.sb([P, 8], F32, "ib")
        fbt, b_fb = kb.sb([P, 8], F32, "fb")
        kb.dma("sp", ibt[:], I["mlstm_i_bias"][l:l + 1].rearrange("o d h -> o (d h)").partition_broadcast(P), (), [b_ib])
        kb.dma("sp", fbt[:], I["mlstm_f_bias"][l:l + 1].rearrange("o d h -> o (d h)").partition_broadcast(P), (), [b_fb])
        ml, bml = kb.sb([P, 1552], F32, "ml")
        CF, bCF = kb.sb([P, 8, 4, 1], F32, "mCF")
        gB, b_gB = kb.sb([P, 4, P], F32, "mgB")
        dect, b_dect = kb.sb([P, 4, P], F32, "mdect")
        kt, bkt = kb.sb([P, 4, 64], F32, "mkt")
        qs, bqs = kb.sb([P, 4, 64], F32, "mqs")
        qg, bqg = kb.sb([P, 4, 64], F32, "mqg")
        kend, bke = kb.sb([P, 4, 64], F32R, "mkend")
        vaug, bva = kb.sb([P, 4, 130], F32R, "vaug")
        ktz, bktz = kb.sb([P, 4, P], F32, "ktz")
        qgz, bqgz = kb.sb([P, 4, P], F32, "qgz")
        KTT, bKTT = kb.sb([P, 4, P], F32R, "KTT")
        QST, bQST = kb.sb([P, 2, P], F32R, "QST")
        QGT, bQGT = kb.sb([P, 4, P], F32R, "QGT")
        G(lambda: pool.memset(ktz[:], 0.0), (), [bktz])
        G(lambda: pool.memset(qgz[:], 0.0), (), [bqgz])
        AT, bAT = kb.sb([P, 4, P], F32R, "mAT")
        dn, bdn = kb.sb([P, 4, 1], F32, "dn")
        Hd, bH = kb.sb([P, 4, P], F32, "Hd")
        o41 = ones[:, 0:4].rearrange("p (a b) -> p a b", b=1)
        V(lambda: vec.tensor_copy(out=vaug[:, :, P:P + 1], in_=o41), [b_on], [bva])
        V(lambda: vec.tensor_scalar(out=vaug[:, :, P + 1:P + 2], in0=o41, scalar1=0.0, scalar2=None, op0=ALU.mult), [b_on], [bva])
        for d in (dsel,):
            V(lambda: vec.tensor_scalar(out=C[:].rearrange("p b c -> p (b c)"), in0=ones[:, 0:1].to_broadcast([P, 260]), scalar1=0.0, scalar2=None, op0=ALU.mult), [b_on], [bC])
            for t in scan_order(d):
                kb.dma("sp", ml[:], PTM[prow(t):prow(t) + P, 2448:4000], [bPTM], [bml])
                V(lambda: vec.tensor_tensor(out=CF[:, 0, :, 0], in0=ml[:, 1536 + d * 4:1540 + d * 4], in1=ibt[:, d * 4:(d + 1) * 4], op=ALU.add), [bml, b_ib], [bCF])
                A(lambda: act.activation(out=CF[:, 0, :, 0], in_=CF[:, 0, :, 0], func=AF.Exp), [bCF], [bCF])
                V(lambda: vec.tensor_tensor(out=CF[:, 1, :, 0], in0=ml[:, 1544 + d * 4:1548 + d * 4], in1=fbt[:, d * 4:(d + 1) * 4], op=ALU.add), [bml, b_fb], [bCF])
                A(lambda: act.activation(out=CF[:, 1, :, 0], in_=CF[:, 1, :, 0], func=AF.Exp, scale=-1.0), [bCF], [bCF])
                V(lambda: vec.tensor_scalar(out=CF[:, 1, :, 0], in0=CF[:, 1, :, 0], scalar1=1.0, scalar2=None, op0=ALU.add), [bCF], [bCF])
                A(lambda: act.activation(out=CF[:, 1, :, 0], in_=CF[:, 1, :, 0], func=AF.Ln), [bCF], [bCF])
                V(lambda: vec.tensor_scalar(out=CF[:, 1, :, 0], in0=CF[:, 1, :, 0], scalar1=-1.0, scalar2=None, op0=ALU.mult), [bCF], [bCF])
                psg, bpsg = kb.bank()
                mm(psg[:, 0:4], MI4[d][:, 0, :], CF[:, 1, :, 0], True, True, [b_c, bCF], [bpsg])
                mm(psg[:, 4:8], ones[:], CF[:, 1, :, 0], True, True, [b_on, bCF], [bpsg])
                V(lambda: vec.tensor_copy(out=CF[:, 2, :, 0], in_=psg[:, 0:4]), [bpsg], [bCF])
                V(lambda: vec.tensor_copy(out=CF[:, 3, :, 0], in_=psg[:, 4:8]), [bpsg], [bCF])
                A(lambda: act.activation(out=CF[:, 4, :, 0], in_=CF[:, 2, :, 0], func=AF.Exp), [bCF], [bCF])
                A(lambda: act.activation(out=CF[:, 5, :, 0], in_=CF[:, 3, :, 0], func=AF.Exp), [bCF], [bCF])
                V(lambda: vec.tensor_tensor(out=CF[:, 6, :, 0], in0=CF[:, 3, :, 0], in1=CF[:, 2, :, 0], op=ALU.subtract), [bCF], [bCF])
                A(lambda: act.activation(out=CF[:, 6, :, 0], in_=CF[:, 6, :, 0], func=AF.Exp), [bCF], [bCF])
                yield
                q3 = ml[:, 0:256].rearrange("p (h c) -> p h c", c=64)
                k3 = ml[:, 256:512].rearrange("p (h c) -> p h c", c=64)
                v3 = ml[:, 512:1024].rearrange("p (h c) -> p h c", c=P)
                V(lambda: vec.tensor_tensor(out=kt[:], in0=k3, in1=bc(CF[:, 0], 64), op=ALU.mult), [bml, bCF], [bkt])
                G(lambda: pool.tensor_scalar(out=qs[:], in0=q3, scalar1=MDQK ** -0.5, scalar2=None, op0=ALU.mult), [bml], [bqs])
                V(lambda: vec.tensor_tensor(out=qg[:], in0=qs[:], in1=bc(CF[:, 4], 64), op=ALU.mult), [bqs, bCF], [bqg])
                V(lambda: vec.tensor_tensor(out=kend[:], in0=kt[:], in1=bc(CF[:, 6], 64), op=ALU.mult), [bkt, bCF], [bke])
                G(lambda: pool.tensor_copy(out=vaug[:, :, 0:P], in_=v3), [bml], [bva])
                G(lambda: pool.tensor_copy(out=gB[:], in_=bc(CF[:, 1], P)), [bCF], [b_gB])
                yield
                for hh in range(2):
                    for (zt2, bz2, src2, bs2, eng2) in ((ktz, bktz, kt, bkt, V), (qgz, bqgz, qg, bqg, G)):
                        o_ = zt2[:].rearrange("p (b h2) (s c) -> p b h2 s c", h2=2, s=2)[:, :, hh, hh, :]
                        i_ = src2[:].rearrange("p (b h2) c -> p b h2 c", h2=2)[:, :, hh, :]
                        eng2(lambda: (vec if eng2 is V else pool).tensor_copy(out=o_, in_=i_), [bs2], [bz2])
                ps, bps = kb.bank()
                for h in range(4):
                    T(lambda: pe.transpose(out=ps[:, h * P:(h + 1) * P], in_=ktz[:, h, :], identity=ident[:]), [bktz, b_id], [bps])
                A(lambda: acopy(KTT[:], ps[:].rearrange("p (b c) -> p b c", c=P)), [bps], [bKTT])
                ps, bps = kb.bank()
                for h in range(4):
                    T(lambda: pe.transpose(out=ps[:, h * P:(h + 1) * P], in_=qgz[:, h, :], identity=ident[:]), [bqgz, b_id], [bps])
                V(lambda: vec.tensor_copy(out=QGT[:], in_=ps[:].rearrange("p (b c) -> p b c", c=P)), [bps], [bQGT])
                ps, bps = kb.bank()
                for b in range(2):
                    T(lambda: pe.transpose(out=ps[:, b * P:(b + 1) * P], in_=qs[:, 2 * b:2 * b + 2, :].rearrange("p h c -> p (h c)"), identity=ident[:]), [bqs, b_id], [bps])
                A(lambda: acopy(QST[:], ps[:, 0:256].rearrange("p (b c) -> p b c", c=P)), [bps], [bQST])
                yield
                decay_T(gB, b_gB, CF[:, 2], bCF, d, dect, b_dect)
                yield
                ps, bps = kb.bank()
                for h in range(4):
                    r0 = (h % 2) * 64
                    mm(ps[:, h * P:(h + 1) * P], KTT[:, h, :], QST[:, h // 2, :], True, True, [bKTT, bQST], [bps])
                V(lambda: vec.tensor_tensor(out=AT[:], in0=ps[:].rearrange("p (h c) -> p h c", c=P), in1=dect[:], op=ALU.mult), [bps, b_dect], [bAT])
                yield
                psn, bpsn = kb.bank()
                psd, bpsd = kb.bank()
                for h in range(4):
                    r0 = (h % 2) * 64
                    mm(psn[:, h * P:(h + 1) * P], QGT[:, h, :], C[:, h // 2, 0:P], True, False, [bQGT, bC], [bpsn])
                    mm(psn[:, h * P:(h + 1) * P], AT[:, h, :], vaug[:, h, 0:P], False, True, [bAT, bva], [bpsn])
                    mm(psd[:, 2 * h:2 * h + 2], QGT[:, h, :], C[:, h // 2, P:P + 2], True, False, [bQGT, bC], [bpsd])
                    mm(psd[:, 2 * h:2 * h + 2], AT[:, h, :], vaug[:, h, P:P + 2], False, True, [bAT, bva], [bpsd])
                A(lambda: act.activation(out=dn[:, :, 0], in_=psd[:, 0:8].rearrange("p (h two) -> p h two", two=2)[:, :, 0], func=AF.Abs), [bpsd], [bdn])
                V(lambda: vec.tensor_scalar(out=dn[:], in0=dn[:], scalar1=1.0, scalar2=None, op0=ALU.max), [bdn], [bdn])
                V(lambda: vec.reciprocal(out=dn[:], in_=dn[:]), [bdn], [bdn])
                V(lambda: vec.tensor_tensor(out=Hd[:], in0=psn[:].rearrange("p (h c) -> p h c", c=P), in1=bc(dn[:], P), op=ALU.mult), [bpsn, bdn], [bH])
                yield
                for b in range(2):
                    ps, bps = kb.bank()
                    mm(ps[:, 0:260], kend[:, 2 * b:2 * b + 2, :].rearrange("p h c -> p (h c)"), vaug[:, 2 * b:2 * b + 2, :].rearrange("p h c -> p (h c)"),
                       True, True, [bke, bva], [bps])
                    for hh in range(2):
                        h = 2 * b + hh
                        r0 = hh * 64
                        V(lambda: vec.scalar_tensor_tensor(out=C[r0:r0 + 64, b, :], in0=f32(C[r0:r0 + 64, b, :]), scalar=CF[r0:r0 + 64, 5, h, :],
                                                           in1=ps[r0:r0 + 64, hh * 130:(hh + 1) * 130], op0=ALU.mult, op1=ALU.add), [bC, bCF, bps], [bC])
                yield
                kb.dma("pool", OGM[d, t * P:(t + 1) * P, :].rearrange("p (h c) -> p h c", c=P), Hd[:], [bH], [bOGM])
                yield

    def mlstm_finish(l, skip_ctx_out):
        m = kb.mark()
        ng, b_ng = kb.sb([P, 1, P], F32, "mng")
        kb.dma("sp", ng[:, 0, :], I["mlstm_norm_g"][l:l + 1, :].partition_broadcast(P), (), [b_ng])
        bufs = [[kb.sb([P, 4, P], F32, "mf") for _ in range(4)] for _ in range(2)]
        dn2 = [kb.sb([P, 4, 1], F32, "mfs") for _ in range(2)]
        for t in range(NTT):
            if skip_ctx_out and t < NCT:
                continue
            (Hd, bH), (og, bog), (og_, bo_), (tmp, btmp) = bufs[t % 2]
            dn, bdn = dn2[t % 2]
            kb.dma("sp", Hd[:], OGM[0, t * P:(t + 1) * P, :].rearrange("p (h c) -> p h c", c=P), [bOGM], [bH])
            kb.dma("act", og[:], OGM[1, t * P:(t + 1) * P, :].rearrange("p (h c) -> p h c", c=P), [bOGM], [bog])
            kb.dma("sp", og_[:], PTM[prow(t):prow(t) + P, 2448 + 1024:2448 + 1536].rearrange("p (h c) -> p h c", c=P), [bPTM], [bo_])
            V(lambda: vec.tensor_tensor(out=Hd[:], in0=Hd[:], in1=og[:], op=ALU.add), [bH, bog], [bH])
            G(lambda: pool.tensor_tensor(out=tmp[:], in0=Hd[:], in1=Hd[:], op=ALU.mult), [bH], [btmp])
            V(lambda: vec.tensor_reduce(out=dn[:, :, 0], in_=tmp[:], axis=AX.X, op=ALU.add), [btmp], [bdn])
            V(lambda: vec.tensor_scalar(out=dn[:], in0=dn[:], scalar1=1.0 / MDV, scalar2=EPS, op0=ALU.mult, op1=ALU.add), [bdn], [bdn])
            A(lambda: act.activation(out=dn[:], in_=dn[:], func=AF.Sqrt), [bdn], [bdn])
            V(lambda: vec.reciprocal(out=dn[:], in_=dn[:]), [bdn], [bdn])
            V(lambda: vec.tensor_tensor(out=Hd[:], in0=Hd[:], in1=bc(dn[:], P), op=ALU.mult), [bH, bdn], [bH])
            G(lambda: pool.tensor_tensor(out=Hd[:], in0=Hd[:], in1=ng[:].to_broadcast([P, 4, P]), op=ALU.mult), [bH, b_ng], [bH])
            A(lambda: act.activation(out=og_[:], in_=og_[:], func=AF.Sigmoid), [bo_], [bo_])
            V(lambda: vec.tensor_tensor(out=Hd[:], in0=Hd[:], in1=og_[:], op=ALU.mult), [bH, bo_], [bH])
            kb.dma("pool", BR[t * P:(t + 1) * P, 1024:1536].rearrange("p (h c) -> p h c", c=P), Hd[:], [bH], [bBR])
        kb.release(m)

    def stage_rwkv_prep(l):
        m = kb.mark()
        def bload(name, shape, src):
            t_, b_ = kb.sb(shape, F32, name)
            kb.dma("sp", t_[:], src, (), [b_])
            return t_, b_
        muB, b_mu = bload("muB", [P, 1920], I["rwkv_mu"][l:l + 1, :].partition_broadcast(P))
        kkB, b_kk = bload("kkB", [P, 512], I["rwkv_k_k"][l:l + 1, :].partition_broadcast(P))
        kaB, b_ka = bload("kaB", [P, 512], I["rwkv_k_a"][l:l + 1, :].partition_broadcast(P))
        rkB, b_rk = bload("rkB", [P, 512], I["rwkv_r_k"][l:l + 1].rearrange("o h n -> o (h n)").partition_broadcast(P))
        w0B, b_w0 = bload("w0B", [P, 2, 512], I["rwkv_w0"][l:l + 1].rearrange("o d n -> o (d n)").partition_broadcast(P))
        a0B, b_a0 = bload("a0B", [P, 2, 512], I["rwkv_a0"][l:l + 1].rearrange("o d n -> o (d n)").partition_broadcast(P))
        gup, b_gup = bload("gup", [P, 512], I["rwkv_g_up"][l])
        WUZ, b_wuz = kb.sb([P, 2, 512], F32, "WUZ")
        AUZ, b_auz = kb.sb([P, 2, 512], F32, "AUZ")
        G(lambda: pool.memset(WUZ[:], 0.0), (), [b_wuz])
        G(lambda: pool.memset(AUZ[:], 0.0), (), [b_auz])
        for d in range(2):
            kb.dma("sp", WUZ[d * 64:(d + 1) * 64, d, :], I["rwkv_w_up"][l, d], (), [b_wuz])
            kb.dma("sp", AUZ[d * 64:(d + 1) * 64, d, :], I["rwkv_a_up"][l, d], (), [b_auz])
        omk, b_omk = kb.sb([P, 512], F32, "omk")
        V(lambda: vec.tensor_scalar(out=omk[:], in0=kaB[:], scalar1=-1.0, scalar2=1.0, op0=ALU.mult, op1=ALU.add), [b_ka], [b_omk])
        pc, bpc = kb.sb([P, 1920], F32, "pc")
        sh = [kb.sb([P, 1920], F32, "sh%d" % i) for i in range(4)]
        qs, bqs = kb.sb([P, 1920], F32, "qs")
        lo, blo = kb.sb([P, 3, P], F32, "lo")
        loT, bloT = kb.sb([P, 3, P], F32, "loT")
        OUTP, bOP = kb.sb([P, 9, 512], F32, "OUTP")
        OUTF, bOF = kb.sb([P, 2, 512], F32, "OUTF")
        ss, bss = kb.sb([P, 8, 1], F32, "rss")
        s2, bs2 = kb.sb([P, 8, 1], F32, "rs2")
        tmp, btmp = kb.sb([P, 512], F32, "rtmp")
        rr, brr = kb.sb([P, 512], F32, "rr")
        offs = (-1, 1, -64, 64)
        for t in range(NTT):
            isc = t < NCT
            r0 = prow(t)
            kb.dma("sp", pc[:], PTM[r0:r0 + P, 528:2448], [bPTM], [bpc])
            qs3 = qs[:].rearrange("p (n f) -> p n f", f=4)
            for cls in range(4):
                if isc and cls >= 2:
                    G(lambda: pool.memset(qs3[:, :, cls], 0.0), (), [bqs])
                    continue
                s_, bs_ = sh[cls]
                kb.dma("act" if cls % 2 else "sp", s_[:], PTM[r0 + offs[cls]:r0 + offs[cls] + P, 528:2448], [bPTM], [bs_])
                s3 = s_[:].rearrange("p (n f) -> p n f", f=4)
                if (not isc) and cls < 2:
                    V(lambda: vec.tensor_scalar(out=qs3[:, :, cls], in0=s3[:, :, cls], scalar1=mLR[:, cls:cls + 1], scalar2=None, op0=ALU.mult), [bs_, b_mLR], [bqs])
                else:
                    G(lambda: pool.tensor_copy(out=qs3[:, :, cls], in_=s3[:, :, cls]), [bs_], [bqs])
            V(lambda: vec.tensor_tensor(out=qs[:], in0=qs[:], in1=pc[:], op=ALU.subtract), [bqs, bpc], [bqs])
            V(lambda: vec.tensor_tensor(out=qs[:], in0=qs[:], in1=muB[:], op=ALU.mult), [bqs, b_mu], [bqs])
            V(lambda: vec.tensor_tensor(out=qs[:], in0=qs[:], in1=pc[:], op=ALU.add), [bqs, bpc], [bqs])
            r_, k_, v_ = qs[:, 0:512], qs[:, 512:1024], qs[:, 1024:1536]
            G(lambda: pool.tensor_copy(out=OUTP[:, 0, :], in_=r_), [bqs], [bOP])
            G(lambda: pool.tensor_copy(out=OUTP[:, 1, :], in_=v_), [bqs], [bOP])
            V(lambda: vec.tensor_tensor(out=OUTP[:, 2, :], in0=k_, in1=kkB[:], op=ALU.mult), [bqs, b_kk], [bOP])
            V(lambda: vec.tensor_tensor(out=tmp[:], in0=OUTP[:, 2, :], in1=OUTP[:, 2, :], op=ALU.mult), [bOP], [btmp])
            V(lambda: vec.tensor_reduce(out=ss[:, :, 0], in_=tmp[:].rearrange("p (h c) -> p h c", c=64), axis=AX.X, op=ALU.add), [btmp], [bss])
            V(lambda: vec.tensor_scalar(out=ss[:], in0=ss[:], scalar1=EPS, scalar2=None, op0=ALU.add), [bss], [bss])
            A(lambda: act.activation(out=ss[:], in_=ss[:], func=AF.Sqrt), [bss], [bss])
            V(lambda: vec.reciprocal(out=ss[:], in_=ss[:]), [bss], [bss])
            kk3 = OUTP[:, 2, :].rearrange("p (h c) -> p h c", c=64)
            V(lambda: vec.tensor_tensor(out=kk3, in0=kk3, in1=bc(ss[:], 64), op=ALU.mult), [bOP, bss], [bOP])
            A(lambda: act.activation(out=lo[:, 0, :], in_=qs[:, 1536:1664], func=AF.Tanh), [bqs], [blo])
            G(lambda: pool.tensor_copy(out=lo[:, 1, :], in_=qs[:, 1664:1792]), [bqs], [blo])
            A(lambda: act.activation(out=lo[:, 2, :], in_=qs[:, 1792:1920], func=AF.Sigmoid), [bqs], [blo])
            ps, bps = kb.bank()
            for j in range(3):
                T(lambda: pe.transpose(out=ps[:, j * P:(j + 1) * P], in_=lo[:, j, :], identity=ident[:]), [blo, b_id], [bps])
            V(lambda: vec.tensor_copy(out=loT[:], in_=ps[:, 0:384].rearrange("p (j c) -> p j c", c=P)), [bps], [bloT])
            ps, bps = kb.bank()
            mm(ps[:], loT[:, 2, :], gup[:], True, True, [bloT, b_gup], [bps])
            A(lambda: acopy(OUTF[:, 1, :], ps[:]), [bps], [bOF])
            V(lambda: vec.tensor_tensor(out=rr[:], in0=r_, in1=rkB[:], op=ALU.mult), [bqs, b_rk], [brr])
            for d in range(2):
                ps, bps = kb.bank()
                mm(ps[:], loT[:, 0, :], WUZ[:, d, :], True, True, [bloT, b_wuz], [bps])
                V(lambda: vec.tensor_tensor(out=tmp[:], in0=ps[:], in1=w0B[:, d, :], op=ALU.add), [bps, b_w0], [btmp])
                A(lambda: act.activation(out=tmp[:], in_=tmp[:], func=AF.Sigmoid), [btmp], [btmp])
                V(lambda: vec.tensor_scalar(out=OUTP[:, 3 + 3 * d, :], in0=tmp[:], scalar1=-math.exp(-0.5), scalar2=None, op0=ALU.mult), [btmp], [bOP])
                ps, bps = kb.bank()
                mm(ps[:], loT[:, 1, :], AUZ[:, d, :], True, True, [bloT, b_auz], [bps])
                V(lambda: vec.tensor_tensor(out=tmp[:], in0=ps[:], in1=a0B[:, d, :], op=ALU.add), [bps, b_a0], [btmp])
                A(lambda: act.activation(out=OUTP[:, 5 + 3 * d, :], in_=tmp[:], func=AF.Sigmoid), [btmp], [bOP])
                V(lambda: vec.tensor_tensor(out=tmp[:], in0=OUTP[:, 5 + 3 * d, :], in1=kaB[:], op=ALU.mult), [bOP, b_ka], [btmp])
                V(lambda: vec.tensor_tensor(out=tmp[:], in0=tmp[:], in1=omk[:], op=ALU.add), [btmp, b_omk], [btmp])
                V(lambda: vec.tensor_tensor(out=OUTP[:, 4 + 3 * d, :], in0=tmp[:], in1=k_, op=ALU.mult), [btmp, bqs], [bOP])
                V(lambda: vec.tensor_tensor(out=tmp[:], in0=rr[:], in1=OUTP[:, 4 + 3 * d, :], op=ALU.mult), [brr, bOP], [btmp])
                V(lambda: vec.tensor_reduce(out=(ss if d == 0 else s2)[:, :, 0], in_=tmp[:].rearrange("p (h c) -> p h c", c=64), axis=AX.X, op=ALU.add),
                  [btmp], [bss if d == 0 else bs2])
            V(lambda: vec.tensor_tensor(out=ss[:], in0=ss[:], in1=s2[:], op=ALU.add), [bss, bs2], [bss])
            V(lambda: vec.tensor_tensor(out=OUTF[:, 0, :].rearrange("p (h c) -> p h c", c=64), in0=v_.rearrange("p (h c) -> p h c", c=64),
                                        in1=bc(ss[:], 64), op=ALU.mult), [bqs, bss], [bOF])
            kb.dma("pool", RWP[t * P:(t + 1) * P, :].rearrange("p (j c) -> p j c", c=512), OUTP[:], [bOP], [bRWP])
            kb.dma("pool", RWF[t * P:(t + 1) * P, :].rearrange("p (j c) -> p j c", c=512), OUTF[:], [bOF], [bRWF])
        kb.release(m)

    def rwkv_chain(l, dsel):
        S, bS = kb.sb([P, 4, 64], F32R, "rS")
        rw, brw = kb.sb([P, 6, 512], F32, "rw")
        E, bE = kb.sb([P, 4, 512], F32, "E")
        X, bX = kb.sb([P, 4, 512], F32, "X")
        XE, bXE = kb.sb([P, 2, 512], F32R, "XE")
        bt_, bbt = kb.sb([P, 512], F32, "btmp")
        vr, bvr = kb.sb([P, 8, 64], F32R, "vr")
        Xz = [kb.sb([P, 8, P], F32, "Xz%d" % i) for i in range(4)]
        XTz = [kb.sb([P, 8, P], F32R, "XTz%d" % i) for i in range(4)]
        XTu = [kb.sb([P, 4, P], F32R, "XTu%d" % i) for i in range(2)]
        for i in range(4):
            G(lambda: pool.memset(Xz[i][0][:], 0.0), (), [Xz[i][1]])
        EWL, bEWL = kb.sb([P, 4], F32, "EWL")
        Pm = [kb.sb([P, 8, P], F32, "rPm") for _ in range(2)]
        Qm = [kb.sb([P, 8, P], F32, "rQm") for _ in range(2)]
        Y, bY = kb.sb([P, 8, P], F32, "rY")
        Yr, bYr = kb.sb([P, 8, P], F32R, "rYr")
        AAK, bAAK = kb.sb([P, 8, P], F32R, "AAK")
        RB, bRB = kb.sb([P, 8, P], F32R, "RB")
        RK, bRK = kb.sb([P, 8, P], F32R, "RK")
        Z, bZ = kb.sb([P, 8, 64], F32R, "Z")
        U, bU = kb.sb([P, 8, 64], F32R, "U")
        Yo, bYo = kb.sb([P, 8, 64], F32, "Yo")
        h3 = lambda ap: ap.rearrange("p (h c) -> p h c", c=64)
        for d in (dsel,):
            V(lambda: vec.tensor_scalar(out=S[:], in0=I4[:, :, 0:64], scalar1=0.0, scalar2=None, op0=ALU.mult), [b_I4], [bS])
            for t in scan_order(d):
                kb.dma("sp", rw[:, 0:3, :], RWP[t * P:(t + 1) * P, 0:1536].rearrange("p (j c) -> p j c", c=512), [bRWP], [brw])
                kb.dma("act", rw[:, 3:6, :], RWP[t * P:(t + 1) * P, (3 + 3 * d) * 512:(6 + 3 * d) * 512].rearrange("p (j c) -> p j c", c=512), [bRWP], [brw])
                r_, v_, kk_, lw_, kd_, a_ = (rw[:, j, :] for j in range(6))
                psA, bpsA = kb.bank()
                mm(psA[:], MI4[d][:, 0, :], lw_, True, True, [b_c, brw], [bpsA])
                psB, bpsB = kb.bank()
                mm(psB[:], ones[:], lw_, True, True, [b_on, brw], [bpsB])
                psC, bpsC = kb.bank()
                for b in range(4):
                    mm(psC[:, b:b + 1], lw_[:, b * P:(b + 1) * P], ones[:, 0:1], True, True, [brw, b_on], [bpsC])
                A(lambda: act.activation(out=EWL[:], in_=psC[:, 0:4], func=AF.Exp), [bpsC], [bEWL])
                A(lambda: act.activation(out=E[:, 0, :], in_=psA[:], func=AF.Exp), [bpsA], [bE])
                A(lambda: act.activation(out=E[:, 1, :], in_=psA[:], func=AF.Exp, scale=-1.0), [bpsA], [bE])
                V(lambda: vec.tensor_tensor(out=E[:, 2, :], in0=psA[:], in1=lw_, op=ALU.subtract), [bpsA, brw], [bE])
                A(lambda: act.activation(out=E[:, 2, :], in_=E[:, 2, :], func=AF.Exp), [bE], [bE])
                V(lambda: vec.tensor_copy(out=E[:, 3, :], in_=psB[:]), [bpsB], [bE])
                V(lambda: vec.tensor_tensor(out=E[:, 3, :], in0=E[:, 3, :], in1=psA[:], op=ALU.subtract), [bE, bpsA], [bE])
                A(lambda: act.activation(out=E[:, 3, :], in_=E[:, 3, :], func=AF.Exp), [bE], [bE])
                yield
                V(lambda: vec.tensor_tensor(out=bt_[:], in0=kk_, in1=a_, op=ALU.mult), [brw], [bbt])
                V(lambda: vec.scalar_tensor_tensor(out=X[:, 0, :], in0=kk_, scalar=-1.0, in1=E[:, 2, :], op0=ALU.mult, op1=ALU.mult), [brw, bE], [bX])
                G(lambda: pool.tensor_tensor(out=X[:, 1, :], in0=bt_[:], in1=E[:, 1, :], op=ALU.mult), [bbt, bE], [bX])
                V(lambda: vec.tensor_tensor(out=X[:, 2, :], in0=kd_, in1=E[:, 1, :], op=ALU.mult), [brw, bE], [bX])
                G(lambda: pool.tensor_tensor(out=X[:, 3, :], in0=r_, in1=E[:, 0, :], op=ALU.mult), [brw, bE], [bX])
                V(lambda: vec.tensor_tensor(out=XE[:, 0, :], in0=kd_, in1=E[:, 3, :], op=ALU.mult), [brw, bE], [bXE])
                G(lambda: pool.tensor_tensor(out=XE[:, 1, :], in0=bt_[:], in1=E[:, 3, :], op=ALU.mult), [bbt, bE], [bXE])
                G(lambda: pool.tensor_copy(out=vr[:], in_=h3(v_)), [brw], [bvr])
                yield
                for i in range(4):
                    xz, bxz = Xz[i]
                    for hh in range(2):
                        o_ = xz[:].rearrange("p (b h2) (s c) -> p b h2 s c", h2=2, s=2)[:, :, hh, hh, :]
                        i_ = X[:, i, :].rearrange("p (b h2 c) -> p b h2 c", h2=2, c=64)[:, :, hh, :]
                        (V if (i + hh) % 2 else G)(lambda: (vec if (i + hh) % 2 else pool).tensor_copy(out=o_, in_=i_), [bX], [bxz])
                    for half in range(2):
                        ps, bps = kb.bank()
                        for h in range(4):
                            T(lambda: pe.transpose(out=ps[:, h * P:(h + 1) * P], in_=xz[:, half * 4 + h, :], identity=ident[:]), [bxz, b_id], [bps])
                        if half:
                            A(lambda: acopy(XTz[i][0][:, 4:8, :], ps[:].rearrange("p (h c) -> p h c", c=P)), [bps], [XTz[i][1]])
                        else:
                            V(lambda: vec.tensor_copy(out=XTz[i][0][:, 0:4, :], in_=ps[:].rearrange("p (h c) -> p h c", c=P)), [bps], [XTz[i][1]])
                for j, i in enumerate((0, 3)):
                    z5 = f32(XTz[i][0][:]).rearrange("p (b h2) c -> p b h2 c", h2=2)
                    G(lambda: pool.tensor_tensor(out=XTu[j][0][:], in0=z5[:, :, 0, :], in1=z5[:, :, 1, :], op=ALU.add), [XTz[i][1]], [XTu[j][1]])
                ATz, BTz, KTz, RTz = (XTz[i][0] for i in range(4))
                bATz, bBTz, bKTz, bRTz = (XTz[i][1] for i in range(4))
                ATu, bATu = XTu[0]
                RTu, bRTu = XTu[1]
                yield
                for (lz, blz, ru, bru, dst, bdst, msk) in ((BTz, bBTz, ATu, bATu, Qm[0][0], Qm[0][1], NMS4[d]), (KTz, bKTz, ATu, bATu, AAK, bAAK, MS4[d]),
                                                            (BTz, bBTz, RTu, bRTu, RB, bRB, MI4[d]), (KTz, bKTz, RTu, bRTu, RK, bRK, MI4[d])):
                    for half in range(2):
                        ps, bps = kb.bank()
                        for h in range(4):
                            hg = half * 4 + h
                            mm(ps[:, h * P:(h + 1) * P], lz[:, hg, :], ru[:, hg // 2, :], True, True, [blz, bru], [bps])
                        V(lambda: vec.tensor_tensor(out=dst[:, half * 4:half * 4 + 4, :], in0=ps[:].rearrange("p (h c) -> p h c", c=P), in1=msk[:], op=ALU.mult),
                          [bps, b_c], [bdst])
                yield
                for half in range(2):
                    ps, bps = kb.bank()
                    for h in range(4):
                        T(lambda: pe.transpose(out=ps[:, h * P:(h + 1) * P], in_=f32(Qm[0][0][:, half * 4 + h, :]), identity=ident[:]), [Qm[0][1], b_id], [bps])
                    A(lambda: acopy(Pm[0][0][:, half * 4:half * 4 + 4, :], ps[:].rearrange("p (h c) -> p h c", c=P)), [bps], [Pm[0][1]])
                    V(lambda: vec.tensor_tensor(out=Y[:, half * 4:half * 4 + 4, :], in0=I4[:], in1=f32(Qm[0][0][:, half * 4:half * 4 + 4, :]), op=ALU.subtract),
                      [b_I4, Qm[0][1]], [bY])
                for half in range(2):
                    sl = slice(half * 4, half * 4 + 4)
                    yield from neumann([Pm[0][0][:, sl, :], Pm[1][0][:, sl, :]], [Pm[0][1], Pm[1][1]], [Qm[0][0][:, sl, :], Qm[1][0][:, sl, :]],
                            [Qm[0][1], Qm[1][1]], Y[:, sl, :], bY, 4)
                G(lambda: pool.tensor_copy(out=Yr[:], in_=Y[:]), [bY], [bYr])
                v8 = vr
                yield
                ps, bps = kb.bank()
                for h in range(8):
                    mm(ps[:, h * 64:(h + 1) * 64], ATz[:, h, :], S[:, h // 2, :], True, False, [bATz, bS], [bps])
                    mm(ps[:, h * 64:(h + 1) * 64], AAK[:, h, :], v8[:, h, :], False, True, [bAAK, bvr], [bps])
                A(lambda: acopy(Z[:], ps[:].rearrange("p (h c) -> p h c", c=64)), [bps], [bZ])
                ps, bps = kb.bank()
                for h in range(8):
                    mm(ps[:, h * 64:(h + 1) * 64], Yr[:, h, :], Z[:, h, :], True, True, [bYr, bZ], [bps])
                V(lambda: vec.tensor_copy(out=U[:], in_=ps[:].rearrange("p (h c) -> p h c", c=64)), [bps], [bU])
                ps, bps = kb.bank()
                for h in range(8):
                    mm(ps[:, h * 64:(h + 1) * 64], RTz[:, h, :], S[:, h // 2, :], True, False, [bRTz, bS], [bps])
                    mm(ps[:, h * 64:(h + 1) * 64], RB[:, h, :], U[:, h, :], False, False, [bRB, bU], [bps])
                    mm(ps[:, h * 64:(h + 1) * 64], RK[:, h, :], v8[:, h, :], False, True, [bRK, bvr], [bps])
                V(lambda: vec.tensor_copy(out=Yo[:], in_=ps[:].rearrange("p (h c) -> p h c", c=64)), [bps], [bYo])
                yield
                for b in range(4):
                    ps, bps = kb.bank()
                    mm(ps[:, 0:P], XE[:, 1, b * P:(b + 1) * P], U[:, 2 * b:2 * b + 2, :].rearrange("p h c -> p (h c)"), True, False, [bXE, bU], [bps])
                    mm(ps[:, 0:P], XE[:, 0, b * P:(b + 1) * P], vr[:, 2 * b:2 * b + 2, :].rearrange("p h c -> p (h c)"), False, True, [bXE, bvr], [bps])
                    for hh in range(2):
                        q0 = hh * 64
                        V(lambda: vec.scalar_tensor_tensor(out=S[q0:q0 + 64, b, :], in0=f32(S[q0:q0 + 64, b, :]), scalar=EWL[q0:q0 + 64, b:b + 1],
                                                           in1=ps[q0:q0 + 64, q0:q0 + 64], op0=ALU.mult, op1=ALU.add), [bS, bEWL, bps], [bS])
                kb.dma("pool", h3(OGR[d, t * P:(t + 1) * P, :]), Yo[:], [bYo], [bOGR])
                yield

    def rwkv_finish(l, skip_ctx_out):
        m = kb.mark()
        h3 = lambda ap: ap.rearrange("p (h c) -> p h c", c=64)
        lngB, b_lg = kb.sb([P, 512], F32, "lngB")
        lnbB, b_lb = kb.sb([P, 512], F32, "lnbB")
        kb.dma("sp", lngB[:], I["rwkv_ln_g"][l:l + 1, :].partition_broadcast(P), (), [b_lg])
        kb.dma("sp", lnbB[:], I["rwkv_ln_b"][l:l + 1, :].partition_broadcast(P), (), [b_lb])
        bufs = [[kb.sb([P, 8, 64], F32, "rf") for _ in range(3)] for _ in range(2)]
        fin2 = [kb.sb([P, 2, 512], F32, "fin") for _ in range(2)]
        st4 = [[kb.sb([P, 8, 1], F32, "rst") for _ in range(2)] for _ in range(2)]
        for t in range(NTT):
            if skip_ctx_out and t < NCT:
                continue
            (Yo, bYo), (og, bog), (tq, btq) = bufs[t % 2]
            fin, bfin = fin2[t % 2]
            (st, bst), (st2, bst2) = st4[t % 2]
            kb.dma("sp", Yo[:], h3(OGR[0, t * P:(t + 1) * P, :]), [bOGR], [bYo])
            kb.dma("act", og[:], h3(OGR[1, t * P:(t + 1) * P, :]), [bOGR], [bog])
            kb.dma("sp", fin[:], RWF[t * P:(t + 1) * P, :].rearrange("p (j c) -> p j c", c=512), [bRWF], [bfin])
            V(lambda: vec.tensor_tensor(out=Yo[:], in0=Yo[:], in1=og[:], op=ALU.add), [bYo, bog], [bYo])
            V(lambda: vec.tensor_reduce(out=st[:, :, 0], in_=Yo[:], axis=AX.X, op=ALU.add), [bYo], [bst])
            V(lambda: vec.tensor_scalar(out=st[:], in0=st[:], scalar1=1.0 / 64, scalar2=None, op0=ALU.mult), [bst], [bst])
            V(lambda: vec.tensor_tensor(out=Yo[:], in0=Yo[:], in1=bc(st[:], 64), op=ALU.subtract), [bYo, bst], [bYo])
            G(lambda: pool.tensor_tensor(out=tq[:], in0=Yo[:], in1=Yo[:], op=ALU.mult), [bYo], [btq])
            V(lambda: vec.tensor_reduce(out=st2[:, :, 0], in_=tq[:], axis=AX.X, op=ALU.add), [btq], [bst2])
            V(lambda: vec.tensor_scalar(out=st2[:], in0=st2[:], scalar1=1.0 / 64, scalar2=GN_EPS, op0=ALU.mult, op1=ALU.add), [bst2], [bst2])
            A(lambda: act.activation(out=st2[:], in_=st2[:], func=AF.Sqrt), [bst2], [bst2])
            V(lambda: vec.reciprocal(out=st2[:], in_=st2[:]), [bst2], [bst2])
            V(lambda: vec.tensor_tensor(out=Yo[:], in0=Yo[:], in1=bc(st2[:], 64), op=ALU.mult), [bYo, bst2], [bYo])
            G(lambda: pool.tensor_tensor(out=Yo[:], in0=Yo[:], in1=h3(lngB[:]), op=ALU.mult), [bYo, b_lg], [bYo])
            V(lambda: vec.tensor_tensor(out=Yo[:], in0=Yo[:], in1=h3(lnbB[:]), op=ALU.add), [bYo, b_lb], [bYo])
            G(lambda: pool.tensor_tensor(out=Yo[:], in0=Yo[:], in1=h3(fin[:, 0, :]), op=ALU.add), [bYo, bfin], [bYo])
            V(lambda: vec.tensor_tensor(out=Yo[:], in0=Yo[:], in1=h3(fin[:, 1, :]), op=ALU.mult), [bYo, bfin], [bYo])
            kb.dma("pool", h3(BR[t * P:(t + 1) * P, 512:1024]), Yo[:], [bYo], [bBR])
        kb.release(m)

    def stage_merge(l, skip_ctx):
        m = kb.mark()
        WBR, bWBR = kb.sb([P, 12, D], BF16, "WBR")
        WO, bWO = kb.sb([P, 8, D], BF16, "WO")
        stg = [kb.sb([P, 4, D], F32, "wstg") for _ in range(2)]
        for i in range(3):
            s_, bs_ = stg[i % 2]
            kb.dma("sp", s_[:], I["w_branch"][l, i].rearrange("(k p) n -> p k n", p=P), (), [bs_])
            (V if i % 2 else G)(lambda: (vec if i % 2 else pool).tensor_copy(out=WBR[:, 4 * i:4 * i + 4, :], in_=s_[:]), [bs_], [bWBR])
        for i in range(2):
            s_, bs_ = stg[(i + 1) % 2]
            kb.dma("sp", s_[:], I["w_out"][l][i * 512:(i + 1) * 512, :].rearrange("(k p) n -> p k n", p=P), (), [bs_])
            (V if i % 2 else G)(lambda: (vec if i % 2 else pool).tensor_copy(out=WO[:, 4 * i:4 * i + 4, :], in_=s_[:]), [bs_], [bWO])
        br, bbr = kb.sb([P, 1536], F32, "br")
        brb, bbrb = kb.sb([P, 1536], BF16, "brb")
        brT, bbrT = kb.sb([P, 12, P], BF16, "brT")
        sg, bsg = kb.sb([P, 3 * D], F32, "sg")
        mg, bmg = kb.sb([P, D], F32, "mg")
        mgb, bmgb = kb.sb([P, D], BF16, "mgb")
        mT, bmT = kb.sb([P, 8, P], BF16, "mT")
        tmp, btmp = kb.sb([P, 512], F32, "mtmp")
        xt, bx = kb.sb([P, D], F32, "mx")
        load_mods([(1, 2)] if skip_ctx else [(0, 2), (1, 2)])
        for t in range(NTT):
            if skip_ctx and t < NCT:
                continue
            s = 0 if t < NCT else 1
            kb.dma("sp", br[:], BR[t * P:(t + 1) * P, :], [bBR], [bbr])
            kb.dma("act", sg[:], PTM[prow(t):prow(t) + P, 4000:7072], [bPTM], [bsg])
            kb.dma("sp", xt[:], XR[t * P:(t + 1) * P, :], [bXR], [bx])
            V(lambda: vec.tensor_copy(out=brb[:], in_=br[:]), [bbr], [bbrb])
            A(lambda: act.activation(out=sg[:], in_=sg[:], func=AF.Sigmoid), [bsg], [bsg])
            for half in range(2):
                n_ = 8 if half == 0 else 4
                ps, bps = kb.bank()
                psb = ps[:].bitcast(BF16)
                for k in range(n_):
                    kk_ = half * 8 + k
                    T(lambda: pe.transpose(out=psb[:, k * P:(k + 1) * P], in_=brb[:, kk_ * P:(kk_ + 1) * P], identity=identb[:]), [bbrb, b_idb], [bps])
                A(lambda: acopy(brT[:, half * 8:half * 8 + n_, :], psb[:, 0:n_ * P].rearrange("p (k c) -> p k c", c=P)), [bps], [bbrT])
            for i in range(3):
                for half in range(2):
                    ps, bps = kb.bank()
                    for k in range(4):
                        mm(ps[:], brT[:, 4 * i + k, :], WBR[:, 4 * i + k, half * 512:(half + 1) * 512], k == 0, k == 3, [bbrT, bWBR], [bps])
                    gsl = sg[:, i * D + half * 512:i * D + (half + 1) * 512]
                    if i == 0:
                        V(lambda: vec.tensor_tensor(out=mg[:, half * 512:(half + 1) * 512], in0=ps[:], in1=gsl, op=ALU.mult), [bps, bsg], [bmg])
                    else:
                        V(lambda: vec.tensor_tensor(out=tmp[:], in0=ps[:], in1=gsl, op=ALU.mult), [bps, bsg], [btmp])
                        G(lambda: pool.tensor_tensor(out=mg[:, half * 512:(half + 1) * 512], in0=mg[:, half * 512:(half + 1) * 512], in1=tmp[:], op=ALU.add), [bmg, btmp], [bmg])
            V(lambda: vec.tensor_copy(out=mgb[:], in_=mg[:]), [bmg], [bmgb])
            ps, bps = kb.bank()
            psb = ps[:].bitcast(BF16)
            for k in range(8):
                T(lambda: pe.transpose(out=psb[:, k * P:(k + 1) * P], in_=mgb[:, k * P:(k + 1) * P], identity=identb[:]), [bmgb, b_idb], [bps])
            A(lambda: acopy(mT[:], psb.rearrange("p (k c) -> p k c", c=P)), [bps], [bmT])
            for half in range(2):
                ps, bps = kb.bank()
                for k in range(8):
                    mm(ps[:], mT[:, k, :], WO[:, k, half * 512:(half + 1) * 512], k == 0, k == 7, [bmT, bWO], [bps])
                V(lambda: vec.tensor_tensor(out=tmp[:], in0=ps[:], in1=MOD[(s, 2)][0][:, half * 512:(half + 1) * 512], op=ALU.mult), [bps, MOD[(s, 2)][1]], [btmp])
                V(lambda: vec.tensor_tensor(out=xt[:, half * 512:(half + 1) * 512], in0=xt[:, half * 512:(half + 1) * 512], in1=tmp[:], op=ALU.add), [bx, btmp], [bx])
            kb.dma("pool", XR[t * P:(t + 1) * P, :], xt[:], [bx], [bXR])
        kb.release(m)

    H2D, bH2D = scratch("H2D", [NT, D], BF16)
    YA, bYA = scratch("YA", [NT, D])

    def moe_wcast_chain(l):
        stg = [kb.sb([P, 4, 512], F32, "estg") for _ in range(2)]
        stb = [kb.sb([P, 4, 512], BF16, "estb") for _ in range(2)]
        ci = 0
        for e in range(NE):
            for (src, dst, bdst) in ((I["moe_w1"][l, e], W1B[e], bW1B), (I["moe_w3"][l, e], W3B[e], bW3B)):
                for hf in range(2):
                    s_, bs_ = stg[ci % 2]
                    b_, bb_ = stb[ci % 2]
                    kb.dma("sp", s_[:], src[hf * 512:(hf + 1) * 512, :].rearrange("(k p) n -> p k n", p=P), (), [bs_])
                    if ci % 2:
                        A(lambda: acopy(b_[:], s_[:]), [bs_], [bb_])
                    else:
                        G(lambda: pool.tensor_copy(out=b_[:], in_=s_[:]), [bs_], [bb_])
                    kb.dma("act", dst[hf * 512:(hf + 1) * 512, :].rearrange("(k p) n -> p k n", p=P), b_[:], [bb_], [bdst])
                    ci += 1
                    yield
            for hf in range(2):
                s_, bs_ = stg[ci % 2]
                b_, bb_ = stb[ci % 2]
                sv = s_[:].rearrange("p k n -> p (k n)").rearrange("p (k n) -> p k n", k=2)
                bv = b_[:].rearrange("p k n -> p (k n)").rearrange("p (k n) -> p k n", k=2)
                kb.dma("sp", sv, I["moe_w2"][l, e][hf * 256:(hf + 1) * 256, :].rearrange("(k p) n -> p k n", p=P), (), [bs_])
                if ci % 2:
                    A(lambda: acopy(bv, sv), [bs_], [bb_])
                else:
                    G(lambda: pool.tensor_copy(out=bv, in_=sv), [bs_], [bb_])
                kb.dma("act", W2B[e][hf * 256:(hf + 1) * 256, :].rearrange("(k p) n -> p k n", p=P), bv, [bb_], [bW2B])
                ci += 1
                yield

    def stage_moe(l, last):
        m = kb.mark()
        load_mods([(1, 3), (1, 4), (1, 5)] + ([] if last else [(0, 3), (0, 4), (0, 5)]))
        RW, bRW = kb.sb([P, 8, NE], F32, "RW")
        with nc.allow_non_contiguous_dma(reason="router w"):
            kb.dma("sp", RW[:], I["router_w"][l].rearrange("(k p) e -> p k e", p=P), (), [bRW])
        CAPM = max(1, 2 * TL // NE)
        PROBT, bPT = kb.sb([NE, NT], F32, "PROBT")
        xt, bx = kb.sb([P, D], F32, "ex")
        sc, bsc = kb.sb([P, 4], F32, "esc")
        zer, bzer = kb.sb([P, D], F32, "zer")
        G(lambda: pool.memset(zer[:], 0.0), (), [bzer])
        m_p1 = kb.mark()
        h2f, bh2 = kb.sb([P, D], F32, "h2f")
        hb, bhb = kb.sb([P, D], BF16, "ehb")
        h2T, bh2T = kb.sb([P, 8, P], F32, "h2T")
        lg, blg = kb.sb([P, NE], F32, "lg")
        sm, bsm = kb.sb([P, 4], F32, "sm")
        t_start = NCT if last else 0
        for t in range(t_start, NTT):
            s = 0 if t < NCT else 1
            kb.dma("sp", xt[:], XR[t * P:(t + 1) * P, :], [bXR], [bx])
            kb.dma("act", YA[t * P:(t + 1) * P, :], zer[:], [bzer], [bYA])
            norm_mod(xt[:], bx, s, 3 * D, 4 * D, h2f[:], bh2, sc, bsc)
            G(lambda: pool.tensor_copy(out=hb[:], in_=h2f[:]), [bh2], [bhb])
            kb.dma("pool", H2D[t * P:(t + 1) * P, :], hb[:], [bhb], [bH2D])
            for half in range(2):
                ps, bps = kb.bank()
                for k in range(4):
                    T(lambda: pe.transpose(out=ps[:, k * P:(k + 1) * P], in_=h2f[:, (half * 4 + k) * P:(half * 4 + k + 1) * P], identity=ident[:]), [bh2, b_id], [bps])
                (A if half else V)(lambda: (acopy(h2T[:, 4:8, :], ps[:].rearrange("p (k c) -> p k c", c=P)) if half else
                                            vec.tensor_copy(out=h2T[:, 0:4, :], in_=ps[:].rearrange("p (k c) -> p k c", c=P))), [bps], [bh2T])
            ps, bps = kb.bank()
            for k in range(8):
                mm(ps[:, 0:NE], h2T[:, k, :], RW[:, k, :], k == 0, k == 7, [bh2T, bRW], [bps])
            V(lambda: vec.tensor_copy(out=lg[:], in_=ps[:, 0:NE]), [bps], [blg])
            V(lambda: vec.tensor_reduce(out=sm[:, 0:1], in_=lg[:], axis=AX.X, op=ALU.max), [blg], [bsm])
            V(lambda: vec.tensor_scalar(out=sm[:, 1:2], in0=sm[:, 0:1], scalar1=-1.0, scalar2=None, op0=ALU.mult), [bsm], [bsm])
            A(lambda: act.activation(out=lg[:], in_=lg[:], func=AF.Exp, bias=sm[:, 1:2], accum_out=sm[:, 2:3]), [blg, bsm], [blg, bsm])
            V(lambda: vec.reciprocal(out=sm[:, 3:4], in_=sm[:, 2:3]), [bsm], [bsm])
            V(lambda: vec.tensor_scalar(out=lg[:], in0=lg[:], scalar1=sm[:, 3:4], scalar2=None, op0=ALU.mult), [blg, bsm], [blg])
            ps, bps = kb.bank()
            T(lambda: pe.transpose(out=ps[0:NE, 0:P], in_=lg[:], identity=ident[:]), [blg, b_id], [bps])
            V(lambda: vec.tensor_copy(out=PROBT[:, t * P:(t + 1) * P], in_=ps[0:NE, 0:P]), [bps], [bPT])
        kb.release(m_p1)
        WORK, bWK = kb.sb([NE, TL], F32, "WORK")
        VAL, bVAL = kb.sb([NE, CAPM], F32, "VAL")
        IDX, bIDX = kb.sb([NE, CAPM], U32, "IDX")
        IDXF, bIDXF = kb.sb([NE, CAPM], F32, "IDXF")
        NST = (CAPM + P - 1) // P
        IDXT, bIDXT = kb.sb([P, NST, NE], I32, "IDXT")
        AFFT, bAFFT = kb.sb([P, NST, NE], F32, "AFFT")
        w13 = [kb.sb([P, 2, 8, 512], BF16, "w13") for _ in range(2)]
        w2 = [kb.sb([P, 4, D], BF16, "w2") for _ in range(2)]
        xe = [kb.sb([P, D], BF16, "xe") for _ in range(2)]
        xeT, bxeT = kb.sb([P, 8, CAPM], BF16, "xeT")
        g1, bg1 = kb.sb([P, CAPM], F32, "g1")
        hid, bhid = kb.sb([P, 4, CAPM], BF16, "hid")
        ye = [kb.sb([P, D], F32, "ye") for _ in range(2)]
        wi = 0
        xi = 0
        for (a0, a1) in ((0, TC), (TC, NT)):
            if last and a0 == 0:
                continue
            n_ = a1 - a0
            cap = max(1, 2 * n_ // NE)
            V(lambda: vec.tensor_copy(out=WORK[:, 0:n_], in_=PROBT[:, a0:a1]), [bPT], [bWK])
            for r in range(cap // 8):
                V(lambda: vec.max(out=VAL[:, r * 8:(r + 1) * 8], in_=WORK[:, 0:n_]), [bWK], [bVAL])
                V(lambda: vec.max_index(out=IDX[:, r * 8:(r + 1) * 8], in_max=VAL[:, r * 8:(r + 1) * 8], in_values=WORK[:, 0:n_]), [bWK, bVAL], [bIDX])
                V(lambda: vec.match_replace(out=WORK[:, 0:n_], in_to_replace=VAL[:, r * 8:(r + 1) * 8], in_values=WORK[:, 0:n_], imm_value=-1.0), [bWK, bVAL], [bWK])
            V(lambda: vec.tensor_copy(out=IDXF[:, 0:cap], in_=IDX[:, 0:cap]), [bIDX], [bIDXF])
            if a0:
                V(lambda: vec.tensor_scalar(out=IDXF[:, 0:cap], in0=IDXF[:, 0:cap], scalar1=float(a0), scalar2=None, op0=ALU.add), [bIDXF], [bIDXF])
            sts = [(st, min(P, cap - st * P)) for st in range((cap + P - 1) // P)]
            for (st, w) in sts:
                ps, bps = kb.bank()
                T(lambda: pe.transpose(out=ps[0:w, 0:NE], in_=IDXF[:, st * P:st * P + w], identity=ident[0:NE, 0:NE]), [bIDXF, b_id], [bps])
                T(lambda: pe.transpose(out=ps[0:w, NE:2 * NE], in_=VAL[:, st * P:st * P + w], identity=ident[0:NE, 0:NE]), [bVAL, b_id], [bps])
                V(lambda: vec.tensor_copy(out=IDXT[0:w, st, :], in_=ps[0:w, 0:NE]), [bps], [bIDXT])
                V(lambda: vec.tensor_copy(out=AFFT[0:w, st, :], in_=ps[0:w, NE:2 * NE]), [bps], [bAFFT])
            for e in range(NE):
                wa, bwa = w13[wi % 2]
                wb_, bwb_ = w2[wi % 2]
                wi += 1
                kb.dma("sp", wa[:, 0], W1B[e].rearrange("(k p) n -> p k n", p=P), [bW1B], [bwa])
                kb.dma("act", wa[:, 1], W3B[e].rearrange("(k p) n -> p k n", p=P), [bW3B], [bwa])
                kb.dma("sp", wb_[:], W2B[e].rearrange("(k p) n -> p k n", p=P), [bW2B], [bwb_])
                for (st, w) in sts:
                    x_, bx_ = xe[xi % 2]
                    xi += 1
                    kb.idma(x_[0:w, :], None, H2D[:, :], bass.IndirectOffsetOnAxis(ap=IDXT[0:w, st, e:e + 1], axis=0), [bH2D, bIDXT], [bx_])
                    ps, bps = kb.bank()
                    psb = ps[:].bitcast(BF16)
                    for k in range(8):
                        T(lambda: pe.transpose(out=psb[:, k * P:k * P + w], in_=x_[0:w, k * P:(k + 1) * P], identity=identb[0:w, 0:w]), [bx_, b_idb], [bps])
                    A(lambda: acopy(xeT[:, :, st * P:st * P + w], psb.rearrange("p (k c) -> p k c", c=P)[:, :, 0:w]), [bps], [bxeT])
                for f in range(4):
                    ps1, bps1 = kb.bank()
                    for k in range(8):
                        mm(ps1[:, 0:cap], wa[:, 0, k, f * P:(f + 1) * P], xeT[:, k, 0:cap], k == 0, k == 7, [bwa, bxeT], [bps1])
                    ps3, bps3 = kb.bank()
                    for k in range(8):
                        mm(ps3[:, 0:cap], wa[:, 1, k, f * P:(f + 1) * P], xeT[:, k, 0:cap], k == 0, k == 7, [bwa, bxeT], [bps3])
                    A(lambda: act.activation(out=g1[:, 0:cap], in_=ps1[:, 0:cap], func=AF.Silu), [bps1], [bg1])
                    V(lambda: vec.tensor_tensor(out=hid[:, f, 0:cap], in0=g1[:, 0:cap], in1=ps3[:, 0:cap], op=ALU.mult), [bg1, bps3], [bhid])
                for (st, w) in sts:
                    y_, by_ = ye[xi % 2]
                    xi += 1
                    for half in range(2):
                        ps, bps = kb.bank()
                        for f in range(4):
                            mm(ps[0:w, :], hid[:, f, st * P:st * P + w], wb_[:, f, half * 512:(half + 1) * 512], f == 0, f == 3, [bhid, bwb_], [bps])
                        (V if half else A)(lambda: (vec.tensor_scalar(out=y_[0:w, half * 512:(half + 1) * 512], in0=ps[0:w, :], scalar1=AFFT[0:w, st, e:e + 1], scalar2=None, op0=ALU.mult)
                                                    if half else act.activation(out=y_[0:w, half * 512:(half + 1) * 512], in_=ps[0:w, :], func=AF.Copy, scale=AFFT[0:w, st, e:e + 1])),
                                           [bps, bAFFT], [by_])
                    kb.idma(YA[:, :], bass.IndirectOffsetOnAxis(ap=IDXT[0:w, st, e:e + 1], axis=0), y_[0:w, :], None, [by_, bIDXT], [bYA], compute_op=ALU.add)
        acc, bacc = kb.sb([P, D], F32, "acc")
        fgB, b_fg = kb.sb([P, D], F32, "fgB")
        if last:
            kb.dma("sp", fgB[:], I["final_g"][0:1, :].partition_broadcast(P), (), [b_fg])
        for t in range(t_start, NTT):
            s = 0 if t < NCT else 1
            kb.dma("sp", xt[:], XR[t * P:(t + 1) * P, :], [bXR], [bx])
            kb.dma("act", acc[:], YA[t * P:(t + 1) * P, :], [bYA], [bacc])
            V(lambda: vec.tensor_tensor(out=acc[:], in0=acc[:], in1=MOD[(s, 5)][0][:], op=ALU.mult), [bacc, MOD[(s, 5)][1]], [bacc])
            V(lambda: vec.tensor_tensor(out=xt[:], in0=xt[:], in1=acc[:], op=ALU.add), [bx, bacc], [bx])
            if not last:
                kb.dma("pool", XR[t * P:(t + 1) * P, :], xt[:], [bx], [bXR])
            else:
                junk, bj = sc_j
                A(lambda: act.activation(out=junk[:], in_=xt[:], func=AF.Square, accum_out=sc[:, 0:1]), [bx], [bj, bsc])
                V(lambda: vec.tensor_scalar(out=sc[:, 1:2], in0=sc[:, 0:1], scalar1=1.0 / D, scalar2=EPS, op0=ALU.mult, op1=ALU.add), [bsc], [bsc])
                A(lambda: act.activation(out=sc[:, 2:3], in_=sc[:, 1:2], func=AF.Sqrt), [bsc], [bsc])
                V(lambda: vec.reciprocal(out=sc[:, 3:4], in_=sc[:, 2:3]), [bsc], [bsc])
                V(lambda: vec.scalar_tensor_tensor(out=xt[:], in0=xt[:], scalar=sc[:, 3:4], in1=fgB[:], op0=ALU.mult, op1=ALU.mult), [bx, bsc, b_fg], [bx])
                kb.dma("pool", OUT[(t - NCT) * P:(t - NCT + 1) * P, :], xt[:], [bx], [bOUT])
        kb.release(m)

    for l in range(depth):
        last = (l == depth - 1)
        stage_mod(l)
        if upto >= 1:
            stage_proj(l)
        if upto >= 2:
            stage_conv(l)
        if upto >= 3:
            m_ = kb.mark()
            kb.run_chains([gdn_chain(l, 0), gdn_chain(l, 1)] + ([moe_wcast_chain(l)] if upto >= 7 else []))
            kb.release(m_)
            gdn_finish(l, last)
            stage_rwkv_prep(l)
            for d_ in range(2):
                m_ = kb.mark()
                kb.run_chains([rwkv_chain(l, d_), mlstm_chain(l, d_)])
                kb.release(m_)
            rwkv_finish(l, last)
            mlstm_finish(l, last)
        if upto >= 6:
            stage_merge(l, last)
        if upto >= 7:
            stage_moe(l, last)
    kb.barrier()
    kb.finish([bOUT, bXR, bPTM, bPQT, bQKV, bOGG, bOGM, bOGR, bBR, bRWP, bRWF])
    return kb


_CACHE = {}


def run(inputs, TC, TL, depth, ncores, dbg=(), upto=99):
    in_maps = []
    for b in range(ncores):
        m = {"x": np.ascontiguousarray(inputs["x"][b]), "c": np.ascontiguousarray(inputs["c"][b:b + 1]),
             "ctx": np.ascontiguousarray(inputs["ctx"][b]), "c_ctx": np.ascontiguousarray(np.asarray(inputs["c_ctx"])[None, :])}
        for n in WNAMES:
            a = np.asarray(inputs[n])
            m[n] = a if a.ndim > 1 else np.ascontiguousarray(a[None, :])
        in_maps.append(m)
    shapes = {n: tuple(in_maps[0][n].shape) for n in WNAMES}
    kb = build(TC, TL, depth, shapes, dbg, upto)
    res = run_bass_kernel_spmd(kb.nc, in_maps, core_ids=list(range(ncores)))
    return res.results, kb


def kernel(**inputs):
    inputs = {k: np.asarray(v) for k, v in inputs.items()}
    B, TL, _ = inputs["x"].shape
    TC = inputs["ctx"].shape[1]
    depth = inputs["w_in"].shape[0]
    results, _ = run(inputs, TC, TL, depth, B)
    return np.stack([r["out"] for r in results], axis=0).astype(np.float32)
```

```python
import math
import numpy as np
import concourse.bass as bass
import concourse.mybir as mybir
from concourse.bass_utils import run_bass_kernel_spmd

F32 = mybir.dt.float32
BF16 = mybir.dt.bfloat16
U32 = mybir.dt.uint32
I32 = mybir.dt.int32
F32R = mybir.dt.float32r


def f32(ap):
    return ap.bitcast(F32)
AF = mybir.ActivationFunctionType
ALU = mybir.AluOpType
AX = mybir.AxisListType

D = 1024
NMOD = 6
GH, GDK = 4, 128
RH, RN = 8, 64
MH, MDQK, MDV = 4, 64, 128
GDN_COLS = 4 * 512 + 16
RWKV_COLS = 3 * 512 + 128 + 128 + 128
MLSTM_COLS = 2 * 256 + 2 * 512 + 16
GATE_COLS = 3 * D
IN_COLS = GDN_COLS + RWKV_COLS + MLSTM_COLS + GATE_COLS
NE = 16
DFF = 512
EPS = 1e-6
GN_EPS = 64e-5
P = 128


class Buf:
    __slots__ = ("name", "w", "r", "multi", "ws")

    def __init__(self, name, multi=False):
        self.name = name
        self.w = None
        self.r = {}
        self.multi = multi
        self.ws = {}


class KB:
    def __init__(self):
        nc = bass.Bass("TRN2", target_bir_lowering=False)
        self.nc = nc
        self.eng = {"pe": nc.tensor, "dve": nc.vector, "act": nc.scalar, "pool": nc.gpsimd, "sp": nc.sync}
        self.sems = {}
        self.cnt = {}
        for e in ("pe", "dve", "act", "pool"):
            self.sems[e] = nc.semaphore("s_" + e).__enter__()
            self.cnt[e] = 0
        self.ndma = 20
        for i in range(self.ndma):
            k = "d%d" % i
            self.sems[k] = nc.semaphore("s_" + k).__enter__()
            self.cnt[k] = 0
        self.dma_rr = 0
        self.seen = {e: {} for e in self.eng}
        self.ninstr = 0
        self._uid = 0
        self.guards = []
        self.banks = []
        self.bank_rr = 0

    def init_psum(self):
        for i in range(8):
            t = self.nc.psum_tensor("bank%d" % i, [P, 512], F32).__enter__()
            self.banks.append((t, Buf("bank%d" % i)))

    def bank(self):
        g = getattr(self, "group", None)
        if g is None:
            b = self.banks[self.bank_rr]
            self.bank_rr = (self.bank_rr + 1) % 8
            return b
        i = self.grr[g]
        self.grr[g] = (i + 1) % 4
        return self.banks[g * 4 + i]

    def run_chains(self, chains):
        self.grr = [0, 0]
        active = list(enumerate(chains))
        while active:
            for it in list(active):
                self.group = it[0]
                try:
                    next(it[1])
                except StopIteration:
                    active.remove(it)
        self.group = None

    def barrier(self):
        allk = [(k, v) for k, v in self.cnt.items() if v > 0]
        for e in self.eng:
            self._wait(e, allk)

    def mark(self):
        return len(self.guards)

    def release(self, m):
        self.barrier()
        while len(self.guards) > m:
            g = self.guards.pop()
            g.__exit__(None, None, None)

    def sb(self, shape, dt=F32, name=None):
        self._uid += 1
        g = self.nc.sbuf_tensor("%s_%d" % (name or "sb", self._uid), list(shape), dt)
        t = g.__enter__()
        self.guards.append(g)
        return t, Buf(name or "sb")

    def ps(self, shape, dt=F32, name=None):
        self._uid += 1
        t = self.nc.psum_tensor("%s_%d" % (name or "ps", self._uid), list(shape), dt).__enter__()
        return t, Buf(name or "ps")

    def dram(self, name, shape, dt=F32, kind="Internal"):
        return self.nc.dram_tensor(name, list(shape), dt, kind=kind).ap()

    def _wait(self, e, deps):
        eng = self.eng[e]
        seen = self.seen[e]
        best = {}
        for d in deps:
            if d is None:
                continue
            k, v = d
            if best.get(k, 0) < v:
                best[k] = v
        for k, v in best.items():
            if e == "pe" and k == "pe":
                continue
            if seen.get(k, 0) < v:
                eng.wait_ge(self.sems[k], v)
                self.ninstr += 1
                seen[k] = v

    def _deps(self, reads, writes):
        deps = []
        for b in reads:
            deps.append(b.w)
            if b.multi:
                deps.extend(b.ws.items())
        for b in writes:
            if not b.multi:
                deps.append(b.w)
            for k, v in b.r.items():
                deps.append((k, v))
        return deps

    def _commit(self, key, val, reads, writes):
        for b in reads:
            if b.r.get(key, 0) < val:
                b.r[key] = val
        for b in writes:
            b.w = (key, val)
            b.r = {}
            if b.multi:
                if b.ws.get(key, 0) < val:
                    b.ws[key] = val

    def op(self, e, fn, reads=(), writes=()):
        self._wait(e, self._deps(reads, writes))
        ins = fn()
        self.cnt[e] += 1
        ins.then_inc(self.sems[e], 1)
        self.ninstr += 1
        self._commit(e, self.cnt[e], reads, writes)
        return ins

    def dma(self, q, out, in_, reads=(), writes=(), **kw):
        k = "d%d" % self.dma_rr
        self.dma_rr = (self.dma_rr + 1) % self.ndma
        deps = self._deps(reads, writes)
        if self.cnt[k] > 0:
            deps.append((k, self.cnt[k]))
        self._wait(q, deps)
        ins = self.eng[q].dma_start(out=out, in_=in_, **kw)
        self.cnt[k] += 16
        ins.then_inc(self.sems[k], 16)
        self.ninstr += 1
        self._commit(k, self.cnt[k], reads, writes)
        return ins

    def idma(self, out, out_offset, in_, in_offset, reads=(), writes=(), **kw):
        k = "d%d" % self.dma_rr
        self.dma_rr = (self.dma_rr + 1) % self.ndma
        deps = self._deps(reads, writes)
        if self.cnt[k] > 0:
            deps.append((k, self.cnt[k]))
        self._wait("pool", deps)
        ins = self.nc.gpsimd.indirect_dma_start(out=out, out_offset=out_offset, in_=in_, in_offset=in_offset, **kw)
        self.cnt[k] += 16
        ins.then_inc(self.sems[k], 16)
        self.ninstr += 1
        self._commit(k, self.cnt[k], reads, writes)
        return ins

    def finish(self, bufs):
        deps = []
        for b in bufs:
            deps.append(b.w)
        self._wait("sp", deps)


WNAMES = ["ada_w", "ada_b", "norm1_g", "norm2_g", "w_in", "gdn_conv", "gdn_a_log", "gdn_dt_bias",
          "gdn_norm_g", "rwkv_mu", "rwkv_w0", "rwkv_w_up", "rwkv_a0", "rwkv_a_up", "rwkv_g_up",
          "rwkv_k_k", "rwkv_k_a", "rwkv_r_k", "rwkv_ln_g", "rwkv_ln_b", "mlstm_i_bias",
          "mlstm_f_bias", "mlstm_norm_g", "w_branch", "w_out", "router_w", "moe_w1", "moe_w3",
          "moe_w2", "final_g"]


def build(TC, TL, depth, shapes, dbg=(), upto=99):
    kb = KB()
    nc = kb.nc
    kb.init_psum()
    NT = TC + TL
    NCT, NTT = TC // P, NT // P
    V, A, G, T = (lambda f, r=(), w=(): kb.op("dve", f, r, w)), (lambda f, r=(), w=(): kb.op("act", f, r, w)), \
        (lambda f, r=(), w=(): kb.op("pool", f, r, w)), (lambda f, r=(), w=(): kb.op("pe", f, r, w))
    vec, act, pool, pe = nc.vector, nc.scalar, nc.gpsimd, nc.tensor

    def mm(ps, lhsT, rhs, st, sp, r, w):
        return T(lambda: pe.matmul(ps, lhsT=lhsT, rhs=rhs, start=st, stop=sp), r, w)

    I = {}
    I["x"] = nc.dram_tensor("x", [TL, D], F32, kind="ExternalInput").ap()
    I["c"] = nc.dram_tensor("c", [1, D], F32, kind="ExternalInput").ap()
    I["ctx"] = nc.dram_tensor("ctx", [TC, D], F32, kind="ExternalInput").ap()
    I["c_ctx"] = nc.dram_tensor("c_ctx", [1, D], F32, kind="ExternalInput").ap()
    for n in WNAMES:
        I[n] = nc.dram_tensor(n, list(shapes[n]), F32, kind="ExternalInput").ap()
    OUT = nc.dram_tensor("out", [TL, D], F32, kind="ExternalOutput").ap()
    bOUT = Buf("out")

    def scratch(name, shape, dt=F32):
        k = "ExternalOutput" if name in dbg else "Internal"
        return nc.dram_tensor(name, list(shape), dt, kind=k).ap(), Buf(name, multi=(name != "YA"))

    NTM = IN_COLS - 1536
    XR, bXR = scratch("XR", [NT, D])
    PTM, bPTM = scratch("PTM", [NT + 192, NTM])
    PQT, bPQT = scratch("PQT", [1536, NT])
    QKV, bQKV = scratch("QKV", [NT, 1536])
    OGG, bOGG = scratch("OGG", [2, NT, 512])
    OGM, bOGM = scratch("OGM", [2, NT, 512])
    OGR, bOGR = scratch("OGR", [2, NT, 512])
    BR, bBR = scratch("BR", [NT, 1536])
    RWP, bRWP = scratch("RWP", [NT, 9 * 512])
    RWF, bRWF = scratch("RWF", [NT, 1024])
    W1B, bW1B = scratch("W1B", [NE, D, DFF], BF16)
    W3B, bW3B = scratch("W3B", [NE, D, DFF], BF16)
    W2B, bW2B = scratch("W2B", [NE, DFF, D], BF16)

    def prow(t):
        return (64 if t < NCT else 128) + t * P

    ident, b_id = kb.sb([P, P], F32, "ident")
    ones, b_on = kb.sb([P, P], F32, "ones")
    identb, b_idb = kb.sb([P, P], BF16, "identb")
    I4, b_I4 = kb.sb([P, 4, P], F32, "I4")
    MI4 = {}
    MS4 = {}
    NMS4 = {}
    b_c = Buf("consts")
    G(lambda: pool.memset(ones[:], 1.0), (), [b_on])
    G(lambda: pool.affine_select(out=ident[:], in_=ones[:], pattern=[[-1, P]], compare_op=ALU.is_equal,
                                 fill=0.0, base=0, channel_multiplier=1), [b_on], [b_id])
    V(lambda: vec.tensor_copy(out=identb[:], in_=ident[:]), [b_id], [b_idb])
    for h in range(4):
        V(lambda: vec.tensor_copy(out=I4[:, h, :], in_=ident[:]), [b_id], [b_I4])
    for d in range(2):
        MI4[d], _ = kb.sb([P, 4, P], F32, "MI4")
        MS4[d], _ = kb.sb([P, 4, P], F32, "MS4")
        NMS4[d], _ = kb.sb([P, 4, P], F32, "NMS4")
        sg = 1 if d == 0 else -1
        for h in range(4):
            G(lambda: pool.affine_select(out=MI4[d][:, h, :], in_=ones[:], pattern=[[sg, P]], compare_op=ALU.is_ge,
                                         fill=0.0, base=0, channel_multiplier=-sg), [b_on], [b_c])
            G(lambda: pool.affine_select(out=MS4[d][:, h, :], in_=ones[:], pattern=[[sg, P]], compare_op=ALU.is_gt,
                                         fill=0.0, base=0, channel_multiplier=-sg), [b_on], [b_c])
        G(lambda: pool.tensor_scalar(out=NMS4[d][:], in0=MS4[d][:], scalar1=-1.0, scalar2=None, op0=ALU.mult), [b_c], [b_c])
    mLR, b_mLR = kb.sb([P, 2], F32, "mLR")
    G(lambda: pool.memset(mLR[:], 1.0), (), [b_mLR])
    for base_p in (0, 64):
        pass
    for q in (0, 64):
        G(lambda: pool.affine_select(out=mLR[:, 0:1], in_=mLR[:, 0:1], pattern=[[0, 1]], compare_op=ALU.not_equal,
                                     fill=0.0, base=-q, channel_multiplier=1), [b_mLR], [b_mLR])
    for q in (63, 127):
        G(lambda: pool.affine_select(out=mLR[:, 1:2], in_=mLR[:, 1:2], pattern=[[0, 1]], compare_op=ALU.not_equal,
                                     fill=0.0, base=-q, channel_multiplier=1), [b_mLR], [b_mLR])

    kb.dma("sp", XR[0:TC, :], I["ctx"], (), [bXR])
    kb.dma("sp", XR[TC:NT, :], I["x"], (), [bXR])
    m0 = kb.mark()
    zt, b_zt = kb.sb([P, 1920], F32, "zeros")
    G(lambda: pool.memset(zt[:], 0.0), (), [b_zt])
    for r0 in (0, 64 + TC, 128 + NT):
        kb.dma("sp", PTM[r0:r0 + 64, 528:2448], zt[0:64, :], [b_zt], [bPTM])
    kb.release(m0)

    SBC = []
    with nc.allow_non_contiguous_dma(reason="tiny vector loads"):
        for s, nm in enumerate(("c_ctx", "c")):
            st_, b_st = kb.sb([P, 8, 1], F32, "sT")
            kb.dma("sp", st_[:, :, 0], I[nm].rearrange("o (k p) -> p (o k)", p=P), (), [b_st])
            A(lambda: act.activation(out=st_[:], in_=st_[:], func=AF.Silu), [b_st], [b_st])
            sbc, b_sbc = kb.sb([P, 8, P], F32, "SBC")
            V(lambda: vec.tensor_copy(out=sbc[:], in_=st_[:].to_broadcast([P, 8, P])), [b_st], [b_sbc])
            SBC.append((sbc, b_sbc))
    MODD, bMODD = scratch("MODD", [2, P, NMOD * D])
    MOD = {}

    def load_mods(items):
        for (s, idx) in items:
            t_, b_ = kb.sb([P, D], F32, "mod")
            kb.dma("act", t_[:], MODD[s][:, idx * D:(idx + 1) * D], [bMODD], [b_])
            MOD[(s, idx)] = (t_, b_)

    def stage_mod(l):
        m = kb.mark()
        MODB = [kb.sb([P, NMOD * D], F32, "MODB") for _ in range(2)]
        wn = [kb.sb([P, 8, 512], F32, "adaw") for _ in range(2)]
        brow = [kb.sb([1, 512], F32, "adab") for _ in range(2)]
        for n in range(12):
            w_, bw = wn[n % 2]
            br_, bbr = brow[n % 2]
            kb.dma("sp", w_[:], I["ada_w"][l][:, n * 512:(n + 1) * 512].rearrange("(k p) n -> p k n", p=P), (), [bw])
            kb.dma("sp", br_[:], I["ada_b"][l:l + 1, n * 512:(n + 1) * 512], (), [bbr])
            for s in range(2):
                ps, bps = kb.bank()
                for k in range(8):
                    mm(ps[:], SBC[s][0][:, k, :], w_[:, k, :], k == 0, False, [SBC[s][1], bw], [bps])
                mm(ps[:], ones[0:1, :], br_[0:1, :], False, True, [b_on, bbr], [bps])
                (A if s else V)(lambda: (act.copy if s else vec.tensor_copy)(out=MODB[s][0][:, n * 512:(n + 1) * 512], in_=ps[:]),
                                [bps], [MODB[s][1]])
        gb, b_gb = kb.sb([P, D], F32, "gB")
        for nm, c0 in (("norm1_g", 1 * D), ("norm2_g", 4 * D)):
            kb.dma("sp", gb[:], I[nm][l:l + 1, :].partition_broadcast(P), (), [b_gb])
            for s in range(2):
                V(lambda: vec.scalar_tensor_tensor(out=MODB[s][0][:, c0:c0 + D], in0=MODB[s][0][:, c0:c0 + D], scalar=1.0,
                                                   in1=gb[:], op0=ALU.add, op1=ALU.mult), [b_gb, MODB[s][1]], [MODB[s][1]])
        for s in range(2):
            kb.dma("sp", MODD[s], MODB[s][0][:], [MODB[s][1]], [bMODD])
        kb.release(m)

    def norm_mod(xt, bx, s, c_shift, c_scale, hb, bhb, sc, bsc, jk=None):
        msc, bmsc = MOD[(s, c_scale // D)]
        msh, bmsh = MOD[(s, c_shift // D)]
        junk, bj = jk or sc_j
        A(lambda: act.activation(out=junk[:], in_=xt, func=AF.Square, accum_out=sc[:, 0:1]), [bx], [bj, bsc])
        V(lambda: vec.tensor_scalar(out=sc[:, 1:2], in0=sc[:, 0:1], scalar1=1.0 / D, scalar2=EPS, op0=ALU.mult, op1=ALU.add), [bsc], [bsc])
        A(lambda: act.activation(out=sc[:, 2:3], in_=sc[:, 1:2], func=AF.Sqrt), [bsc], [bsc])
        V(lambda: vec.reciprocal(out=sc[:, 3:4], in_=sc[:, 2:3]), [bsc], [bsc])
        V(lambda: vec.scalar_tensor_tensor(out=junk[:], in0=xt, scalar=sc[:, 3:4], in1=msc[:],
                                           op0=ALU.mult, op1=ALU.mult), [bx, bsc, bmsc], [bj])
        V(lambda: vec.tensor_tensor(out=hb, in0=junk[:], in1=msh[:], op=ALU.add), [bj, bmsh], [bhb])

    sc_j = kb.sb([P, D], F32, "junk")
    HTbox = [None, None]

    def to_HT(hb, bhb, t):
        HT, b_HT = HTbox
        ps, bps = kb.bank()
        psb = ps[:].bitcast(BF16)
        for k in range(8):
            T(lambda: pe.transpose(out=psb[:, k * P:(k + 1) * P], in_=hb[:, k * P:(k + 1) * P], identity=identb[:]), [bhb, b_idb], [bps])
        A(lambda: act.copy(out=HT[:, :, t * P:(t + 1) * P], in_=psb.rearrange("p (k t) -> p k t", k=8)), [bps], [b_HT])

    def stage_proj(l):
        m = kb.mark()
        HTbox[0], HTbox[1] = kb.sb([P, 8, NT], BF16, "HT")
        HT, b_HT = HTbox
        load_mods([(0, 0), (0, 1), (1, 0), (1, 1)])
        xt2 = [kb.sb([P, D], F32, "xt") for _ in range(2)]
        hb2 = [kb.sb([P, D], BF16, "hb") for _ in range(2)]
        sc2 = [kb.sb([P, 4], F32, "sc") for _ in range(2)]
        for t in range(NTT):
            xt, bx = xt2[t % 2]
            hb, bhb = hb2[t % 2]
            sc, bsc = sc2[t % 2]
            kb.dma("sp", xt[:], XR[t * P:(t + 1) * P, :], [bXR], [bx])
            norm_mod(xt[:], bx, 0 if t < NCT else 1, 0, D, hb[:], bhb, sc, bsc)
            to_HT(hb, bhb, t)
        wf2 = [kb.sb([P, 8, 512], F32, "wf") for _ in range(2)]
        wb2 = [kb.sb([P, 8, 512], BF16, "wb") for _ in range(2)]
        ob2 = [kb.sb([P, 512], F32, "ob") for _ in range(4)]
        nblk = (IN_COLS + 511) // 512
        oi = 0
        for bi in range(nblk):
            c0 = bi * 512
            cw = min(512, IN_COLS - c0)
            wf, bwf = wf2[bi % 2]
            wb, bwb = wb2[bi % 2]
            kb.dma("sp", wf[:, :, 0:cw], I["w_in"][l][:, c0:c0 + cw].rearrange("(k p) n -> p k n", p=P), (), [bwf])
            (G if bi % 2 else V)(lambda: (pool if bi % 2 else vec).tensor_copy(out=wb[:, :, 0:cw], in_=wf[:, :, 0:cw]), [bwf], [bwb])
            if c0 < 1536:
                for cc in range(4):
                    for t0 in range(0, NT, 512):
                        tw = min(512, NT - t0)
                        ps, bps = kb.bank()
                        for k in range(8):
                            mm(ps[:, 0:tw], wb[:, k, cc * P:(cc + 1) * P], HT[:, k, t0:t0 + tw], k == 0, k == 7, [bwb, b_HT], [bps])
                        ob, bob = ob2[oi % 4]
                        oi += 1
                        (A if oi % 2 else V)(lambda: (act.copy if oi % 2 else vec.tensor_copy)(out=ob[:, 0:tw], in_=ps[:, 0:tw]), [bps], [bob])
                        kb.dma("pool", PQT[c0 + cc * P:c0 + (cc + 1) * P, t0:t0 + tw], ob[:, 0:tw], [bob], [bPQT])
            else:
                for t in range(NTT):
                    ps, bps = kb.bank()
                    for k in range(8):
                        mm(ps[:, 0:cw], HT[:, k, t * P:(t + 1) * P], wb[:, k, 0:cw], k == 0, k == 7, [bwb, b_HT], [bps])
                    ob, bob = ob2[oi % 4]
                    oi += 1
                    (A if oi % 2 else V)(lambda: (act.copy if oi % 2 else vec.tensor_copy)(out=ob[:, 0:cw], in_=ps[:, 0:cw]), [bps], [bob])
                    kb.dma("pool", PTM[prow(t):prow(t) + P, c0 - 1536:c0 - 1536 + cw], ob[:, 0:cw], [bob], [bPTM])
        kb.release(m)

    def acopy(out, in_):
        return act.activation(out=out, in_=in_, func=AF.Copy)

    def stage_conv(l):
        m = kb.mark()
        ROWS = TL // 64
        X2 = [kb.sb([P, NT], F32, "cx") for _ in range(2)]
        A2 = [kb.sb([P, NT], F32, "ca") for _ in range(2)]
        cw, b_cw = kb.sb([P, 12, 9], F32, "cw")
        with nc.allow_non_contiguous_dma(reason="conv w"):
            for ct in range(12):
                kb.dma("sp", cw[:, ct, :], I["gdn_conv"][l].rearrange("a b (t c) -> t c (a b)", c=P)[ct], (), [b_cw])
        ob2 = [kb.sb([P, 4, P], F32, "cob") for _ in range(2)]
        oi = 0
        for ct in range(12):
            X, bX = X2[ct % 2]
            AC, bA = A2[ct % 2]
            kb.dma("sp", X[:], PQT[ct * P:(ct + 1) * P, :], [bPQT], [bX])
            V(lambda: vec.tensor_scalar(out=AC[:], in0=X[:], scalar1=cw[:, ct, 4:5], scalar2=None, op0=ALU.mult), [bX, b_cw], [bA])
            for dx in (-1, 1):
                tap = 3 + dx + 1
                o0, o1 = max(0, -dx), TC - max(0, dx)
                V(lambda: vec.scalar_tensor_tensor(out=AC[:, o0:o1], in0=X[:, o0 + dx:o1 + dx], scalar=cw[:, ct, tap:tap + 1],
                                                   in1=AC[:, o0:o1], op0=ALU.mult, op1=ALU.add), [bX, b_cw, bA], [bA])
            Xl = X[:, TC:NT].rearrange("p (r c) -> p r c", c=64)
            Al = AC[:, TC:NT].rearrange("p (r c) -> p r c", c=64)
            for dy in (-1, 0, 1):
                for dx in (-1, 0, 1):
                    if dy == 0 and dx == 0:
                        continue
                    tap = (dy + 1) * 3 + dx + 1
                    r0, r1 = max(0, -dy), ROWS - max(0, dy)
                    c0, c1 = max(0, -dx), 64 - max(0, dx)
                    V(lambda: vec.scalar_tensor_tensor(out=Al[:, r0:r1, c0:c1], in0=Xl[:, r0 + dy:r1 + dy, c0 + dx:c1 + dx],
                                                       scalar=cw[:, ct, tap:tap + 1], in1=Al[:, r0:r1, c0:c1],
                                                       op0=ALU.mult, op1=ALU.add), [bX, b_cw, bA], [bA])
            A(lambda: act.activation(out=AC[:], in_=AC[:], func=AF.Silu), [bA], [bA])
            for t0 in range(0, NTT, 4):
                n_ = min(4, NTT - t0)
                ps, bps = kb.bank()
                for j in range(n_):
                    T(lambda: pe.transpose(out=ps[:, j * P:(j + 1) * P], in_=AC[:, (t0 + j) * P:(t0 + j + 1) * P], identity=ident[:]),
                      [bA, b_id], [bps])
                ob, bob = ob2[oi % 2]
                oi += 1
                A(lambda: acopy(ob[:, 0:n_, :], ps[:, 0:n_ * P].rearrange("p (j c) -> p j c", c=P)), [bps], [bob])
                kb.dma("pool", QKV[t0 * P:(t0 + n_) * P, ct * P:(ct + 1) * P].rearrange("(j p) c -> p j c", p=P), ob[:, 0:n_, :], [bob], [bQKV])
        kb.release(m)

    def scan_order(d):
        if d == 0:
            return list(range(NTT))
        return list(range(NCT))[::-1] + list(range(NCT, NTT))[::-1]

    def bc(ap, n):
        return ap.to_broadcast([P, ap.shape[1], n])

    def neumann(Pm, bP, Qm, bQ, Y, bY, nb, levels=6):
        cur = 0
        for lv in range(levels):
            nxt = 1 - cur
            psP, bpsP = kb.bank()
            for h in range(nb):
                mm(psP[:, h * P:(h + 1) * P], Qm[cur][:, h, :], Pm[cur][:, h, :], True, True, [bP[cur], bQ[cur]], [bpsP])
            A(lambda: acopy(Pm[nxt][:, 0:nb, :], psP[:, 0:nb * P].rearrange("p (h c) -> p h c", c=P)), [bpsP], [bP[nxt]])
            if lv < levels - 1:
                psQ, bpsQ = kb.bank()
                for h in range(nb):
                    mm(psQ[:, h * P:(h + 1) * P], Pm[cur][:, h, :], Qm[cur][:, h, :], True, True, [bP[cur], bQ[cur]], [bpsQ])
                V(lambda: vec.tensor_copy(out=Qm[nxt][:, 0:nb, :], in_=psQ[:, 0:nb * P].rearrange("p (h c) -> p h c", c=P)), [bpsQ], [bQ[nxt]])
            psY, bpsY = kb.bank()
            for h in range(nb):
                mm(psY[:, h * P:(h + 1) * P], Pm[nxt][:, h, :], Y[:, h, :], True, True, [bP[nxt], bY], [bpsY])
            V(lambda: vec.tensor_tensor(out=Y[:, 0:nb, :], in0=f32(Y[:, 0:nb, :]), in1=psY[:, 0:nb * P].rearrange("p (h c) -> p h c", c=P), op=ALU.add),
              [bpsY, bY], [bY])
            cur = nxt
            yield

    def decay_T(gB, b_gB, Gcol, bG, d, dect, b_dect, nh=4):
        ps, bps = kb.bank()
        for h in range(nh):
            mm(ps[:, h * P:(h + 1) * P], gB[:, h, :], MI4[d][:, 0, :], True, True, [b_gB, b_c], [bps])
        V(lambda: vec.tensor_tensor(out=dect[:, 0:nh, :], in0=ps[:, 0:nh * P].rearrange("p (h c) -> p h c", c=P), in1=bc(Gcol, P), op=ALU.subtract),
          [bps, bG], [b_dect])
        V(lambda: vec.tensor_scalar(out=dect[:, 0:nh, :], in0=dect[:, 0:nh, :], scalar1=0.0, scalar2=None, op0=ALU.min), [b_dect], [b_dect])
        A(lambda: act.activation(out=dect[:, 0:nh, :], in_=dect[:, 0:nh, :], func=AF.Exp), [b_dect], [b_dect])
        V(lambda: vec.tensor_tensor(out=dect[:, 0:nh, :], in0=dect[:, 0:nh, :], in1=MI4[d][:, 0:nh, :], op=ALU.mult), [b_dect, b_c], [b_dect])

    def gdn_chain(l, dsel):
        S, bS = kb.sb([P, 4, P], F32R, "gS")
        aexp, b_ae = kb.sb([P, 8], F32, "aexp")
        dtb, b_dt = kb.sb([P, 8], F32, "dtb")
        kb.dma("sp", aexp[:], I["gdn_a_log"][l:l + 1].rearrange("o d h -> o (d h)").partition_broadcast(P), (), [b_ae])
        kb.dma("sp", dtb[:], I["gdn_dt_bias"][l:l + 1].rearrange("o d h -> o (d h)").partition_broadcast(P), (), [b_dt])
        A(lambda: act.activation(out=aexp[:], in_=aexp[:], func=AF.Exp), [b_ae], [b_ae])
        V(lambda: vec.tensor_scalar(out=aexp[:], in0=aexp[:], scalar1=-1.0, scalar2=None, op0=ALU.mult), [b_ae], [b_ae])
        qkv, bq = kb.sb([P, 1536], F32, "qkv")
        gt, bgt = kb.sb([P, 16], F32, "gt")
        tmp, btmp = kb.sb([P, 8, P], F32, "tmp")
        ss, bss = kb.sb([P, 8, 1], F32, "ss")
        qkh, bqkh = kb.sb([P, 8, P], F32, "qkh")
        CF, bCF = kb.sb([P, 8, 4, 1], F32, "CF")
        gB, b_gB = kb.sb([P, 4, P], F32, "gB")
        dect, b_dect = kb.sb([P, 4, P], F32, "dect")
        dects, b_dects = kb.sb([P, 4, P], F32, "dects")
        KBt, bKBt = kb.sb([P, 4, P], F32, "kb_")
        R2, bR2 = kb.sb([P, 4, P], F32R, "R2")
        KG, bKG = kb.sb([P, 4, P], F32R, "KG")
        QG, bQG = kb.sb([P, 4, P], F32, "qg")
        R1, bR1 = kb.sb([P, 4, P], F32R, "R1")
        KT, bKT = kb.sb([P, 4, P], F32R, "KT")
        QT, bQT = kb.sb([P, 4, P], F32R, "QT")
        KBT, bKBT = kb.sb([P, 4, P], F32R, "KBT")
        QGT, bQGT = kb.sb([P, 4, P], F32R, "QGT")
        AT, bAT = kb.sb([P, 4, P], F32R, "AT")
        Pm = [kb.sb([P, 4, P], F32, "Pm") for _ in range(2)]
        Qm = [kb.sb([P, 4, P], F32, "Qm") for _ in range(2)]
        Y, bY = kb.sb([P, 4, P], F32, "Y")
        Yr, bYr = kb.sb([P, 4, P], F32R, "Yr")
        NWT, bNWT = kb.sb([P, 4, P], F32R, "NWT")
        VN, bVN = kb.sb([P, 4, P], F32R, "VN")
        O, bO = kb.sb([P, 4, P], F32, "O")
        for d in (dsel,):
            V(lambda: vec.tensor_scalar(out=S[:], in0=I4[:], scalar1=0.0, scalar2=None, op0=ALU.mult), [b_I4], [bS])
            for t in scan_order(d):
                kb.dma("sp", qkv[:], QKV[t * P:(t + 1) * P, :], [bQKV], [bq])
                kb.dma("sp", gt[:], PTM[prow(t):prow(t) + P, 512:528], [bPTM], [bgt])
                A(lambda: act.activation(out=CF[:, 0, :, 0], in_=gt[:, d * 4:(d + 1) * 4], func=AF.Sigmoid), [bgt], [bCF])
                V(lambda: vec.tensor_tensor(out=CF[:, 6, :, 0], in0=gt[:, 8 + d * 4:12 + d * 4], in1=dtb[:, d * 4:(d + 1) * 4], op=ALU.add), [bgt, b_dt], [bCF])
                A(lambda: act.activation(out=CF[:, 6, :, 0], in_=CF[:, 6, :, 0], func=AF.Exp), [bCF], [bCF])
                V(lambda: vec.tensor_scalar(out=CF[:, 6, :, 0], in0=CF[:, 6, :, 0], scalar1=1.0, scalar2=None, op0=ALU.add), [bCF], [bCF])
                A(lambda: act.activation(out=CF[:, 6, :, 0], in_=CF[:, 6, :, 0], func=AF.Ln), [bCF], [bCF])
                V(lambda: vec.tensor_tensor(out=CF[:, 6, :, 0], in0=CF[:, 6, :, 0], in1=aexp[:, d * 4:(d + 1) * 4], op=ALU.mult), [bCF, b_ae], [bCF])
                yield
                qk3 = qkv[:, 0:1024].rearrange("p (h c) -> p h c", c=P)
                V(lambda: vec.tensor_tensor(out=tmp[:], in0=qk3, in1=qk3, op=ALU.mult), [bq], [btmp])
                V(lambda: vec.tensor_reduce(out=ss[:, :, 0], in_=tmp[:], axis=AX.X, op=ALU.add), [btmp], [bss])
                V(lambda: vec.tensor_scalar(out=ss[:], in0=ss[:], scalar1=EPS, scalar2=None, op0=ALU.add), [bss], [bss])
                A(lambda: act.activation(out=ss[:], in_=ss[:], func=AF.Sqrt), [bss], [bss])
                V(lambda: vec.reciprocal(out=ss[:], in_=ss[:]), [bss], [bss])
                V(lambda: vec.tensor_scalar(out=ss[:, 0:4, :], in0=ss[:, 0:4, :], scalar1=GDK ** -0.5, scalar2=None, op0=ALU.mult), [bss], [bss])
                V(lambda: vec.tensor_tensor(out=qkh[:], in0=qk3, in1=bc(ss[:], P), op=ALU.mult), [bq, bss], [bqkh])
                yield
                psg, bpsg = kb.bank()
                mm(psg[:, 0:4], MI4[d][:, 0, :], CF[:, 6, :, 0], True, True, [b_c, bCF], [bpsg])
                mm(psg[:, 4:8], ones[:], CF[:, 6, :, 0], True, True, [b_on, bCF], [bpsg])
                V(lambda: vec.tensor_copy(out=CF[:, 5, :, 0], in_=psg[:, 0:4]), [bpsg], [bCF])
                V(lambda: vec.tensor_copy(out=CF[:, 7, :, 0], in_=psg[:, 4:8]), [bpsg], [bCF])
                A(lambda: act.activation(out=CF[:, 1, :, 0], in_=CF[:, 5, :, 0], func=AF.Exp), [bCF], [bCF])
                A(lambda: act.activation(out=CF[:, 2, :, 0], in_=CF[:, 7, :, 0], func=AF.Exp), [bCF], [bCF])
                V(lambda: vec.tensor_tensor(out=CF[:, 3, :, 0], in0=CF[:, 7, :, 0], in1=CF[:, 5, :, 0], op=ALU.subtract), [bCF], [bCF])
                A(lambda: act.activation(out=CF[:, 3, :, 0], in_=CF[:, 3, :, 0], func=AF.Exp), [bCF], [bCF])
                V(lambda: vec.tensor_tensor(out=CF[:, 4, :, 0], in0=CF[:, 0, :, 0], in1=CF[:, 1, :, 0], op=ALU.mult), [bCF], [bCF])
                yield
                kh = qkh[:, 4:8, :]
                qh = qkh[:, 0:4, :]
                v3 = qkv[:, 1024:1536].rearrange("p (h c) -> p h c", c=P)
                V(lambda: vec.tensor_tensor(out=KBt[:], in0=kh, in1=bc(CF[:, 0], P), op=ALU.mult), [bqkh, bCF], [bKBt])
                G(lambda: pool.tensor_tensor(out=R2[:], in0=kh, in1=bc(CF[:, 4], P), op=ALU.mult), [bqkh, bCF], [bR2])
                V(lambda: vec.tensor_tensor(out=KG[:], in0=kh, in1=bc(CF[:, 3], P), op=ALU.mult), [bqkh, bCF], [bKG])
                G(lambda: pool.tensor_tensor(out=QG[:], in0=qh, in1=bc(CF[:, 1], P), op=ALU.mult), [bqkh, bCF], [bQG])
                V(lambda: vec.tensor_tensor(out=R1[:], in0=v3, in1=bc(CF[:, 0], P), op=ALU.mult), [bq, bCF], [bR1])
                G(lambda: pool.tensor_copy(out=gB[:], in_=bc(CF[:, 6], P)), [bCF], [b_gB])
                yield
                for src, bsrc, dst, bdst, eng in ((kh, bqkh, KT, bKT, "a"), (qh, bqkh, QT, bQT, "v"), (KBt[:], bKBt, KBT, bKBT, "a"), (QG[:], bQG, QGT, bQGT, "v")):
                    ps, bps = kb.bank()
                    for h in range(4):
                        T(lambda: pe.transpose(out=ps[:, h * P:(h + 1) * P], in_=src[:, h, :], identity=ident[:]), [bsrc, b_id], [bps])
                    if eng == "a":
                        A(lambda: acopy(dst[:], ps[:].rearrange("p (h c) -> p h c", c=P)), [bps], [bdst])
                    else:
                        V(lambda: vec.tensor_copy(out=dst[:], in_=ps[:].rearrange("p (h c) -> p h c", c=P)), [bps], [bdst])
                yield
                decay_T(gB, b_gB, CF[:, 5], bCF, d, dect, b_dect)
                G(lambda: pool.tensor_tensor(out=dects[:], in0=dect[:], in1=I4[:], op=ALU.subtract), [b_dect, b_I4], [b_dects])
                yield
                ps, bps = kb.bank()
                for h in range(4):
                    mm(ps[:, h * P:(h + 1) * P], KT[:, h, :], KBT[:, h, :], True, True, [bKT, bKBT], [bps])
                V(lambda: vec.tensor_tensor(out=Qm[0][0][:], in0=ps[:].rearrange("p (h c) -> p h c", c=P), in1=dects[:], op=ALU.mult), [bps, b_dects], [Qm[0][1]])
                ps, bps = kb.bank()
                for h in range(4):
                    mm(ps[:, h * P:(h + 1) * P], KT[:, h, :], QT[:, h, :], True, True, [bKT, bQT], [bps])
                V(lambda: vec.tensor_tensor(out=AT[:], in0=ps[:].rearrange("p (h c) -> p h c", c=P), in1=dect[:], op=ALU.mult), [bps, b_dect], [bAT])
                ps, bps = kb.bank()
                for h in range(4):
                    T(lambda: pe.transpose(out=ps[:, h * P:(h + 1) * P], in_=f32(Qm[0][0][:, h, :]), identity=ident[:]), [Qm[0][1], b_id], [bps])
                A(lambda: acopy(Pm[0][0][:], ps[:].rearrange("p (h c) -> p h c", c=P)), [bps], [Pm[0][1]])
                V(lambda: vec.tensor_tensor(out=Y[:], in0=I4[:], in1=f32(Qm[0][0][:]), op=ALU.subtract), [b_I4, Qm[0][1]], [bY])
                yield from neumann([Pm[0][0], Pm[1][0]], [Pm[0][1], Pm[1][1]], [Qm[0][0], Qm[1][0]], [Qm[0][1], Qm[1][1]], Y, bY, 4)
                G(lambda: pool.tensor_copy(out=Yr[:], in_=Y[:]), [bY], [bYr])
                ps, bps = kb.bank()
                for h in range(4):
                    mm(ps[:, h * P:(h + 1) * P], R2[:, h, :], Yr[:, h, :], True, True, [bR2, bYr], [bps])
                V(lambda: vec.tensor_scalar(out=NWT[:], in0=ps[:].rearrange("p (h c) -> p h c", c=P), scalar1=-1.0, scalar2=None, op0=ALU.mult), [bps], [bNWT])
                yield
                ps, bps = kb.bank()
                for h in range(4):
                    mm(ps[:, h * P:(h + 1) * P], Yr[:, h, :], R1[:, h, :], True, False, [bYr, bR1], [bps])
                    mm(ps[:, h * P:(h + 1) * P], NWT[:, h, :], S[:, h, :], False, True, [bNWT, bS], [bps])
                A(lambda: acopy(VN[:], ps[:].rearrange("p (h c) -> p h c", c=P)), [bps], [bVN])
                ps, bps = kb.bank()
                for h in range(4):
                    mm(ps[:, h * P:(h + 1) * P], QGT[:, h, :], S[:, h, :], True, False, [bQGT, bS], [bps])
                    mm(ps[:, h * P:(h + 1) * P], AT[:, h, :], VN[:, h, :], False, True, [bAT, bVN], [bps])
                V(lambda: vec.tensor_copy(out=O[:], in_=ps[:].rearrange("p (h c) -> p h c", c=P)), [bps], [bO])
                yield
                ps, bps = kb.bank()
                for h in range(4):
                    mm(ps[:, h * P:(h + 1) * P], KG[:, h, :], VN[:, h, :], True, True, [bKG, bVN], [bps])
                V(lambda: vec.tensor_tensor(out=S[:], in0=f32(S[:]), in1=bc(CF[:, 2], P), op=ALU.mult), [bS, bCF], [bS])
                V(lambda: vec.tensor_tensor(out=S[:], in0=f32(S[:]), in1=ps[:].rearrange("p (h c) -> p h c", c=P), op=ALU.add), [bS, bps], [bS])
                kb.dma("pool", OGG[d, t * P:(t + 1) * P, :].rearrange("p (h c) -> p h c", c=P), O[:], [bO], [bOGG])
                yield

    def gdn_finish(l, skip_ctx_out):
        m = kb.mark()
        ng, b_ng = kb.sb([P, 1, P], F32, "ng")
        kb.dma("sp", ng[:, 0, :], I["gdn_norm_g"][l:l + 1, :].partition_broadcast(P), (), [b_ng])
        bufs = [[kb.sb([P, 4, P], F32, "gf") for _ in range(4)] for _ in range(2)]
        ss2 = [kb.sb([P, 4, 1], F32, "gfs") for _ in range(2)]
        for t in range(NTT):
            if skip_ctx_out and t < NCT:
                continue
            (O, bO), (og, bog), (zt_, bz), (tmp, btmp) = bufs[t % 2]
            ss, bss = ss2[t % 2]
            kb.dma("sp", O[:], OGG[0, t * P:(t + 1) * P, :].rearrange("p (h c) -> p h c", c=P), [bOGG], [bO])
            kb.dma("act", og[:], OGG[1, t * P:(t + 1) * P, :].rearrange("p (h c) -> p h c", c=P), [bOGG], [bog])
            kb.dma("sp", zt_[:], PTM[prow(t):prow(t) + P, 0:512].rearrange("p (h c) -> p h c", c=P), [bPTM], [bz])
            V(lambda: vec.tensor_tensor(out=O[:], in0=O[:], in1=og[:], op=ALU.add), [bO, bog], [bO])
            G(lambda: pool.tensor_tensor(out=tmp[:], in0=O[:], in1=O[:], op=ALU.mult), [bO], [btmp])
            V(lambda: vec.tensor_reduce(out=ss[:, :, 0], in_=tmp[:], axis=AX.X, op=ALU.add), [btmp], [bss])
            V(lambda: vec.tensor_scalar(out=ss[:], in0=ss[:], scalar1=1.0 / GDK, scalar2=EPS, op0=ALU.mult, op1=ALU.add), [bss], [bss])
            A(lambda: act.activation(out=ss[:], in_=ss[:], func=AF.Sqrt), [bss], [bss])
            V(lambda: vec.reciprocal(out=ss[:], in_=ss[:]), [bss], [bss])
            V(lambda: vec.tensor_tensor(out=O[:], in0=O[:], in1=bc(ss[:], P), op=ALU.mult), [bO, bss], [bO])
            G(lambda: pool.tensor_tensor(out=O[:], in0=O[:], in1=ng[:].to_broadcast([P, 4, P]), op=ALU.mult), [bO, b_ng], [bO])
            A(lambda: act.activation(out=zt_[:], in_=zt_[:], func=AF.Silu), [bz], [bz])
            V(lambda: vec.tensor_tensor(out=O[:], in0=O[:], in1=zt_[:], op=ALU.mult), [bO, bz], [bO])
            kb.dma("pool", BR[t * P:(t + 1) * P, 0:512].rearrange("p (h c) -> p h c", c=P), O[:], [bO], [bBR])
        kb.release(m)

    def mlstm_chain(l, dsel):
        C, bC = kb.sb([P, 2, 130], F32R, "mC")
        ibt, b_ib = kb.sb([P, 8], F32, "ib")
        fbt, b_fb = kb.sb([P, 8], F32, "fb")
        kb.dma("sp", ibt[:], I["mlstm_i_bias"][l:l + 1].rearrange("o d h -> o (d h)").partition_broadcast(P), (), [b_ib])
        kb.dma("sp", fbt[:], I["mlstm_f_bias"][l:l + 1].rearrange("o d h -> o (d h)").partition_broadcast(P), (), [b_fb])
        ml, bml = kb.sb([P, 1552], F32, "ml")
        CF, bCF = kb.sb([P, 8, 4, 1], F32, "mCF")
        gB, b_gB = kb.sb([P, 4, P], F32, "mgB")
        dect, b_dect = kb.sb([P, 4, P], F32, "mdect")
        kt, bkt = kb.sb([P, 4, 64], F32, "mkt")
        qs, bqs = kb.sb([P, 4, 64], F32, "mqs")
        qg, bqg = kb.sb([P, 4, 64], F32, "mqg")
        kend, bke = kb.sb([P, 4, 64], F32R, "mkend")
        vaug, bva = kb.sb([P, 4, 130], F32R, "vaug")
        ktz, bktz = kb.sb([P, 4, P], F32, "ktz")
        qgz, bqgz = kb.sb([P, 4, P], F32, "qgz")
        KTT, bKTT = kb.sb([P, 4, P], F32R, "KTT")
        QST, bQST = kb.sb([P, 2, P], F32R, "QST")
        QGT, bQGT = kb.sb([P, 4, P], F32R, "QGT")
        G(lambda: pool.memset(ktz[:], 0.0), (), [bktz])
        G(lambda: pool.memset(qgz[:], 0.0), (), [bqgz])
        AT, bAT = kb.sb([P, 4, P], F32R, "mAT")
        dn, bdn = kb.sb([P, 4, 1], F32, "dn")
        Hd, bH = kb.sb([P, 4, P], F32, "Hd")
        o41 = ones[:, 0:4].rearrange("p (a b) -> p a b", b=1)
        V(lambda: vec.tensor_copy(out=vaug[:, :, P:P + 1], in_=o41), [b_on], [bva])
        V(lambda: vec.tensor_scalar(out=vaug[:, :, P + 1:P + 2], in0=o41, scalar1=0.0, scalar2=None, op0=ALU.mult), [b_on], [bva])
        for d in (dsel,):
            V(lambda: vec.tensor_scalar(out=C[:].rearrange("p b c -> p (b c)"), in0=ones[:, 0:1].to_broadcast([P, 260]), scalar1=0.0, scalar2=None, op0=ALU.mult), [b_on], [bC])
            for t in scan_order(d):
                kb.dma("sp", ml[:], PTM[prow(t):prow(t) + P, 2448:4000], [bPTM], [bml])
                V(lambda: vec.tensor_tensor(out=CF[:, 0, :, 0], in0=ml[:, 1536 + d * 4:1540 + d * 4], in1=ibt[:, d * 4:(d + 1) * 4], op=ALU.add), [bml, b_ib], [bCF])
                A(lambda: act.activation(out=CF[:, 0, :, 0], in_=CF[:, 0, :, 0], func=AF.Exp), [bCF], [bCF])
                V(lambda: vec.tensor_tensor(out=CF[:, 1, :, 0], in0=ml[:, 1544 + d * 4:1548 + d * 4], in1=fbt[:, d * 4:(d + 1) * 4], op=ALU.add), [bml, b_fb], [bCF])
                A(lambda: act.activation(out=CF[:, 1, :, 0], in_=CF[:, 1, :, 0], func=AF.Exp, scale=-1.0), [bCF], [bCF])
                V(lambda: vec.tensor_scalar(out=CF[:, 1, :, 0], in0=CF[:, 1, :, 0], scalar1=1.0, scalar2=None, op0=ALU.add), [bCF], [bCF])
                A(lambda: act.activation(out=CF[:, 1, :, 0], in_=CF[:, 1, :, 0], func=AF.Ln), [bCF], [bCF])
                V(lambda: vec.tensor_scalar(out=CF[:, 1, :, 0], in0=CF[:, 1, :, 0], scalar1=-1.0, scalar2=None, op0=ALU.mult), [bCF], [bCF])
                psg, bpsg = kb.bank()
                mm(psg[:, 0:4], MI4[d][:, 0, :], CF[:, 1, :, 0], True, True, [b_c, bCF], [bpsg])
                mm(psg[:, 4:8], ones[:], CF[:, 1, :, 0], True, True, [b_on, bCF], [bpsg])
                V(lambda: vec.tensor_copy(out=CF[:, 2, :, 0], in_=psg[:, 0:4]), [bpsg], [bCF])
                V(lambda: vec.tensor_copy(out=CF[:, 3, :, 0], in_=psg[:, 4:8]), [bpsg], [bCF])
                A(lambda: act.activation(out=CF[:, 4, :, 0], in_=CF[:, 2, :, 0], func=AF.Exp), [bCF], [bCF])
                A(lambda: act.activation(out=CF[:, 5, :, 0], in_=CF[:, 3, :, 0], func=AF.Exp), [bCF], [bCF])
                V(lambda: vec.tensor_tensor(out=CF[:, 6, :, 0], in0=CF[:, 3, :, 0], in1=CF[:, 2, :, 0], op=ALU.subtract), [bCF], [bCF])
                A(lambda: act.activation(out=CF[:, 6, :, 0], in_=CF[:, 6, :, 0], func=AF.Exp), [bCF], [bCF])
                yield
                q3 = ml[:, 0:256].rearrange("p (h c) -> p h c", c=64)
                k3 = ml[:, 256:512].rearrange("p (h c) -> p h c", c=64)
                v3 = ml[:, 512:1024].rearrange("p (h c) -> p h c", c=P)
                V(lambda: vec.tensor_tensor(out=kt[:], in0=k3, in1=bc(CF[:, 0], 64), op=ALU.mult), [bml, bCF], [bkt])
                G(lambda: pool.tensor_scalar(out=qs[:], in0=q3, scalar1=MDQK ** -0.5, scalar2=None, op0=ALU.mult), [bml], [bqs])
                V(lambda: vec.tensor_tensor(out=qg[:], in0=qs[:], in1=bc(CF[:, 4], 64), op=ALU.mult), [bqs, bCF], [bqg])
                V(lambda: vec.tensor_tensor(out=kend[:], in0=kt[:], in1=bc(CF[:, 6], 64), op=ALU.mult), [bkt, bCF], [bke])
                G(lambda: pool.tensor_copy(out=vaug[:, :, 0:P], in_=v3), [bml], [bva])
                G(lambda: pool.tensor_copy(out=gB[:], in_=bc(CF[:, 1], P)), [bCF], [b_gB])
                yield
                for hh in range(2):
                    for (zt2, bz2, src2, bs2, eng2) in ((ktz, bktz, kt, bkt, V), (qgz, bqgz, qg, bqg, G)):
                        o_ = zt2[:].rearrange("p (b h2) (s c) -> p b h2 s c", h2=2, s=2)[:, :, hh, hh, :]
                        i_ = src2[:].rearrange("p (b h2) c -> p b h2 c", h2=2)[:, :, hh, :]
                        eng2(lambda: (vec if eng2 is V else pool).tensor_copy(out=o_, in_=i_), [bs2], [bz2])
                ps, bps = kb.bank()
                for h in range(4):
                    T(lambda: pe.transpose(out=ps[:, h * P:(h + 1) * P], in_=ktz[:, h, :], identity=ident[:]), [bktz, b_id], [bps])
                A(lambda: acopy(KTT[:], ps[:].rearrange("p (b c) -> p b c", c=P)), [bps], [bKTT])
                ps, bps = kb.bank()
                for h in range(4):
                    T(lambda: pe.transpose(out=ps[:, h * P:(h + 1) * P], in_=qgz[:, h, :], identity=ident[:]), [bqgz, b_id], [bps])
                V(lambda: vec.tensor_copy(out=QGT[:], in_=ps[:].rearrange("p (b c) -> p b c", c=P)), [bps], [bQGT])
                ps, bps = kb.bank()
                for b in range(2):
                    T(lambda: pe.transpose(out=ps[:, b * P:(b + 1) * P], in_=qs[:, 2 * b:2 * b + 2, :].rearrange("p h c -> p (h c)"), identity=ident[:]), [bqs, b_id], [bps])
                A(lambda: acopy(QST[:], ps[:, 0:256].rearrange("p (b c) -> p b c", c=P)), [bps], [bQST])
                yield
                decay_T(gB, b_gB, CF[:, 2], bCF, d, dect, b_dect)
                yield
                ps, bps = kb.bank()
                for h in range(4):
                    r0 = (h % 2) * 64
                    mm(ps[:, h * P:(h + 1) * P], KTT[:, h, :], QST[:, h // 2, :], True, True, [bKTT, bQST], [bps])
                V(lambda: vec.tensor_tensor(out=AT[:], in0=ps[:].rearrange("p (h c) -> p h c", c=P), in1=dect[:], op=ALU.mult), [bps, b_dect], [bAT])
                yield
                psn, bpsn = kb.bank()
                psd, bpsd = kb.bank()
                for h in range(4):
                    r0 = (h % 2) * 64
                    mm(psn[:, h * P:(h + 1) * P], QGT[:, h, :], C[:, h // 2, 0:P], True, False, [bQGT, bC], [bpsn])
                    mm(psn[:, h * P:(h + 1) * P], AT[:, h, :], vaug[:, h, 0:P], False, True, [bAT, bva], [bpsn])
                    mm(psd[:, 2 * h:2 * h + 2], QGT[:, h, :], C[:, h // 2, P:P + 2], True, False, [bQGT, bC], [bpsd])
                    mm(psd[:, 2 * h:2 * h + 2], AT[:, h, :], vaug[:, h, P:P + 2], False, True, [bAT, bva], [bpsd])
                A(lambda: act.activation(out=dn[:, :, 0], in_=psd[:, 0:8].rearrange("p (h two) -> p h two", two=2)[:, :, 0], func=AF.Abs), [bpsd], [bdn])
                V(lambda: vec.tensor_scalar(out=dn[:], in0=dn[:], scalar1=1.0, scalar2=None, op0=ALU.max), [bdn], [bdn])
                V(lambda: vec.reciprocal(out=dn[:], in_=dn[:]), [bdn], [bdn])
                V(lambda: vec.tensor_tensor(out=Hd[:], in0=psn[:].rearrange("p (h c) -> p h c", c=P), in1=bc(dn[:], P), op=ALU.mult), [bpsn, bdn], [bH])
                yield
                for b in range(2):
                    ps, bps = kb.bank()
                    mm(ps[:, 0:260], kend[:, 2 * b:2 * b + 2, :].rearrange("p h c -> p (h c)"), vaug[:, 2 * b:2 * b + 2, :].rearrange("p h c -> p (h c)"),
                       True, True, [bke, bva], [bps])
                    for hh in range(2):
                        h = 2 * b + hh
                        r0 = hh * 64
                        V(lambda: vec.scalar_tensor_tensor(out=C[r0:r0 + 64, b, :], in0=f32(C[r0:r0 + 64, b, :]), scalar=CF[r0:r0 + 64, 5, h, :],
                                                           in1=ps[r0:r0 + 64, hh * 130:(hh + 1) * 130], op0=ALU.mult, op1=ALU.add), [bC, bCF, bps], [bC])
                yield
                kb.dma("pool", OGM[d, t * P:(t + 1) * P, :].rearrange("p (h c) -> p h c", c=P), Hd[:], [bH], [bOGM])
                yield

    def mlstm_finish(l, skip_ctx_out):
        m = kb.mark()
        ng, b_ng = kb.sb([P, 1, P], F32, "mng")
        kb.dma("sp", ng[:, 0, :], I["mlstm_norm_g"][l:l + 1, :].partition_broadcast(P), (), [b_ng])
        bufs = [[kb.sb([P, 4, P], F32, "mf") for _ in range(4)] for _ in range(2)]
        dn2 = [kb.sb([P, 4, 1], F32, "mfs") for _ in range(2)]
        for t in range(NTT):
            if skip_ctx_out and t < NCT:
                continue
            (Hd, bH), (og, bog), (og_, bo_), (tmp, btmp) = bufs[t % 2]
            dn, bdn = dn2[t % 2]
            kb.dma("sp", Hd[:], OGM[0, t * P:(t + 1) * P, :].rearrange("p (h c) -> p h c", c=P), [bOGM], [bH])
            kb.dma("act", og[:], OGM[1, t * P:(t + 1) * P, :].rearrange("p (h c) -> p h c", c=P), [bOGM], [bog])
            kb.dma("sp", og_[:], PTM[prow(t):prow(t) + P, 2448 + 1024:2448 + 1536].rearrange("p (h c) -> p h c", c=P), [bPTM], [bo_])
            V(lambda: vec.tensor_tensor(out=Hd[:], in0=Hd[:], in1=og[:], op=ALU.add), [bH, bog], [bH])
            G(lambda: pool.tensor_tensor(out=tmp[:], in0=Hd[:], in1=Hd[:], op=ALU.mult), [bH], [btmp])
            V(lambda: vec.tensor_reduce(out=dn[:, :, 0], in_=tmp[:], axis=AX.X, op=ALU.add), [btmp], [bdn])
            V(lambda: vec.tensor_scalar(out=dn[:], in0=dn[:], scalar1=1.0 / MDV, scalar2=EPS, op0=ALU.mult, op1=ALU.add), [bdn], [bdn])
            A(lambda: act.activation(out=dn[:], in_=dn[:], func=AF.Sqrt), [bdn], [bdn])
            V(lambda: vec.reciprocal(out=dn[:], in_=dn[:]), [bdn], [bdn])
            V(lambda: vec.tensor_tensor(out=Hd[:], in0=Hd[:], in1=bc(dn[:], P), op=ALU.mult), [bH, bdn], [bH])
            G(lambda: pool.tensor_tensor(out=Hd[:], in0=Hd[:], in1=ng[:].to_broadcast([P, 4, P]), op=ALU.mult), [bH, b_ng], [bH])
            A(lambda: act.activation(out=og_[:], in_=og_[:], func=AF.Sigmoid), [bo_], [bo_])
            V(lambda: vec.tensor_tensor(out=Hd[:], in0=Hd[:], in1=og_[:], op=ALU.mult), [bH, bo_], [bH])
            kb.dma("pool", BR[t * P:(t + 1) * P, 1024:1536].rearrange("p (h c) -> p h c", c=P), Hd[:], [bH], [bBR])
        kb.release(m)

    def stage_rwkv_prep(l):
        m = kb.mark()
        def bload(name, shape, src):
            t_, b_ = kb.sb(shape, F32, name)
            kb.dma("sp", t_[:], src, (), [b_])
            return t_, b_
        muB, b_mu = bload("muB", [P, 1920], I["rwkv_mu"][l:l + 1, :].partition_broadcast(P))
        kkB, b_kk = bload("kkB", [P, 512], I["rwkv_k_k"][l:l + 1, :].partition_broadcast(P))
        kaB, b_ka = bload("kaB", [P, 512], I["rwkv_k_a"][l:l + 1, :].partition_broadcast(P))
        rkB, b_rk = bload("rkB", [P, 512], I["rwkv_r_k"][l:l + 1].rearrange("o h n -> o (h n)").partition_broadcast(P))
        w0B, b_w0 = bload("w0B", [P, 2, 512], I["rwkv_w0"][l:l + 1].rearrange("o d n -> o (d n)").partition_broadcast(P))
        a0B, b_a0 = bload("a0B", [P, 2, 512], I["rwkv_a0"][l:l + 1].rearrange("o d n -> o (d n)").partition_broadcast(P))
        gup, b_gup = bload("gup", [P, 512], I["rwkv_g_up"][l])
        WUZ, b_wuz = kb.sb([P, 2, 512], F32, "WUZ")
        AUZ, b_auz = kb.sb([P, 2, 512], F32, "AUZ")
        G(lambda: pool.memset(WUZ[:], 0.0), (), [b_wuz])
        G(lambda: pool.memset(AUZ[:], 0.0), (), [b_auz])
        for d in range(2):
            kb.dma("sp", WUZ[d * 64:(d + 1) * 64, d, :], I["rwkv_w_up"][l, d], (), [b_wuz])
            kb.dma("sp", AUZ[d * 64:(d + 1) * 64, d, :], I["rwkv_a_up"][l, d], (), [b_auz])
        omk, b_omk = kb.sb([P, 512], F32, "omk")
        V(lambda: vec.tensor_scalar(out=omk[:], in0=kaB[:], scalar1=-1.0, scalar2=1.0, op0=ALU.mult, op1=ALU.add), [b_ka], [b_omk])
        pc, bpc = kb.sb([P, 1920], F32, "pc")
        sh = [kb.sb([P, 1920], F32, "sh%d" % i) for i in range(4)]
        qs, bqs = kb.sb([P, 1920], F32, "qs")
        lo, blo = kb.sb([P, 3, P], F32, "lo")
        loT, bloT = kb.sb([P, 3, P], F32, "loT")
        OUTP, bOP = kb.sb([P, 9, 512], F32, "OUTP")
        OUTF, bOF = kb.sb([P, 2, 512], F32, "OUTF")
        ss, bss = kb.sb([P, 8, 1], F32, "rss")
        s2, bs2 = kb.sb([P, 8, 1], F32, "rs2")
        tmp, btmp = kb.sb([P, 512], F32, "rtmp")
        rr, brr = kb.sb([P, 512], F32, "rr")
        offs = (-1, 1, -64, 64)
        for t in range(NTT):
            isc = t < NCT
            r0 = prow(t)
            kb.dma("sp", pc[:], PTM[r0:r0 + P, 528:2448], [bPTM], [bpc])
            qs3 = qs[:].rearrange("p (n f) -> p n f", f=4)
            for cls in range(4):
                if isc and cls >= 2:
                    G(lambda: pool.memset(qs3[:, :, cls], 0.0), (), [bqs])
                    continue
                s_, bs_ = sh[cls]
                kb.dma("act" if cls % 2 else "sp", s_[:], PTM[r0 + offs[cls]:r0 + offs[cls] + P, 528:2448], [bPTM], [bs_])
                s3 = s_[:].rearrange("p (n f) -> p n f", f=4)
                if (not isc) and cls < 2:
                    V(lambda: vec.tensor_scalar(out=qs3[:, :, cls], in0=s3[:, :, cls], scalar1=mLR[:, cls:cls + 1], scalar2=None, op0=ALU.mult), [bs_, b_mLR], [bqs])
                else:
                    G(lambda: pool.tensor_copy(out=qs3[:, :, cls], in_=s3[:, :, cls]), [bs_], [bqs])
            V(lambda: vec.tensor_tensor(out=qs[:], in0=qs[:], in1=pc[:], op=ALU.subtract), [bqs, bpc], [bqs])
            V(lambda: vec.tensor_tensor(out=qs[:], in0=qs[:], in1=muB[:], op=ALU.mult), [bqs, b_mu], [bqs])
            V(lambda: vec.tensor_tensor(out=qs[:], in0=qs[:], in1=pc[:], op=ALU.add), [bqs, bpc], [bqs])
            r_, k_, v_ = qs[:, 0:512], qs[:, 512:1024], qs[:, 1024:1536]
            G(lambda: pool.tensor_copy(out=OUTP[:, 0, :], in_=r_), [bqs], [bOP])
            G(lambda: pool.tensor_copy(out=OUTP[:, 1, :], in_=v_), [bqs], [bOP])
            V(lambda: vec.tensor_tensor(out=OUTP[:, 2, :], in0=k_, in1=kkB[:], op=ALU.mult), [bqs, b_kk], [bOP])
            V(lambda: vec.tensor_tensor(out=tmp[:], in0=OUTP[:, 2, :], in1=OUTP[:, 2, :], op=ALU.mult), [bOP], [btmp])
            V(lambda: vec.tensor_reduce(out=ss[:, :, 0], in_=tmp[:].rearrange("p (h c) -> p h c", c=64), axis=AX.X, op=ALU.add), [btmp], [bss])
            V(lambda: vec.tensor_scalar(out=ss[:], in0=ss[:], scalar1=EPS, scalar2=None, op0=ALU.add), [bss], [bss])
            A(lambda: act.activation(out=ss[:], in_=ss[:], func=AF.Sqrt), [bss], [bss])
            V(lambda: vec.reciprocal(out=ss[:], in_=ss[:]), [bss], [bss])
            kk3 = OUTP[:, 2, :].rearrange("p (h c) -> p h c", c=64)
            V(lambda: vec.tensor_tensor(out=kk3, in0=kk3, in1=bc(ss[:], 64), op=ALU.mult), [bOP, bss], [bOP])
            A(lambda: act.activation(out=lo[:, 0, :], in_=qs[:, 1536:1664], func=AF.Tanh), [bqs], [blo])
            G(lambda: pool.tensor_copy(out=lo[:, 1, :], in_=qs[:, 1664:1792]), [bqs], [blo])
            A(lambda: act.activation(out=lo[:, 2, :], in_=qs[:, 1792:1920], func=AF.Sigmoid), [bqs], [blo])
            ps, bps = kb.bank()
            for j in range(3):
                T(lambda: pe.transpose(out=ps[:, j * P:(j + 1) * P], in_=lo[:, j, :], identity=ident[:]), [blo, b_id], [bps])
            V(lambda: vec.tensor_copy(out=loT[:], in_=ps[:, 0:384].rearrange("p (j c) -> p j c", c=P)), [bps], [bloT])
            ps, bps = kb.bank()
            mm(ps[:], loT[:, 2, :], gup[:], True, True, [bloT, b_gup], [bps])
            A(lambda: acopy(OUTF[:, 1, :], ps[:]), [bps], [bOF])
            V(lambda: vec.tensor_tensor(out=rr[:], in0=r_, in1=rkB[:], op=ALU.mult), [bqs, b_rk], [brr])
            for d in range(2):
                ps, bps = kb.bank()
                mm(ps[:], loT[:, 0, :], WUZ[:, d, :], True, True, [bloT, b_wuz], [bps])
                V(lambda: vec.tensor_tensor(out=tmp[:], in0=ps[:], in1=w0B[:, d, :], op=ALU.add), [bps, b_w0], [btmp])
                A(lambda: act.activation(out=tmp[:], in_=tmp[:], func=AF.Sigmoid), [btmp], [btmp])
                V(lambda: vec.tensor_scalar(out=OUTP[:, 3 + 3 * d, :], in0=tmp[:], scalar1=-math.exp(-0.5), scalar2=None, op0=ALU.mult), [btmp], [bOP])
                ps, bps = kb.bank()
                mm(ps[:], loT[:, 1, :], AUZ[:, d, :], True, True, [bloT, b_auz], [bps])
                V(lambda: vec.tensor_tensor(out=tmp[:], in0=ps[:], in1=a0B[:, d, :], op=ALU.add), [bps, b_a0], [btmp])
                A(lambda: act.activation(out=OUTP[:, 5 + 3 * d, :], in_=tmp[:], func=AF.Sigmoid), [btmp], [bOP])
                V(lambda: vec.tensor_tensor(out=tmp[:], in0=OUTP[:, 5 + 3 * d, :], in1=kaB[:], op=ALU.mult), [bOP, b_ka], [btmp])
                V(lambda: vec.tensor_tensor(out=tmp[:], in0=tmp[:], in1=omk[:], op=ALU.add), [btmp, b_omk], [btmp])
                V(lambda: vec.tensor_tensor(out=OUTP[:, 4 + 3 * d, :], in0=tmp[:], in1=k_, op=ALU.mult), [btmp, bqs], [bOP])
                V(lambda: vec.tensor_tensor(out=tmp[:], in0=rr[:], in1=OUTP[:, 4 + 3 * d, :], op=ALU.mult), [brr, bOP], [btmp])
                V(lambda: vec.tensor_reduce(out=(ss if d == 0 else s2)[:, :, 0], in_=tmp[:].rearrange("p (h c) -> p h c", c=64), axis=AX.X, op=ALU.add),
                  [btmp], [bss if d == 0 else bs2])
            V(lambda: vec.tensor_tensor(out=ss[:], in0=ss[:], in1=s2[:], op=ALU.add), [bss, bs2], [bss])
            V(lambda: vec.tensor_tensor(out=OUTF[:, 0, :].rearrange("p (h c) -> p h c", c=64), in0=v_.rearrange("p (h c) -> p h c", c=64),
                                        in1=bc(ss[:], 64), op=ALU.mult), [bqs, bss], [bOF])
            kb.dma("pool", RWP[t * P:(t + 1) * P, :].rearrange("p (j c) -> p j c", c=512), OUTP[:], [bOP], [bRWP])
            kb.dma("pool", RWF[t * P:(t + 1) * P, :].rearrange("p (j c) -> p j c", c=512), OUTF[:], [bOF], [bRWF])
        kb.release(m)

    def rwkv_chain(l, dsel):
        S, bS = kb.sb([P, 4, 64], F32R, "rS")
        rw, brw = kb.sb([P, 6, 512], F32, "rw")
        E, bE = kb.sb([P, 4, 512], F32, "E")
        X, bX = kb.sb([P, 4, 512], F32, "X")
        XE, bXE = kb.sb([P, 2, 512], F32R, "XE")
        bt_, bbt = kb.sb([P, 512], F32, "btmp")
        vr, bvr = kb.sb([P, 8, 64], F32R, "vr")
        Xz = [kb.sb([P, 8, P], F32, "Xz%d" % i) for i in range(4)]
        XTz = [kb.sb([P, 8, P], F32R, "XTz%d" % i) for i in range(4)]
        XTu = [kb.sb([P, 4, P], F32R, "XTu%d" % i) for i in range(2)]
        for i in range(4):
            G(lambda: pool.memset(Xz[i][0][:], 0.0), (), [Xz[i][1]])
        EWL, bEWL = kb.sb([P, 4], F32, "EWL")
        Pm = [kb.sb([P, 8, P], F32, "rPm") for _ in range(2)]
        Qm = [kb.sb([P, 8, P], F32, "rQm") for _ in range(2)]
        Y, bY = kb.sb([P, 8, P], F32, "rY")
        Yr, bYr = kb.sb([P, 8, P], F32R, "rYr")
        AAK, bAAK = kb.sb([P, 8, P], F32R, "AAK")
        RB, bRB = kb.sb([P, 8, P], F32R, "RB")
        RK, bRK = kb.sb([P, 8, P], F32R, "RK")
        Z, bZ = kb.sb([P, 8, 64], F32R, "Z")
        U, bU = kb.sb([P, 8, 64], F32R, "U")
        Yo, bYo = kb.sb([P, 8, 64], F32, "Yo")
        h3 = lambda ap: ap.rearrange("p (h c) -> p h c", c=64)
        for d in (dsel,):
            V(lambda: vec.tensor_scalar(out=S[:], in0=I4[:, :, 0:64], scalar1=0.0, scalar2=None, op0=ALU.mult), [b_I4], [bS])
            for t in scan_order(d):
                kb.dma("sp", rw[:, 0:3, :], RWP[t * P:(t + 1) * P, 0:1536].rearrange("p (j c) -> p j c", c=512), [bRWP], [brw])
                kb.dma("act", rw[:, 3:6, :], RWP[t * P:(t + 1) * P, (3 + 3 * d) * 512:(6 + 3 * d) * 512].rearrange("p (j c) -> p j c", c=512), [bRWP], [brw])
                r_, v_, kk_, lw_, kd_, a_ = (rw[:, j, :] for j in range(6))
                psA, bpsA = kb.bank()
                mm(psA[:], MI4[d][:, 0, :], lw_, True, True, [b_c, brw], [bpsA])
                psB, bpsB = kb.bank()
                mm(psB[:], ones[:], lw_, True, True, [b_on, brw], [bpsB])
                psC, bpsC = kb.bank()
                for b in range(4):
                    mm(psC[:, b:b + 1], lw_[:, b * P:(b + 1) * P], ones[:, 0:1], True, True, [brw, b_on], [bpsC])
                A(lambda: act.activation(out=EWL[:], in_=psC[:, 0:4], func=AF.Exp), [bpsC], [bEWL])
                A(lambda: act.activation(out=E[:, 0, :], in_=psA[:], func=AF.Exp), [bpsA], [bE])
                A(lambda: act.activation(out=E[:, 1, :], in_=psA[:], func=AF.Exp, scale=-1.0), [bpsA], [bE])
                V(lambda: vec.tensor_tensor(out=E[:, 2, :], in0=psA[:], in1=lw_, op=ALU.subtract), [bpsA, brw], [bE])
                A(lambda: act.activation(out=E[:, 2, :], in_=E[:, 2, :], func=AF.Exp), [bE], [bE])
                V(lambda: vec.tensor_copy(out=E[:, 3, :], in_=psB[:]), [bpsB], [bE])
                V(lambda: vec.tensor_tensor(out=E[:, 3, :], in0=E[:, 3, :], in1=psA[:], op=ALU.subtract), [bE, bpsA], [bE])
                A(lambda: act.activation(out=E[:, 3, :], in_=E[:, 3, :], func=AF.Exp), [bE], [bE])
                yield
                V(lambda: vec.tensor_tensor(out=bt_[:], in0=kk_, in1=a_, op=ALU.mult), [brw], [bbt])
                V(lambda: vec.scalar_tensor_tensor(out=X[:, 0, :], in0=kk_, scalar=-1.0, in1=E[:, 2, :], op0=ALU.mult, op1=ALU.mult), [brw, bE], [bX])
                G(lambda: pool.tensor_tensor(out=X[:, 1, :], in0=bt_[:], in1=E[:, 1, :], op=ALU.mult), [bbt, bE], [bX])
                V(lambda: vec.tensor_tensor(out=X[:, 2, :], in0=kd_, in1=E[:, 1, :], op=ALU.mult), [brw, bE], [bX])
                G(lambda: pool.tensor_tensor(out=X[:, 3, :], in0=r_, in1=E[:, 0, :], op=ALU.mult), [brw, bE], [bX])
                V(lambda: vec.tensor_tensor(out=XE[:, 0, :], in0=kd_, in1=E[:, 3, :], op=ALU.mult), [brw, bE], [bXE])
                G(lambda: pool.tensor_tensor(out=XE[:, 1, :], in0=bt_[:], in1=E[:, 3, :], op=ALU.mult), [bbt, bE], [bXE])
                G(lambda: pool.tensor_copy(out=vr[:], in_=h3(v_)), [brw], [bvr])
                yield
                for i in range(4):
                    xz, bxz = Xz[i]
                    for hh in range(2):
                        o_ = xz[:].rearrange("p (b h2) (s c) -> p b h2 s c", h2=2, s=2)[:, :, hh, hh, :]
                        i_ = X[:, i, :].rearrange("p (b h2 c) -> p b h2 c", h2=2, c=64)[:, :, hh, :]
                        (V if (i + hh) % 2 else G)(lambda: (vec if (i + hh) % 2 else pool).tensor_copy(out=o_, in_=i_), [bX], [bxz])
                    for half in range(2):
                        ps, bps = kb.bank()
                        for h in range(4):
                            T(lambda: pe.transpose(out=ps[:, h * P:(h + 1) * P], in_=xz[:, half * 4 + h, :], identity=ident[:]), [bxz, b_id], [bps])
                        if half:
                            A(lambda: acopy(XTz[i][0][:, 4:8, :], ps[:].rearrange("p (h c) -> p h c", c=P)), [bps], [XTz[i][1]])
                        else:
                            V(lambda: vec.tensor_copy(out=XTz[i][0][:, 0:4, :], in_=ps[:].rearrange("p (h c) -> p h c", c=P)), [bps], [XTz[i][1]])
                for j, i in enumerate((0, 3)):
                    z5 = f32(XTz[i][0][:]).rearrange("p (b h2) c -> p b h2 c", h2=2)
                    G(lambda: pool.tensor_tensor(out=XTu[j][0][:], in0=z5[:, :, 0, :], in1=z5[:, :, 1, :], op=ALU.add), [XTz[i][1]], [XTu[j][1]])
                ATz, BTz, KTz, RTz = (XTz[i][0] for i in range(4))
                bATz, bBTz, bKTz, bRTz = (XTz[i][1] for i in range(4))
                ATu, bATu = XTu[0]
                RTu, bRTu = XTu[1]
                yield
                for (lz, blz, ru, bru, dst, bdst, msk) in ((BTz, bBTz, ATu, bATu, Qm[0][0], Qm[0][1], NMS4[d]), (KTz, bKTz, ATu, bATu, AAK, bAAK, MS4[d]),
                                                            (BTz, bBTz, RTu, bRTu, RB, bRB, MI4[d]), (KTz, bKTz, RTu, bRTu, RK, bRK, MI4[d])):
                    for half in range(2):
                        ps, bps = kb.bank()
                        for h in range(4):
                            hg = half * 4 + h
                            mm(ps[:, h * P:(h + 1) * P], lz[:, hg, :], ru[:, hg // 2, :], True, True, [blz, bru], [bps])
                        V(lambda: vec.tensor_tensor(out=dst[:, half * 4:half * 4 + 4, :], in0=ps[:].rearrange("p (h c) -> p h c", c=P), in1=msk[:], op=ALU.mult),
                          [bps, b_c], [bdst])
                yield
                for half in range(2):
                    ps, bps = kb.bank()
                    for h in range(4):
                        T(lambda: pe.transpose(out=ps[:, h * P:(h + 1) * P], in_=f32(Qm[0][0][:, half * 4 + h, :]), identity=ident[:]), [Qm[0][1], b_id], [bps])
                    A(lambda: acopy(Pm[0][0][:, half * 4:half * 4 + 4, :], ps[:].rearrange("p (h c) -> p h c", c=P)), [bps], [Pm[0][1]])
                    V(lambda: vec.tensor_tensor(out=Y[:, half * 4:half * 4 + 4, :], in0=I4[:], in1=f32(Qm[0][0][:, half * 4:half * 4 + 4, :]), op=ALU.subtract),
                      [b_I4, Qm[0][1]], [bY])
                for half in range(2):
                    sl = slice(half * 4, half * 4 + 4)
                    yield from neumann([Pm[0][0][:, sl, :], Pm[1][0][:, sl, :]], [Pm[0][1], Pm[1][1]], [Qm[0][0][:, sl, :], Qm[1][0][:, sl, :]],
                            [Qm[0][1], Qm[1][1]], Y[:, sl, :], bY, 4)
                G(lambda: pool.tensor_copy(out=Yr[:], in_=Y[:]), [bY], [bYr])
                v8 = vr
                yield
                ps, bps = kb.bank()
                for h in range(8):
                    mm(ps[:, h * 64:(h + 1) * 64], ATz[:, h, :], S[:, h // 2, :], True, False, [bATz, bS], [bps])
                    mm(ps[:, h * 64:(h + 1) * 64], AAK[:, h, :], v8[:, h, :], False, True, [bAAK, bvr], [bps])
                A(lambda: acopy(Z[:], ps[:].rearrange("p (h c) -> p h c", c=64)), [bps], [bZ])
                ps, bps = kb.bank()
                for h in range(8):
                    mm(ps[:, h * 64:(h + 1) * 64], Yr[:, h, :], Z[:, h, :], True, True, [bYr, bZ], [bps])
                V(lambda: vec.tensor_copy(out=U[:], in_=ps[:].rearrange("p (h c) -> p h c", c=64)), [bps], [bU])
                ps, bps = kb.bank()
                for h in range(8):
                    mm(ps[:, h * 64:(h + 1) * 64], RTz[:, h, :], S[:, h // 2, :], True, False, [bRTz, bS], [bps])
                    mm(ps[:, h * 64:(h + 1) * 64], RB[:, h, :], U[:, h, :], False, False, [bRB, bU], [bps])
                    mm(ps[:, h * 64:(h + 1) * 64], RK[:, h, :], v8[:, h, :], False, True, [bRK, bvr], [bps])
                V(lambda: vec.tensor_copy(out=Yo[:], in_=ps[:].rearrange("p (h c) -> p h c", c=64)), [bps], [bYo])
                yield
                for b in range(4):
                    ps, bps = kb.bank()
                    mm(ps[:, 0:P], XE[:, 1, b * P:(b + 1) * P], U[:, 2 * b:2 * b + 2, :].rearrange("p h c -> p (h c)"), True, False, [bXE, bU], [bps])
                    mm(ps[:, 0:P], XE[:, 0, b * P:(b + 1) * P], vr[:, 2 * b:2 * b + 2, :].rearrange("p h c -> p (h c)"), False, True, [bXE, bvr], [bps])
                    for hh in range(2):
                        q0 = hh * 64
                        V(lambda: vec.scalar_tensor_tensor(out=S[q0:q0 + 64, b, :], in0=f32(S[q0:q0 + 64, b, :]), scalar=EWL[q0:q0 + 64, b:b + 1],
                                                           in1=ps[q0:q0 + 64, q0:q0 + 64], op0=ALU.mult, op1=ALU.add), [bS, bEWL, bps], [bS])
                kb.dma("pool", h3(OGR[d, t * P:(t + 1) * P, :]), Yo[:], [bYo], [bOGR])
                yield

    def rwkv_finish(l, skip_ctx_out):
        m = kb.mark()
        h3 = lambda ap: ap.rearrange("p (h c) -> p h c", c=64)
        lngB, b_lg = kb.sb([P, 512], F32, "lngB")
        lnbB, b_lb = kb.sb([P, 512], F32, "lnbB")
        kb.dma("sp", lngB[:], I["rwkv_ln_g"][l:l + 1, :].partition_broadcast(P), (), [b_lg])
        kb.dma("sp", lnbB[:], I["rwkv_ln_b"][l:l + 1, :].partition_broadcast(P), (), [b_lb])
        bufs = [[kb.sb([P, 8, 64], F32, "rf") for _ in range(3)] for _ in range(2)]
        fin2 = [kb.sb([P, 2, 512], F32, "fin") for _ in range(2)]
        st4 = [[kb.sb([P, 8, 1], F32, "rst") for _ in range(2)] for _ in range(2)]
        for t in range(NTT):
            if skip_ctx_out and t < NCT:
                continue
            (Yo, bYo), (og, bog), (tq, btq) = bufs[t % 2]
            fin, bfin = fin2[t % 2]
            (st, bst), (st2, bst2) = st4[t % 2]
            kb.dma("sp", Yo[:], h3(OGR[0, t * P:(t + 1) * P, :]), [bOGR], [bYo])
            kb.dma("act", og[:], h3(OGR[1, t * P:(t + 1) * P, :]), [bOGR], [bog])
            kb.dma("sp", fin[:], RWF[t * P:(t + 1) * P, :].rearrange("p (j c) -> p j c", c=512), [bRWF], [bfin])
            V(lambda: vec.tensor_tensor(out=Yo[:], in0=Yo[:], in1=og[:], op=ALU.add), [bYo, bog], [bYo])
            V(lambda: vec.tensor_reduce(out=st[:, :, 0], in_=Yo[:], axis=AX.X, op=ALU.add), [bYo], [bst])
            V(lambda: vec.tensor_scalar(out=st[:], in0=st[:], scalar1=1.0 / 64, scalar2=None, op0=ALU.mult), [bst], [bst])
            V(lambda: vec.tensor_tensor(out=Yo[:], in0=Yo[:], in1=bc(st[:], 64), op=ALU.subtract), [bYo, bst], [bYo])
            G(lambda: pool.tensor_tensor(out=tq[:], in0=Yo[:], in1=Yo[:], op=ALU.mult), [bYo], [btq])
            V(lambda: vec.tensor_reduce(out=st2[:, :, 0], in_=tq[:], axis=AX.X, op=ALU.add), [btq], [bst2])
            V(lambda: vec.tensor_scalar(out=st2[:], in0=st2[:], scalar1=1.0 / 64, scalar2=GN_EPS, op0=ALU.mult, op1=ALU.add), [bst2], [bst2])
            A(lambda: act.activation(out=st2[:], in_=st2[:], func=AF.Sqrt), [bst2], [bst2])
            V(lambda: vec.reciprocal(out=st2[:], in_=st2[:]), [bst2], [bst2])
            V(lambda: vec.tensor_tensor(out=Yo[:], in0=Yo[:], in1=bc(st2[:], 64), op=ALU.mult), [bYo, bst2], [bYo])
            G(lambda: pool.tensor_tensor(out=Yo[:], in0=Yo[:], in1=h3(lngB[:]), op=ALU.mult), [bYo, b_lg], [bYo])
            V(lambda: vec.tensor_tensor(out=Yo[:], in0=Yo[:], in1=h3(lnbB[:]), op=ALU.add), [bYo, b_lb], [bYo])
            G(lambda: pool.tensor_tensor(out=Yo[:], in0=Yo[:], in1=h3(fin[:, 0, :]), op=ALU.add), [bYo, bfin], [bYo])
            V(lambda: vec.tensor_tensor(out=Yo[:], in0=Yo[:], in1=h3(fin[:, 1, :]), op=ALU.mult), [bYo, bfin], [bYo])
            kb.dma("pool", h3(BR[t * P:(t + 1) * P, 512:1024]), Yo[:], [bYo], [bBR])
        kb.release(m)

    def stage_merge(l, skip_ctx):
        m = kb.mark()
        WBR, bWBR = kb.sb([P, 12, D], BF16, "WBR")
        WO, bWO = kb.sb([P, 8, D], BF16, "WO")
        stg = [kb.sb([P, 4, D], F32, "wstg") for _ in range(2)]
        for i in range(3):
            s_, bs_ = stg[i % 2]
            kb.dma("sp", s_[:], I["w_branch"][l, i].rearrange("(k p) n -> p k n", p=P), (), [bs_])
            (V if i % 2 else G)(lambda: (vec if i % 2 else pool).tensor_copy(out=WBR[:, 4 * i:4 * i + 4, :], in_=s_[:]), [bs_], [bWBR])
        for i in range(2):
            s_, bs_ = stg[(i + 1) % 2]
            kb.dma("sp", s_[:], I["w_out"][l][i * 512:(i + 1) * 512, :].rearrange("(k p) n -> p k n", p=P), (), [bs_])
            (V if i % 2 else G)(lambda: (vec if i % 2 else pool).tensor_copy(out=WO[:, 4 * i:4 * i + 4, :], in_=s_[:]), [bs_], [bWO])
        mset = [dict(br=kb.sb([P, 1536], F32, "br"), brb=kb.sb([P, 1536], BF16, "brb"), brT=kb.sb([P, 12, P], BF16, "brT"),
                     sg=kb.sb([P, 3 * D], F32, "sg"), mg=kb.sb([P, D], F32, "mg"), mgb=kb.sb([P, D], BF16, "mgb"),
                     mT=kb.sb([P, 8, P], BF16, "mT"), tmp=kb.sb([P, 512], F32, "mtmp"), xt=kb.sb([P, D], F32, "mx")) for _ in range(2)]
        load_mods([(1, 2)] if skip_ctx else [(0, 2), (1, 2)])
        for t in range(NTT):
            if skip_ctx and t < NCT:
                continue
            s = 0 if t < NCT else 1
            ms_ = mset[t % 2]
            (br, bbr), (brb, bbrb), (brT, bbrT), (sg, bsg), (mg, bmg) = ms_["br"], ms_["brb"], ms_["brT"], ms_["sg"], ms_["mg"]
            (mgb, bmgb), (mT, bmT), (tmp, btmp), (xt, bx) = ms_["mgb"], ms_["mT"], ms_["tmp"], ms_["xt"]
            kb.dma("sp", br[:], BR[t * P:(t + 1) * P, :], [bBR], [bbr])
            kb.dma("act", sg[:], PTM[prow(t):prow(t) + P, 4000:7072], [bPTM], [bsg])
            kb.dma("sp", xt[:], XR[t * P:(t + 1) * P, :], [bXR], [bx])
            V(lambda: vec.tensor_copy(out=brb[:], in_=br[:]), [bbr], [bbrb])
            A(lambda: act.activation(out=sg[:], in_=sg[:], func=AF.Sigmoid), [bsg], [bsg])
            for half in range(2):
                n_ = 8 if half == 0 else 4
                ps, bps = kb.bank()
                psb = ps[:].bitcast(BF16)
                for k in range(n_):
                    kk_ = half * 8 + k
                    T(lambda: pe.transpose(out=psb[:, k * P:(k + 1) * P], in_=brb[:, kk_ * P:(kk_ + 1) * P], identity=identb[:]), [bbrb, b_idb], [bps])
                A(lambda: acopy(brT[:, half * 8:half * 8 + n_, :], psb[:, 0:n_ * P].rearrange("p (k c) -> p k c", c=P)), [bps], [bbrT])
            for i in range(3):
                for half in range(2):
                    ps, bps = kb.bank()
                    for k in range(4):
                        mm(ps[:], brT[:, 4 * i + k, :], WBR[:, 4 * i + k, half * 512:(half + 1) * 512], k == 0, k == 3, [bbrT, bWBR], [bps])
                    gsl = sg[:, i * D + half * 512:i * D + (half + 1) * 512]
                    if i == 0:
                        V(lambda: vec.tensor_tensor(out=mg[:, half * 512:(half + 1) * 512], in0=ps[:], in1=gsl, op=ALU.mult), [bps, bsg], [bmg])
                    else:
                        V(lambda: vec.tensor_tensor(out=tmp[:], in0=ps[:], in1=gsl, op=ALU.mult), [bps, bsg], [btmp])
                        G(lambda: pool.tensor_tensor(out=mg[:, half * 512:(half + 1) * 512], in0=mg[:, half * 512:(half + 1) * 512], in1=tmp[:], op=ALU.add), [bmg, btmp], [bmg])
            V(lambda: vec.tensor_copy(out=mgb[:], in_=mg[:]), [bmg], [bmgb])
            ps, bps = kb.bank()
            psb = ps[:].bitcast(BF16)
            for k in range(8):
                T(lambda: pe.transpose(out=psb[:, k * P:(k + 1) * P], in_=mgb[:, k * P:(k + 1) * P], identity=identb[:]), [bmgb, b_idb], [bps])
            A(lambda: acopy(mT[:], psb.rearrange("p (k c) -> p k c", c=P)), [bps], [bmT])
            for half in range(2):
                ps, bps = kb.bank()
                for k in range(8):
                    mm(ps[:], mT[:, k, :], WO[:, k, half * 512:(half + 1) * 512], k == 0, k == 7, [bmT, bWO], [bps])
                V(lambda: vec.tensor_tensor(out=tmp[:], in0=ps[:], in1=MOD[(s, 2)][0][:, half * 512:(half + 1) * 512], op=ALU.mult), [bps, MOD[(s, 2)][1]], [btmp])
                V(lambda: vec.tensor_tensor(out=xt[:, half * 512:(half + 1) * 512], in0=xt[:, half * 512:(half + 1) * 512], in1=tmp[:], op=ALU.add), [bx, btmp], [bx])
            kb.dma("pool", XR[t * P:(t + 1) * P, :], xt[:], [bx], [bXR])
        kb.release(m)

    H2D, bH2D = scratch("H2D", [NT, D], BF16)
    YA, bYA = scratch("YA", [NT, D])

    def moe_wcast_chain(l):
        stg = [kb.sb([P, 4, 512], F32, "estg") for _ in range(2)]
        stb = [kb.sb([P, 4, 512], BF16, "estb") for _ in range(2)]
        ci = 0
        for e in range(NE):
            for (src, dst, bdst) in ((I["moe_w1"][l, e], W1B[e], bW1B), (I["moe_w3"][l, e], W3B[e], bW3B)):
                for hf in range(2):
                    s_, bs_ = stg[ci % 2]
                    b_, bb_ = stb[ci % 2]
                    kb.dma("sp", s_[:], src[hf * 512:(hf + 1) * 512, :].rearrange("(k p) n -> p k n", p=P), (), [bs_])
                    if ci % 2:
                        A(lambda: acopy(b_[:], s_[:]), [bs_], [bb_])
                    else:
                        G(lambda: pool.tensor_copy(out=b_[:], in_=s_[:]), [bs_], [bb_])
                    kb.dma("act", dst[hf * 512:(hf + 1) * 512, :].rearrange("(k p) n -> p k n", p=P), b_[:], [bb_], [bdst])
                    ci += 1
                    yield
            for hf in range(2):
                s_, bs_ = stg[ci % 2]
                b_, bb_ = stb[ci % 2]
                sv = s_[:].rearrange("p k n -> p (k n)").rearrange("p (k n) -> p k n", k=2)
                bv = b_[:].rearrange("p k n -> p (k n)").rearrange("p (k n) -> p k n", k=2)
                kb.dma("sp", sv, I["moe_w2"][l, e][hf * 256:(hf + 1) * 256, :].rearrange("(k p) n -> p k n", p=P), (), [bs_])
                if ci % 2:
                    A(lambda: acopy(bv, sv), [bs_], [bb_])
                else:
                    G(lambda: pool.tensor_copy(out=bv, in_=sv), [bs_], [bb_])
                kb.dma("act", W2B[e][hf * 256:(hf + 1) * 256, :].rearrange("(k p) n -> p k n", p=P), bv, [bb_], [bW2B])
                ci += 1
                yield

    def stage_moe(l, last):
        m = kb.mark()
        load_mods([(1, 3), (1, 4), (1, 5)] + ([] if last else [(0, 3), (0, 4), (0, 5)]))
        RW, bRW = kb.sb([P, 8, NE], F32, "RW")
        with nc.allow_non_contiguous_dma(reason="router w"):
            kb.dma("sp", RW[:], I["router_w"][l].rearrange("(k p) e -> p k e", p=P), (), [bRW])
        CAPM = max(1, 2 * TL // NE)
        PROBT, bPT = kb.sb([NE, NT], F32, "PROBT")
        xt2 = [kb.sb([P, D], F32, "ex") for _ in range(2)]
        sc2 = [kb.sb([P, 4], F32, "esc") for _ in range(2)]
        jk2 = [kb.sb([P, D], F32, "ejk") for _ in range(2)]
        zer, bzer = kb.sb([P, D], F32, "zer")
        G(lambda: pool.memset(zer[:], 0.0), (), [bzer])
        m_p1 = kb.mark()
        p1 = [dict(h2f=kb.sb([P, D], F32, "h2f"), hb=kb.sb([P, D], BF16, "ehb"), h2T=kb.sb([P, 8, P], F32, "h2T"),
                   lg=kb.sb([P, NE], F32, "lg"), sm=kb.sb([P, 4], F32, "sm")) for _ in range(2)]
        t_start = NCT if last else 0
        for t in range(t_start, NTT):
            s = 0 if t < NCT else 1
            (xt, bx), (sc, bsc) = xt2[t % 2], sc2[t % 2]
            q_ = p1[t % 2]
            (h2f, bh2), (hb, bhb), (h2T, bh2T), (lg, blg), (sm, bsm) = q_["h2f"], q_["hb"], q_["h2T"], q_["lg"], q_["sm"]
            kb.dma("sp", xt[:], XR[t * P:(t + 1) * P, :], [bXR], [bx])
            kb.dma("act", YA[t * P:(t + 1) * P, :], zer[:], [bzer], [bYA])
            norm_mod(xt[:], bx, s, 3 * D, 4 * D, h2f[:], bh2, sc, bsc, jk=jk2[t % 2])
            G(lambda: pool.tensor_copy(out=hb[:], in_=h2f[:]), [bh2], [bhb])
            kb.dma("pool", H2D[t * P:(t + 1) * P, :], hb[:], [bhb], [bH2D])
            for half in range(2):
                ps, bps = kb.bank()
                for k in range(4):
                    T(lambda: pe.transpose(out=ps[:, k * P:(k + 1) * P], in_=h2f[:, (half * 4 + k) * P:(half * 4 + k + 1) * P], identity=ident[:]), [bh2, b_id], [bps])
                (A if half else V)(lambda: (acopy(h2T[:, 4:8, :], ps[:].rearrange("p (k c) -> p k c", c=P)) if half else
                                            vec.tensor_copy(out=h2T[:, 0:4, :], in_=ps[:].rearrange("p (k c) -> p k c", c=P))), [bps], [bh2T])
            ps, bps = kb.bank()
            for k in range(8):
                mm(ps[:, 0:NE], h2T[:, k, :], RW[:, k, :], k == 0, k == 7, [bh2T, bRW], [bps])
            V(lambda: vec.tensor_copy(out=lg[:], in_=ps[:, 0:NE]), [bps], [blg])
            V(lambda: vec.tensor_reduce(out=sm[:, 0:1], in_=lg[:], axis=AX.X, op=ALU.max), [blg], [bsm])
            V(lambda: vec.tensor_scalar(out=sm[:, 1:2], in0=sm[:, 0:1], scalar1=-1.0, scalar2=None, op0=ALU.mult), [bsm], [bsm])
            A(lambda: act.activation(out=lg[:], in_=lg[:], func=AF.Exp, bias=sm[:, 1:2], accum_out=sm[:, 2:3]), [blg, bsm], [blg, bsm])
            V(lambda: vec.reciprocal(out=sm[:, 3:4], in_=sm[:, 2:3]), [bsm], [bsm])
            V(lambda: vec.tensor_scalar(out=lg[:], in0=lg[:], scalar1=sm[:, 3:4], scalar2=None, op0=ALU.mult), [blg, bsm], [blg])
            ps, bps = kb.bank()
            T(lambda: pe.transpose(out=ps[0:NE, 0:P], in_=lg[:], identity=ident[:]), [blg, b_id], [bps])
            V(lambda: vec.tensor_copy(out=PROBT[:, t * P:(t + 1) * P], in_=ps[0:NE, 0:P]), [bps], [bPT])
        kb.release(m_p1)
        WORK, bWK = kb.sb([NE, TL], F32, "WORK")
        VAL, bVAL = kb.sb([NE, CAPM], F32, "VAL")
        IDX, bIDX = kb.sb([NE, CAPM], U32, "IDX")
        IDXF, bIDXF = kb.sb([NE, CAPM], F32, "IDXF")
        NST = (CAPM + P - 1) // P
        IDXT, bIDXT = kb.sb([P, NST, NE], I32, "IDXT")
        AFFT, bAFFT = kb.sb([P, NST, NE], F32, "AFFT")
        w13 = [kb.sb([P, 2, 8, 512], BF16, "w13") for _ in range(2)]
        w2 = [kb.sb([P, 4, D], BF16, "w2") for _ in range(2)]
        xe = [kb.sb([P, D], BF16, "xe") for _ in range(2)]
        xeT, bxeT = kb.sb([P, 8, CAPM], BF16, "xeT")
        g1, bg1 = kb.sb([P, CAPM], F32, "g1")
        hid, bhid = kb.sb([P, 4, CAPM], BF16, "hid")
        ye = [kb.sb([P, D], F32, "ye") for _ in range(2)]
        wi = 0
        xi = 0
        for (a0, a1) in ((0, TC), (TC, NT)):
            if last and a0 == 0:
                continue
            n_ = a1 - a0
            cap = max(1, 2 * n_ // NE)
            V(lambda: vec.tensor_copy(out=WORK[:, 0:n_], in_=PROBT[:, a0:a1]), [bPT], [bWK])
            for r in range(cap // 8):
                V(lambda: vec.max(out=VAL[:, r * 8:(r + 1) * 8], in_=WORK[:, 0:n_]), [bWK], [bVAL])
                V(lambda: vec.max_index(out=IDX[:, r * 8:(r + 1) * 8], in_max=VAL[:, r * 8:(r + 1) * 8], in_values=WORK[:, 0:n_]), [bWK, bVAL], [bIDX])
                V(lambda: vec.match_replace(out=WORK[:, 0:n_], in_to_replace=VAL[:, r * 8:(r + 1) * 8], in_values=WORK[:, 0:n_], imm_value=-1.0), [bWK, bVAL], [bWK])
            V(lambda: vec.tensor_copy(out=IDXF[:, 0:cap], in_=IDX[:, 0:cap]), [bIDX], [bIDXF])
            if a0:
                V(lambda: vec.tensor_scalar(out=IDXF[:, 0:cap], in0=IDXF[:, 0:cap], scalar1=float(a0), scalar2=None, op0=ALU.add), [bIDXF], [bIDXF])
            sts = [(st, min(P, cap - st * P)) for st in range((cap + P - 1) // P)]
            for (st, w) in sts:
                ps, bps = kb.bank()
                T(lambda: pe.transpose(out=ps[0:w, 0:NE], in_=IDXF[:, st * P:st * P + w], identity=ident[0:NE, 0:NE]), [bIDXF, b_id], [bps])
                T(lambda: pe.transpose(out=ps[0:w, NE:2 * NE], in_=VAL[:, st * P:st * P + w], identity=ident[0:NE, 0:NE]), [bVAL, b_id], [bps])
                V(lambda: vec.tensor_copy(out=IDXT[0:w, st, :], in_=ps[0:w, 0:NE]), [bps], [bIDXT])
                V(lambda: vec.tensor_copy(out=AFFT[0:w, st, :], in_=ps[0:w, NE:2 * NE]), [bps], [bAFFT])
            for e in range(NE):
                wa, bwa = w13[wi % 2]
                wb_, bwb_ = w2[wi % 2]
                wi += 1
                kb.dma("sp", wa[:, 0], W1B[e].rearrange("(k p) n -> p k n", p=P), [bW1B], [bwa])
                kb.dma("act", wa[:, 1], W3B[e].rearrange("(k p) n -> p k n", p=P), [bW3B], [bwa])
                kb.dma("sp", wb_[:], W2B[e].rearrange("(k p) n -> p k n", p=P), [bW2B], [bwb_])
                for (st, w) in sts:
                    x_, bx_ = xe[xi % 2]
                    xi += 1
                    kb.idma(x_[0:w, :], None, H2D[:, :], bass.IndirectOffsetOnAxis(ap=IDXT[0:w, st, e:e + 1], axis=0), [bH2D, bIDXT], [bx_])
                    ps, bps = kb.bank()
                    psb = ps[:].bitcast(BF16)
                    for k in range(8):
                        T(lambda: pe.transpose(out=psb[:, k * P:k * P + w], in_=x_[0:w, k * P:(k + 1) * P], identity=identb[0:w, 0:w]), [bx_, b_idb], [bps])
                    A(lambda: acopy(xeT[:, :, st * P:st * P + w], psb.rearrange("p (k c) -> p k c", c=P)[:, :, 0:w]), [bps], [bxeT])
                for f in range(4):
                    ps1, bps1 = kb.bank()
                    for k in range(8):
                        mm(ps1[:, 0:cap], wa[:, 0, k, f * P:(f + 1) * P], xeT[:, k, 0:cap], k == 0, k == 7, [bwa, bxeT], [bps1])
                    ps3, bps3 = kb.bank()
                    for k in range(8):
                        mm(ps3[:, 0:cap], wa[:, 1, k, f * P:(f + 1) * P], xeT[:, k, 0:cap], k == 0, k == 7, [bwa, bxeT], [bps3])
                    A(lambda: act.activation(out=g1[:, 0:cap], in_=ps1[:, 0:cap], func=AF.Silu), [bps1], [bg1])
                    V(lambda: vec.tensor_tensor(out=hid[:, f, 0:cap], in0=g1[:, 0:cap], in1=ps3[:, 0:cap], op=ALU.mult), [bg1, bps3], [bhid])
                for (st, w) in sts:
                    y_, by_ = ye[xi % 2]
                    xi += 1
                    for half in range(2):
                        ps, bps = kb.bank()
                        for f in range(4):
                            mm(ps[0:w, :], hid[:, f, st * P:st * P + w], wb_[:, f, half * 512:(half + 1) * 512], f == 0, f == 3, [bhid, bwb_], [bps])
                        (V if half else A)(lambda: (vec.tensor_scalar(out=y_[0:w, half * 512:(half + 1) * 512], in0=ps[0:w, :], scalar1=AFFT[0:w, st, e:e + 1], scalar2=None, op0=ALU.mult)
                                                    if half else act.activation(out=y_[0:w, half * 512:(half + 1) * 512], in_=ps[0:w, :], func=AF.Copy, scale=AFFT[0:w, st, e:e + 1])),
                                           [bps, bAFFT], [by_])
                    kb.idma(YA[:, :], bass.IndirectOffsetOnAxis(ap=IDXT[0:w, st, e:e + 1], axis=0), y_[0:w, :], None, [by_, bIDXT], [bYA], compute_op=ALU.add)
        acc2 = [kb.sb([P, D], F32, "acc") for _ in range(2)]
        fgB, b_fg = kb.sb([P, D], F32, "fgB")
        if last:
            kb.dma("sp", fgB[:], I["final_g"][0:1, :].partition_broadcast(P), (), [b_fg])
        for t in range(t_start, NTT):
            s = 0 if t < NCT else 1
            (xt, bx), (sc, bsc), (acc, bacc) = xt2[t % 2], sc2[t % 2], acc2[t % 2]
            kb.dma("sp", xt[:], XR[t * P:(t + 1) * P, :], [bXR], [bx])
            kb.dma("act", acc[:], YA[t * P:(t + 1) * P, :], [bYA], [bacc])
            V(lambda: vec.tensor_tensor(out=acc[:], in0=acc[:], in1=MOD[(s, 5)][0][:], op=ALU.mult), [bacc, MOD[(s, 5)][1]], [bacc])
            V(lambda: vec.tensor_tensor(out=xt[:], in0=xt[:], in1=acc[:], op=ALU.add), [bx, bacc], [bx])
            if not last:
                kb.dma("pool", XR[t * P:(t + 1) * P, :], xt[:], [bx], [bXR])
            else:
                junk, bj = jk2[t % 2]
                A(lambda: act.activation(out=junk[:], in_=xt[:], func=AF.Square, accum_out=sc[:, 0:1]), [bx], [bj, bsc])
                V(lambda: vec.tensor_scalar(out=sc[:, 1:2], in0=sc[:, 0:1], scalar1=1.0 / D, scalar2=EPS, op0=ALU.mult, op1=ALU.add), [bsc], [bsc])
                A(lambda: act.activation(out=sc[:, 2:3], in_=sc[:, 1:2], func=AF.Sqrt), [bsc], [bsc])
                V(lambda: vec.reciprocal(out=sc[:, 3:4], in_=sc[:, 2:3]), [bsc], [bsc])
                V(lambda: vec.scalar_tensor_tensor(out=xt[:], in0=xt[:], scalar=sc[:, 3:4], in1=fgB[:], op0=ALU.mult, op1=ALU.mult), [bx, bsc, b_fg], [bx])
                kb.dma("pool", OUT[(t - NCT) * P:(t - NCT + 1) * P, :], xt[:], [bx], [bOUT])
        kb.release(m)

    for l in range(depth):
        last = (l == depth - 1)
        stage_mod(l)
        if upto >= 1:
            stage_proj(l)
        if upto >= 2:
            stage_conv(l)
        if upto >= 3:
            m_ = kb.mark()
            kb.run_chains([gdn_chain(l, 0), gdn_chain(l, 1)] + ([moe_wcast_chain(l)] if upto >= 7 else []))
            kb.release(m_)
            gdn_finish(l, last)
            stage_rwkv_prep(l)
            for d_ in range(2):
                m_ = kb.mark()
                kb.run_chains([rwkv_chain(l, d_), mlstm_chain(l, d_)])
                kb.release(m_)
            rwkv_finish(l, last)
            mlstm_finish(l, last)
        if upto >= 6:
            stage_merge(l, last)
        if upto >= 7:
            stage_moe(l, last)
    kb.barrier()
    kb.finish([bOUT, bXR, bPTM, bPQT, bQKV, bOGG, bOGM, bOGR, bBR, bRWP, bRWF])
    return kb


_CACHE = {}


def run(inputs, TC, TL, depth, ncores, dbg=(), upto=99):
    in_maps = []
    for b in range(ncores):
        m = {"x": np.ascontiguousarray(inputs["x"][b]), "c": np.ascontiguousarray(inputs["c"][b:b + 1]),
             "ctx": np.ascontiguousarray(inputs["ctx"][b]), "c_ctx": np.ascontiguousarray(np.asarray(inputs["c_ctx"])[None, :])}
        for n in WNAMES:
            a = np.asarray(inputs[n])
            m[n] = a if a.ndim > 1 else np.ascontiguousarray(a[None, :])
        in_maps.append(m)
    shapes = {n: tuple(in_maps[0][n].shape) for n in WNAMES}
    kb = build(TC, TL, depth, shapes, dbg, upto)
    res = run_bass_kernel_spmd(kb.nc, in_maps, core_ids=list(range(ncores)))
    return res.results, kb


def kernel(**inputs):
    inputs = {k: np.asarray(v) for k, v in inputs.items()}
    B, TL, _ = inputs["x"].shape
    TC = inputs["ctx"].shape[1]
    depth = inputs["w_in"].shape[0]
    results, _ = run(inputs, TC, TL, depth, B)
    return np.stack([r["out"] for r in results], axis=0).astype(np.float32)
```
